# Optimizing a Trainium2 kernel written in Bass

```python
import math
import jax, jax.numpy as jnp
from jax import lax
import numpy as np

D_MODEL = 1024
BATCH = 8
SEQ = 4096
DEPTH = 2

DEEPNORM_ALPHA = (2 * DEPTH) ** 0.25
DEEPNORM_BETA = (8 * DEPTH) ** -0.25
LN_EPS = 1e-5
N_EVEN = (DEPTH + 1) // 2
N_ODD = DEPTH // 2

GDN_HEADS = D_MODEL // 256
GDN_KDIM = 128
GDN_VDIM = 128
GDN_W = GDN_HEADS * GDN_VDIM
CONV_K = 4
GDN_CHUNK = 64

S5_W = D_MODEL // 2
S5_GROUP_CH = 16
S5_GROUPS = S5_W // S5_GROUP_CH
S5_STATE = 64

MIX_W = GDN_W + S5_W
IN_AB = 4 * GDN_W + 2 * GDN_HEADS + S5_W

ATTN_HEAD_DIM = 64
ATTN_HEADS = D_MODEL // ATTN_HEAD_DIM
DILATED_CONFIGS = ((128, 1), (512, 4), (2048, 16))

MOE_GROUPS = 4
EXPERTS_PER_GROUP = 8
N_EXPERTS = MOE_GROUPS * EXPERTS_PER_GROUP
EXPERT_FF = D_MODEL // 2
MOE_TOP_K = 2
MOE_BLOCK = 128

kernel_name = "hybrid_gdn_s5_dilated_hmoe_trunk"


def _layer_norm(x, g, b):
    xf = x.astype(jnp.float32)
    mu = xf.mean(-1, keepdims=True)
    var = jnp.square(xf - mu).mean(-1, keepdims=True)
    return ((xf - mu) * lax.rsqrt(var + LN_EPS) * g.astype(jnp.float32) + b.astype(jnp.float32)).astype(x.dtype)


def _l2norm(t):
    return t * lax.rsqrt(jnp.sum(t * t, axis=-1, keepdims=True) + 1e-6)


def _causal_depthwise_conv(x, w):
    k = w.shape[0]
    return lax.conv_general_dilated(x, w[:, None, :], window_strides=(1,), padding=((k - 1, 0),),
                                    dimension_numbers=('NWC', 'WIO', 'NWC'), feature_group_count=x.shape[-1])


def _chunked_gated_delta_rule(q, k, v, g, beta):
    bsz, s, h, dk = q.shape
    dv = v.shape[-1]
    n = s // GDN_CHUNK

    def to_chunks(t):
        return t.reshape(bsz, n, GDN_CHUNK, h, t.shape[-1]).transpose(0, 3, 1, 2, 4)

    q = to_chunks(q) * (dk ** -0.5)
    k = to_chunks(k)
    v = to_chunks(v)
    g = to_chunks(g[..., None])[..., 0]
    beta = to_chunks(beta[..., None])[..., 0]
    gc = jnp.cumsum(g, axis=-1)
    idx = jnp.arange(GDN_CHUNK)
    causal = idx[:, None] >= idx[None, :]
    strict = idx[:, None] > idx[None, :]
    decay = jnp.exp(jnp.where(causal, gc[..., :, None] - gc[..., None, :], -jnp.inf))
    kb = k * beta[..., None]
    lower = jnp.where(strict, jnp.einsum('bhnik,bhnjk->bhnij', kb, k) * decay, 0.0)
    eye = jnp.eye(GDN_CHUNK, dtype=jnp.float32)
    rhs = jnp.concatenate([v * beta[..., None], kb * jnp.exp(gc)[..., None]], axis=-1)
    sol = lax.linalg.triangular_solve(lower + eye, rhs, left_side=True, lower=True, unit_diagonal=True)
    u, w = sol[..., :dv], sol[..., dv:]
    qk = jnp.where(causal, jnp.einsum('bhnik,bhnjk->bhnij', q, k) * decay, 0.0)
    q_dec = q * jnp.exp(gc)[..., None]
    k_dec = k * jnp.exp(gc[..., -1:] - gc)[..., None]
    g_last = jnp.exp(gc[..., -1])
    xs = (jnp.moveaxis(u, 2, 0), jnp.moveaxis(w, 2, 0), jnp.moveaxis(qk, 2, 0),
          jnp.moveaxis(q_dec, 2, 0), jnp.moveaxis(k_dec, 2, 0), jnp.moveaxis(g_last, 2, 0))

    def step(state, inp):
        u_n, w_n, qk_n, qd_n, kd_n, gl_n = inp
        v_new = u_n - jnp.einsum('bhck,bhkv->bhcv', w_n, state)
        o_n = jnp.einsum('bhck,bhkv->bhcv', qd_n, state) + jnp.einsum('bhij,bhjv->bhiv', qk_n, v_new)
        state = state * gl_n[..., None, None] + jnp.einsum('bhck,bhcv->bhkv', kd_n, v_new)
        return state, o_n

    state0 = jnp.zeros((bsz, h, dk, dv), jnp.float32)
    _, o = lax.scan(step, state0, xs)
    return o.transpose(1, 0, 3, 2, 4).reshape(bsz, s, h, dv)


def _gated_deltanet(qkv_in, z, b_logit, a_logit, conv_w, a_log, dt_bias, norm_w):
    bsz, s, _ = qkv_in.shape
    qkv = jax.nn.silu(_causal_depthwise_conv(qkv_in, conv_w)).astype(jnp.float32)
    q, k, v = jnp.split(qkv, 3, axis=-1)
    q = _l2norm(q.reshape(bsz, s, GDN_HEADS, GDN_KDIM))
    k = _l2norm(k.reshape(bsz, s, GDN_HEADS, GDN_KDIM))
    v = v.reshape(bsz, s, GDN_HEADS, GDN_VDIM)
    beta = jax.nn.sigmoid(b_logit.astype(jnp.float32))
    g = -jnp.exp(a_log.astype(jnp.float32)) * jax.nn.softplus(a_logit.astype(jnp.float32) + dt_bias.astype(jnp.float32))
    o = _chunked_gated_delta_rule(q, k, v, g, beta)
    zh = z.astype(jnp.float32).reshape(bsz, s, GDN_HEADS, GDN_VDIM)
    o = o * lax.rsqrt(jnp.mean(o * o, axis=-1, keepdims=True) + 1e-6) * norm_w.astype(jnp.float32) * jax.nn.silu(zh)
    return o.reshape(bsz, s, GDN_W)


def _s5_layer(u, lam_re, lam_im, log_dt, b_re, b_im, c_re, c_im, d_skip, w_glu):
    bsz, s, _ = u.shape
    uf = u.astype(jnp.float32)
    ug = uf.reshape(bsz, s, S5_GROUPS, S5_GROUP_CH)
    dt = jnp.exp(log_dt.astype(jnp.float32))[:, None]
    lr, li = lam_re.astype(jnp.float32), lam_im.astype(jnp.float32)
    mag = jnp.exp(lr * dt)
    a_re, a_im = mag * jnp.cos(li * dt), mag * jnp.sin(li * dt)
    den = lr * lr + li * li
    nr, ni = a_re - 1.0, a_im
    cr = (nr * lr + ni * li) / den
    ci = (ni * lr - nr * li) / den
    br, bi = b_re.astype(jnp.float32), b_im.astype(jnp.float32)
    bb_re = cr[..., None] * br - ci[..., None] * bi
    bb_im = cr[..., None] * bi + ci[..., None] * br
    bu_re = jnp.einsum('bsgh,gph->bsgp', ug, bb_re)
    bu_im = jnp.einsum('bsgh,gph->bsgp', ug, bb_im)
    a_re_t = jnp.broadcast_to(a_re, (1, s) + a_re.shape)
    a_im_t = jnp.broadcast_to(a_im, (1, s) + a_im.shape)

    def combine(e1, e2):
        a1r, a1i, b1r, b1i = e1
        a2r, a2i, b2r, b2i = e2
        return (a2r * a1r - a2i * a1i, a2r * a1i + a2i * a1r,
                a2r * b1r - a2i * b1i + b2r, a2r * b1i + a2i * b1r + b2i)

    _, _, h_re, h_im = lax.associative_scan(combine, (a_re_t, a_im_t, bu_re, bu_im), axis=1)
    y = (jnp.einsum('gkp,bsgp->bsgk', c_re.astype(jnp.float32), h_re)
         - jnp.einsum('gkp,bsgp->bsgk', c_im.astype(jnp.float32), h_im))
    y = y.reshape(bsz, s, S5_W) + d_skip.astype(jnp.float32) * uf
    y = jax.nn.gelu(y)
    return y * jax.nn.sigmoid(y @ w_glu.astype(jnp.float32))


def _mixer_ab(x, w_in, conv_w, a_log, dt_bias, norm_w, lam_re, lam_im, log_dt,
              b_re, b_im, c_re, c_im, d_skip, w_glu, w_out):
    proj = x @ w_in
    qkv, z, b_logit, a_logit, u = jnp.split(
        proj, [3 * GDN_W, 4 * GDN_W, 4 * GDN_W + GDN_HEADS, 4 * GDN_W + 2 * GDN_HEADS], axis=-1)
    ya = _gated_deltanet(qkv, z, b_logit, a_logit, conv_w, a_log, dt_bias, norm_w)
    yb = _s5_layer(u, lam_re, lam_im, log_dt, b_re, b_im, c_re, c_im, d_skip, w_glu)
    y = jnp.concatenate([ya, yb], axis=-1).astype(x.dtype)
    return y @ w_out


def _dilated_branch(q, k, v, dilation, steps):
    bsz, s, h, e = q.shape
    length = s // dilation
    nb = -(-length // steps)
    lp = nb * steps

    def by_residue(t):
        t = t.reshape(bsz, length, dilation, h, e).transpose(0, 2, 1, 3, 4)
        t = jnp.pad(t, ((0, 0), (0, 0), (0, lp - length), (0, 0), (0, 0)))
        return t.reshape(bsz, dilation, nb, steps, h, e)

    def with_prev(t):
        prev = jnp.pad(t, ((0, 0), (0, 0), (1, 0), (0, 0), (0, 0), (0, 0)))[:, :, :-1]
        return jnp.concatenate([prev, t], axis=3)

    qb = by_residue(q)
    kk = with_prev(by_residue(k))
    vv = with_prev(by_residue(v))
    scores = jnp.einsum('bdnqhe,bdnkhe->bdhnqk', qb, kk).astype(jnp.float32) * (e ** -0.5)
    i = jnp.arange(steps)[:, None]
    j = jnp.arange(2 * steps)[None, :]
    dist = steps + i - j
    blk = jnp.arange(nb)[:, None, None]
    valid = (dist >= 0) & (dist <= steps) & (blk * steps + j - steps >= 0)
    scores = jnp.where(valid, scores, -1e30)
    m = jnp.max(scores, axis=-1, keepdims=True)
    p = jnp.exp(scores - m)
    ssum = jnp.sum(p, axis=-1, keepdims=True)
    out = jnp.einsum('bdhnqk,bdnkhe->bdnqhe', p / ssum, vv.astype(jnp.float32))
    lse = (m + jnp.log(ssum))[..., 0]
    out = out.reshape(bsz, dilation, lp, h, e)[:, :, :length].transpose(0, 2, 1, 3, 4).reshape(bsz, s, h, e)
    lse = lse.transpose(0, 1, 3, 4, 2).reshape(bsz, dilation, lp, h)[:, :, :length]
    lse = lse.transpose(0, 2, 1, 3).reshape(bsz, s, h)
    return out, lse


def _mixer_c(x, w_qkv, w_out):
    bsz, s, _ = x.shape
    qkv = (x @ w_qkv).reshape(bsz, s, 3, ATTN_HEADS, ATTN_HEAD_DIM)
    q, k, v = qkv[:, :, 0], qkv[:, :, 1], qkv[:, :, 2]
    outs, lses = [], []
    for window, dilation in DILATED_CONFIGS:
        o, l = _dilated_branch(q, k, v, dilation, window // dilation)
        outs.append(o)
        lses.append(l)
    wts = jax.nn.softmax(jnp.stack(lses, axis=0), axis=0)
    o = jnp.einsum('cbsh,cbshe->bshe', wts, jnp.stack(outs, axis=0))
    return o.reshape(bsz, s, D_MODEL).astype(x.dtype) @ w_out


def _hier_moe(x, wg_r, bg_r, we_r, be_r, w_gate, w_up, w_down):
    bsz, s, d = x.shape
    n = bsz * s
    xt = x.reshape(n, d)
    group_prob = jax.nn.softmax((xt @ wg_r + bg_r).astype(jnp.float32), axis=-1)
    p_group, group = lax.top_k(group_prob, 1)
    expert_logits = (xt @ we_r + be_r).astype(jnp.float32).reshape(n, MOE_GROUPS, EXPERTS_PER_GROUP)
    in_group = jnp.einsum('ng,nge->ne', jax.nn.one_hot(group[:, 0], MOE_GROUPS, dtype=jnp.float32), expert_logits)
    top_logit, top_local = lax.top_k(in_group, MOE_TOP_K)
    gate = p_group * jax.nn.softmax(top_logit, axis=-1)
    expert = group * EXPERTS_PER_GROUP + top_local
    nk = n * MOE_TOP_K
    flat_expert = expert.reshape(nk)
    flat_token = jnp.arange(nk, dtype=jnp.int32) // MOE_TOP_K
    flat_gate = gate.reshape(nk)
    order = jnp.argsort(flat_expert)
    sorted_expert = flat_expert[order]
    counts = jnp.bincount(flat_expert, length=N_EXPERTS)
    padded = (counts + MOE_BLOCK - 1) // MOE_BLOCK * MOE_BLOCK
    start = jnp.cumsum(counts) - counts
    padded_end = jnp.cumsum(padded)
    padded_start = padded_end - padded
    slot = padded_start[sorted_expert] + jnp.arange(nk) - start[sorted_expert]
    n_slots = (-(-nk // MOE_BLOCK) + N_EXPERTS) * MOE_BLOCK
    n_blocks = n_slots // MOE_BLOCK
    slot_token = jnp.zeros((n_slots,), jnp.int32).at[slot].set(flat_token[order])
    slot_gate = jnp.zeros((n_slots,), jnp.float32).at[slot].set(flat_gate[order])
    block_expert = jnp.minimum(
        jnp.searchsorted(padded_end, jnp.arange(n_blocks) * MOE_BLOCK, side='right'), N_EXPERTS - 1)
    xb = xt[slot_token].reshape(n_blocks, MOE_BLOCK, d)

    def expert_block(args):
        xi, e = args
        hdn = jax.nn.silu(xi @ w_gate[e]) * (xi @ w_up[e])
        return hdn @ w_down[e]

    yb = lax.map(expert_block, (xb, block_expert))
    y = yb.reshape(n_slots, d) * slot_gate[:, None].astype(yb.dtype)
    out = jnp.zeros((n, d), y.dtype).at[slot_token].add(y)
    return out.reshape(bsz, s, d)


def setup_inputs(seed: int = 0) -> dict:
    key = jax.random.key(seed)
    ks = iter(jax.random.split(key, 48))
    f32 = jnp.float32

    def nrm(shape, scale):
        return jax.random.normal(next(ks), shape, f32) * scale

    def unif(shape, lo, hi):
        return jax.random.uniform(next(ks), shape, f32, lo, hi)

    x = nrm((BATCH, SEQ, D_MODEL), 1.0)
    w_in_ab = nrm((N_EVEN, D_MODEL, IN_AB), D_MODEL ** -0.5)
    conv_qkv = nrm((N_EVEN, CONV_K, 3 * GDN_W), CONV_K ** -0.5)
    gdn_a_log = jnp.log(unif((N_EVEN, GDN_HEADS), 1.0, 16.0))
    dt0 = jnp.exp(unif((N_EVEN, GDN_HEADS), math.log(1e-3), math.log(1e-1)))
    gdn_dt_bias = dt0 + jnp.log(-jnp.expm1(-dt0))
    gdn_norm = 1.0 + nrm((N_EVEN, GDN_VDIM), 0.02)
    s5_lam_re = -0.5 + nrm((N_EVEN, S5_GROUPS, S5_STATE), 0.01)
    s5_lam_im = (math.pi * jnp.arange(S5_STATE, dtype=f32))[None, None, :] + nrm((N_EVEN, S5_GROUPS, S5_STATE), 0.01)
    s5_log_dt = unif((N_EVEN, S5_GROUPS), math.log(1e-3), math.log(1e-1))
    s5_b_re = nrm((N_EVEN, S5_GROUPS, S5_STATE, S5_GROUP_CH), (2 * S5_GROUP_CH) ** -0.5)
    s5_b_im = nrm((N_EVEN, S5_GROUPS, S5_STATE, S5_GROUP_CH), (2 * S5_GROUP_CH) ** -0.5)
    s5_c_re = nrm((N_EVEN, S5_GROUPS, S5_GROUP_CH, S5_STATE), S5_STATE ** -0.5)
    s5_c_im = nrm((N_EVEN, S5_GROUPS, S5_GROUP_CH, S5_STATE), S5_STATE ** -0.5)
    s5_d = nrm((N_EVEN, S5_W), 1.0)
    s5_w_glu = nrm((N_EVEN, S5_W, S5_W), S5_W ** -0.5)
    w_out_ab = nrm((N_EVEN, MIX_W, D_MODEL), MIX_W ** -0.5 * DEEPNORM_BETA)
    w_qkv_c = nrm((N_ODD, D_MODEL, 3 * D_MODEL), D_MODEL ** -0.5)
    w_out_c = nrm((N_ODD, D_MODEL, D_MODEL), D_MODEL ** -0.5 * DEEPNORM_BETA)
    ln_mix_g = 1.0 + nrm((DEPTH, D_MODEL), 0.02)
    ln_mix_b = nrm((DEPTH, D_MODEL), 0.02)
    router_group_w = nrm((DEPTH, D_MODEL, MOE_GROUPS), D_MODEL ** -0.5)
    router_group_b = nrm((DEPTH, MOE_GROUPS), 0.01)
    router_expert_w = nrm((DEPTH, D_MODEL, N_EXPERTS), D_MODEL ** -0.5)
    router_expert_b = nrm((DEPTH, N_EXPERTS), 0.01)
    moe_w_gate = nrm((DEPTH, N_EXPERTS, D_MODEL, EXPERT_FF), D_MODEL ** -0.5)
    moe_w_up = nrm((DEPTH, N_EXPERTS, D_MODEL, EXPERT_FF), D_MODEL ** -0.5)
    moe_w_down = nrm((DEPTH, N_EXPERTS, EXPERT_FF, D_MODEL), EXPERT_FF ** -0.5 * DEEPNORM_BETA)
    ln_ffn_g = 1.0 + nrm((DEPTH, D_MODEL), 0.02)
    ln_ffn_b = nrm((DEPTH, D_MODEL), 0.02)
    return {
        "x": x, "w_in_ab": w_in_ab, "conv_qkv": conv_qkv, "gdn_a_log": gdn_a_log,
        "gdn_dt_bias": gdn_dt_bias, "gdn_norm": gdn_norm, "s5_lam_re": s5_lam_re,
        "s5_lam_im": s5_lam_im, "s5_log_dt": s5_log_dt, "s5_b_re": s5_b_re, "s5_b_im": s5_b_im,
        "s5_c_re": s5_c_re, "s5_c_im": s5_c_im, "s5_d": s5_d, "s5_w_glu": s5_w_glu,
        "w_out_ab": w_out_ab, "w_qkv_c": w_qkv_c, "w_out_c": w_out_c,
        "ln_mix_g": ln_mix_g, "ln_mix_b": ln_mix_b,
        "router_group_w": router_group_w, "router_group_b": router_group_b,
        "router_expert_w": router_expert_w, "router_expert_b": router_expert_b,
        "moe_w_gate": moe_w_gate, "moe_w_up": moe_w_up, "moe_w_down": moe_w_down,
        "ln_ffn_g": ln_ffn_g, "ln_ffn_b": ln_ffn_b,
    }


def reference(x, w_in_ab, conv_qkv, gdn_a_log, gdn_dt_bias, gdn_norm, s5_lam_re, s5_lam_im,
              s5_log_dt, s5_b_re, s5_b_im, s5_c_re, s5_c_im, s5_d, s5_w_glu, w_out_ab,
              w_qkv_c, w_out_c, ln_mix_g, ln_mix_b, router_group_w, router_group_b,
              router_expert_w, router_expert_b, moe_w_gate, moe_w_up, moe_w_down,
              ln_ffn_g, ln_ffn_b):
    h = x
    for layer in range(DEPTH):
        i = layer // 2
        if layer % 2 == 0:
            mix = _mixer_ab(h, w_in_ab[i], conv_qkv[i], gdn_a_log[i], gdn_dt_bias[i], gdn_norm[i],
                            s5_lam_re[i], s5_lam_im[i], s5_log_dt[i], s5_b_re[i], s5_b_im[i],
                            s5_c_re[i], s5_c_im[i], s5_d[i], s5_w_glu[i], w_out_ab[i])
        else:
            mix = _mixer_c(h, w_qkv_c[i], w_out_c[i])
        h = _layer_norm(DEEPNORM_ALPHA * h + mix, ln_mix_g[layer], ln_mix_b[layer])
        ffn = _hier_moe(h, router_group_w[layer], router_group_b[layer], router_expert_w[layer],
                        router_expert_b[layer], moe_w_gate[layer], moe_w_up[layer], moe_w_down[layer])
        h = _layer_norm(DEEPNORM_ALPHA * h + ffn, ln_ffn_g[layer], ln_ffn_b[layer])
    return h
```

```python
import numpy as np
from contextlib import ExitStack, contextmanager
import concourse.bass as bass
import concourse.mybir as mybir
from concourse.bass_utils import run_bass_kernel_spmd

F32 = mybir.dt.float32
BF16 = mybir.dt.bfloat16
I32 = mybir.dt.int32
AF = mybir.ActivationFunctionType
ALU = mybir.AluOpType
AX = mybir.AxisListType

ENGS = ("pe", "act", "dve", "pool", "sp")
DBG = {}
NPOOL = 64
NPOOL_SW = 24

T = 4096
NT = 32
D = 1024
KD = 8
NE = 32
FF = 512
CAP = 384
NB = CAP // 128
NSLOT = NE * CAP
ALPHA = 4.0 ** 0.25
LN_EPS = 1e-5


class Res:
    __slots__ = ("name", "w", "r")

    def __init__(self, name=""):
        self.name = name
        self.w = None
        self.r = []


class Stream:
    def __init__(self, sem):
        self.sem = sem
        self.n = 0


class Op:
    __slots__ = ("eng", "fn", "deps", "stream", "sig", "val", "done")

    def __init__(self, eng, fn, stream):
        self.eng = eng
        self.fn = fn
        self.deps = []
        self.stream = stream
        self.sig = stream is not None
        self.val = None
        self.done = False


class Sched:
    def __init__(self, nc, stack):
        self.nc = nc
        self.stack = stack
        self.ops = {e: [] for e in ENGS}
        self.sems = {e: stack.enter_context(nc.semaphore("sem_" + e)) for e in ENGS if e != "sp"}
        self.cnt = {e: 0 for e in ENGS}
        self.obs = {e: {} for e in ENGS}
        self.streams = [Stream(stack.enter_context(nc.semaphore("dmap%d" % i))) for i in range(NPOOL)]
        for st_ in self.streams:
            st_.last_op = None
        self.pool_i = 0
        self.pool_sw = 0
        self.last = {e: None for e in ENGS}
        self.nstream = 0

    def stream(self, name=""):
        return True

    def op(self, eng, fn, reads=(), writes=(), stream=None):
        deps = set()
        if stream is not None:
            if eng == "pool":
                stream = self.streams[self.pool_sw % NPOOL_SW]
                self.pool_sw += 1
            else:
                stream = self.streams[NPOOL_SW + self.pool_i % (NPOOL - NPOOL_SW)]
                self.pool_i += 1
            if stream.last_op is not None:
                deps.add(stream.last_op)
        o = Op(eng, fn, stream)
        for r in reads:
            if r.w is not None:
                deps.add(r.w)
        for w in writes:
            if w.w is not None:
                deps.add(w.w)
            for rr in w.r:
                deps.add(rr)
        for d in deps:
            if d is o or d.done:
                continue
            if d.eng == eng and eng == "pe" and d.stream is None and stream is None:
                continue
            d.sig = True
            o.deps.append((d, None))
        for r in reads:
            r.r.append(o)
        for w in writes:
            w.w = o
            w.r = []
        if stream is not None:
            stream.n += 1
            o.val = 16 * stream.n
            stream.last_op = o
        self.ops[eng].append(o)
        self.last[eng] = o
        return o

    def barrier(self):
        lasts = [self.last[e] for e in ENGS if self.last[e] is not None and self.last[e].stream is None]
        stream_last = []
        seen = set()
        for e in ENGS:
            for o in reversed(self.ops[e]):
                if o.stream is not None and id(o.stream) not in seen:
                    seen.add(id(o.stream))
                    stream_last.append(o)
        for e in ENGS:
            o = Op(e, None, None)
            for d in lasts + stream_last:
                if d.eng == e and d.stream is None and e == "pe":
                    continue
                d.sig = True
                o.deps.append((d, None))
            self.ops[e].append(o)

    def flush(self, final=False):
        nc = self.nc
        for e in ENGS:
            for o in self.ops[e]:
                if o.stream is None and o.sig and o.fn is not None:
                    assert e != "sp"
                    self.cnt[e] += 1
                    o.val = self.cnt[e]
        sems, obs, ops, streams = self.sems, self.obs, self.ops, self.streams

        def emit(e, engobj):
            ob = obs[e]
            for o in ops[e]:
                for d, need in o.deps:
                    sem = d.stream.sem if d.stream is not None else sems[d.eng]
                    val = need if need is not None else d.val
                    key = id(sem)
                    if ob.get(key, 0) >= val:
                        continue
                    ob[key] = val
                    engobj.wait_ge(sem, val)
                if o.fn is None:
                    continue
                ins = o.fn(engobj)
                if o.stream is not None:
                    ins.then_inc(o.stream.sem, 16)
                elif o.sig:
                    ins.then_inc(sems[e], 1)
            if final and e == "sp":
                for st in streams:
                    if st.n > 0:
                        engobj.wait_ge(st.sem, 16 * st.n)

        with nc.Block() as block:
            @block.tensor
            def _(t):
                emit("pe", t)

            @block.scalar
            def _(t):
                emit("act", t)

            @block.vector
            def _(t):
                emit("dve", t)

            @block.gpsimd
            def _(t):
                emit("pool", t)

            @block.sync
            def _(t):
                emit("sp", t)
        for e in ENGS:
            for o in self.ops[e]:
                o.done = True
        self.ops = {e: [] for e in ENGS}
        self.last = {e: None for e in ENGS}

    @contextmanager
    def phase(self, final=False):
        with ExitStack() as st:
            nc = self.nc

            class A:
                pass
            a = A()
            def _nm(n):
                self.nstream += 1
                return "%s_u%d" % (n, self.nstream)
            a.sb = lambda n, sh, dt=F32: st.enter_context(nc.sbuf_tensor(_nm(n), list(sh), dt))
            a.ps = lambda n, sh, dt=F32: st.enter_context(nc.psum_tensor(_nm(n), list(sh), dt))
            yield a
            self.barrier()
            self.flush(final=final)


def interleave(gen_fns, depth):
    active = []
    it = iter(gen_fns)
    exhausted = False
    while True:
        if not exhausted and len(active) < depth:
            try:
                active.append(next(it)())
            except StopIteration:
                exhausted = True
        if not active:
            if exhausted:
                break
            continue
        for g in list(active):
            try:
                next(g)
            except StopIteration:
                active.remove(g)


class Tl:
    def __init__(self, t, name=""):
        self.t = t
        self.r = Res(name)


def make_consts(S, a):
    c = {}
    ident = Tl(a.sb("ident", [128, 128], F32))
    S.op("pool", lambda e: e.memset(ident.t[:], 0.0), writes=[ident.r])
    S.op("pool", lambda e: e.affine_select(out=ident.t[:], in_=ident.t[:], pattern=[[-1, 128]],
                                            compare_op=ALU.not_equal, fill=1.0, base=0, channel_multiplier=1),
         reads=[ident.r], writes=[ident.r])
    c["ident"] = ident
    identb = Tl(a.sb("identb", [128, 128], BF16))
    S.op("pool", lambda e: e.tensor_copy(identb.t[:], ident.t[:]), reads=[ident.r], writes=[identb.r])
    c["identb"] = identb
    return c


def layer_norm_gen(S, r, out, g, b, stats, mv, rstd):
    for j in range(2):
        S.op("dve", lambda e, j=j: e.bn_stats(stats.t[:, j, :], r.t[:, j * 512:(j + 1) * 512]),
             reads=[r.r], writes=[stats.r])
    S.op("dve", lambda e: e.bn_aggr(mv.t[:], stats.t[:]), reads=[stats.r], writes=[mv.r])
    S.op("dve", lambda e: e.tensor_scalar(out=rstd.t[:, 0:1], in0=mv.t[:, 1:2], scalar1=LN_EPS, scalar2=None,
                                          op0=ALU.add), reads=[mv.r], writes=[rstd.r])
    S.op("act", lambda e: e.sqrt(rstd.t[:, 0:1], rstd.t[:, 0:1]), reads=[rstd.r], writes=[rstd.r])
    yield
    S.op("dve", lambda e: e.reciprocal(rstd.t[:, 0:1], rstd.t[:, 0:1]), reads=[rstd.r], writes=[rstd.r])
    S.op("dve", lambda e: e.scalar_tensor_tensor(out=rstd.t[:, 1:2], in0=mv.t[:, 0:1], scalar=-1.0, in1=rstd.t[:, 0:1], op0=ALU.mult, op1=ALU.mult),
         reads=[mv.r, rstd.r], writes=[rstd.r])
    S.op("act", lambda e: e.activation(out=out.t[:], in_=r.t[:], func=AF.Identity, scale=rstd.t[:, 0:1], bias=rstd.t[:, 1:2]),
         reads=[r.r, rstd.r], writes=[out.r])
    yield
    S.op("dve", lambda e: e.tensor_tensor(out=out.t[:], in0=out.t[:], in1=g.t[:], op=ALU.mult),
         reads=[out.r, g.r], writes=[out.r])
    S.op("dve", lambda e: e.tensor_tensor(out=out.t[:], in0=out.t[:], in1=b.t[:], op=ALU.add),
         reads=[out.r, b.r], writes=[out.r])


def moe_stage(nc, S, h_in, h_out, W, l, scr, Rh_in, Rh_out):
    Xs, Ys = scr["Xs"], scr["Ys"]
    RXs, RYs = Res("Xs"), Res("Ys")

    with ExitStack() as st0:
        slots_f = Tl(st0.enter_context(nc.sbuf_tensor("slots_f%d" % l, [128, NT, 2], F32)))
        gates = Tl(st0.enter_context(nc.sbuf_tensor("gates%d" % l, [128, NT, 2], F32)))
        slot_r = [Res() for _ in range(NT)]
        gate_r = [Res() for _ in range(NT)]

        with S.phase() as a:
            c = make_consts(S, a)
            ident = c["ident"]
            su = Tl(a.sb("su", [128, 128], BF16))
            onesb = Tl(a.sb("onesb", [128, 128], BF16))
            ones1 = Tl(a.sb("ones1", [1, 128], F32))
            ebase = Tl(a.sb("ebase", [128, NE], F32))
            S.op("pool", lambda e: e.memset(su.t[:], 1.0), writes=[su.r])
            S.op("pool", lambda e: e.affine_select(out=su.t[:], in_=su.t[:], pattern=[[1, 128]],
                                                    compare_op=ALU.is_gt, fill=0.0, base=0, channel_multiplier=-1),
                 reads=[su.r], writes=[su.r])
            S.op("pool", lambda e: e.memset(onesb.t[:], 1.0), writes=[onesb.r])
            S.op("pool", lambda e: e.memset(ones1.t[:], 1.0), writes=[ones1.r])
            S.op("pool", lambda e: e.iota(ebase.t[:], pattern=[[CAP, NE]], base=0, channel_multiplier=0,
                                          allow_small_or_imprecise_dtypes=True), writes=[ebase.r])
            pdump = Tl(a.sb("pdump", [128, 1], F32))
            S.op("pool", lambda e: e.iota(pdump.t[:], pattern=[[0, 1]], base=NSLOT, channel_multiplier=1,
                                          allow_small_or_imprecise_dtypes=True), writes=[pdump.r])
            wr = Tl(a.sb("wr", [128, KD, 36], F32))
            brow = Tl(a.sb("brow", [1, 36], F32))
            ws = S.stream("wr")
            S.op("sp", lambda e: e.dma_start(out=wr.t[:, :, 0:4], in_=W["router_group_w"][l].rearrange("(k p) g -> p k g", p=128)),
                 writes=[wr.r], stream=ws)
            S.op("sp", lambda e: e.dma_start(out=wr.t[:, :, 4:36], in_=W["router_expert_w"][l].rearrange("(k p) g -> p k g", p=128)),
                 writes=[wr.r], stream=ws)
            S.op("sp", lambda e: e.dma_start(out=brow.t[:, 0:4], in_=W["router_group_b"][l:l + 1, :]), writes=[brow.r], stream=ws)
            S.op("sp", lambda e: e.dma_start(out=brow.t[:, 4:36], in_=W["router_expert_b"][l:l + 1, :]), writes=[brow.r], stream=ws)
            zt = Tl(a.sb("zt", [128, 8192], BF16))
            S.op("pool", lambda e: e.memset(zt.t[:], 0.0), writes=[zt.r])
            zs = S.stream("zs")
            Xs_v = Xs[0:NSLOT, :].rearrange("(c p r) d -> c p (r d)", p=128, r=8)
            for ci in range(NSLOT // 1024):
                S.op("act", lambda e, ci=ci: e.dma_start(out=Xs_v[ci], in_=zt.t[:]), reads=[zt.r], writes=[RXs], stream=zs)

            asum = Tl(a.sb("asum", [128, NE], BF16))
            S.op("dve", lambda e: e.memset(asum.t[:], 0.0), writes=[asum.r])

            NBUF = 3
            ht = [Tl(a.sb("ht%d" % i, [128, D], F32)) for i in range(NBUF)]
            hb = [Tl(a.sb("hb%d" % i, [128, D], BF16)) for i in range(NBUF)]
            hT = [Tl(a.sb("hT%d" % i, [128, KD, 128], F32)) for i in range(NBUF)]
            tp = [Tl(a.ps("tp%d" % i, [128, 2, 512], F32)) for i in range(2)]
            sm = [Tl(a.ps("sm%d" % i, [128, 512], F32)) for i in range(NBUF)]
            ld = [S.stream("ld%d" % i) for i in range(NBUF)]
            sc = [[S.stream("sc%d_%d" % (i, k)) for k in range(2)] for i in range(NBUF)]
            def smalls(i):
                d = {}
                for nm, sh, dt in [("L", [128, 36], F32), ("gmax", [128, 1], F32), ("negm", [128, 1], F32),
                                   ("ohg", [128, 4], F32), ("gexp", [128, 4], F32), ("gsum", [128, 1], F32),
                                   ("pg", [128, 1], F32), ("sel", [128, 4, 8], F32), ("ing", [128, 8], F32),
                                   ("m1", [128, 1], F32), ("oh1", [128, 8], F32), ("msk", [128, 8], F32),
                                   ("m2", [128, 1], F32), ("oh2", [128, 8], F32), ("dd", [128, 1], F32),
                                   ("ee", [128, 1], F32), ("g12", [128, 2], F32), ("A1", [128, 4, 8], F32),
                                   ("A2", [128, 4, 8], F32), ("Ab", [128, NE], BF16), ("posb", [128, NE], F32),
                                   ("tmp", [128, NE], F32), ("sp4", [128, 4], F32), ("ovf", [128, 2], F32),
                                   ("slf", [128, 2], F32), ("nov", [128, 2], F32), ("si0", [128, 1], I32), ("si1", [128, 1], I32)]:
                    d[nm] = Tl(a.sb("%s_%d" % (nm, i), sh, dt))
                return d
            sml = [smalls(i) for i in range(NBUF)]

            def rt_gen(i):
                bi = i % NBUF
                h, hbb, hTt, tpp, smm, w = ht[bi], hb[bi], hT[bi], tp[i % 2], sm[bi], sml[bi]
                S.op("sp", lambda e, i=i, h=h: e.dma_start(out=h.t[:], in_=h_in[i * 128:(i + 1) * 128, :]),
                     reads=[Rh_in], writes=[h.r], stream=ld[bi])
                for k in range(KD):
                    S.op("pe", lambda e, k=k, h=h, tpp=tpp: e.transpose(tpp.t[:, k // 4, (k % 4) * 128:(k % 4 + 1) * 128],
                                                                        h.t[:, k * 128:(k + 1) * 128], ident.t[:]),
                         reads=[h.r, ident.r], writes=[tpp.r])
                for j in range(2):
                    S.op("act", lambda e, j=j, hTt=hTt, tpp=tpp: e.copy(hTt.t[:, j * 4:(j + 1) * 4, :].rearrange("p k t -> p (k t)"), tpp.t[:, j, :]),
                         reads=[tpp.r], writes=[hTt.r])
                S.op("act", lambda e, h=h, hbb=hbb: e.copy(hbb.t[:], h.t[:]), reads=[h.r], writes=[hbb.r])
                yield
                lg = smm.t[:, 0:36]
                pos = smm.t[:, 64:96]
                for k in range(KD):
                    S.op("pe", lambda e, k=k, hTt=hTt, lg=lg: e.matmul(lg, lhsT=hTt.t[:, k, :], rhs=wr.t[:, k, :], start=(k == 0), stop=False),
                         reads=[hTt.r, wr.r], writes=[smm.r])
                S.op("pe", lambda e, lg=lg: e.matmul(lg, lhsT=ones1.t[:, :], rhs=brow.t[:, :], start=False, stop=True),
                     reads=[ones1.r, brow.r], writes=[smm.r])
                yield
                V = lambda fn, rd, wrt: S.op("dve", fn, reads=[x.r for x in rd], writes=[x.r for x in wrt])
                L = w["L"]
                V(lambda e, L=L, lg=lg: e.tensor_copy(L.t[:], lg), [smm], [L])
                gl = L.t[:, 0:4]
                el = L.t[:, 4:36].rearrange("p (g x) -> p g x", g=4)
                V(lambda e, w=w, gl=gl: e.reduce_max(out=w["gmax"].t[:], in_=gl, axis=AX.X), [L], [w["gmax"]])
                V(lambda e, w=w: e.tensor_scalar(out=w["negm"].t[:], in0=w["gmax"].t[:], scalar1=-1.0, scalar2=None, op0=ALU.mult), [w["gmax"]], [w["negm"]])
                V(lambda e, w=w, gl=gl: e.tensor_scalar(out=w["ohg"].t[:], in0=gl, scalar1=w["gmax"].t[:, 0:1], scalar2=None, op0=ALU.is_equal), [L, w["gmax"]], [w["ohg"]])
                S.op("act", lambda e, w=w, gl=gl: e.activation(out=w["gexp"].t[:], in_=gl, func=AF.Exp, bias=w["negm"].t[:, 0:1], scale=1.0, accum_out=w["gsum"].t[:, 0:1]),
                     reads=[L.r, w["negm"].r], writes=[w["gexp"].r, w["gsum"].r])
                yield
                V(lambda e, w=w: e.reciprocal(w["pg"].t[:], w["gsum"].t[:]), [w["gsum"]], [w["pg"]])
                V(lambda e, w=w, el=el: e.tensor_tensor(out=w["sel"].t[:], in0=el, in1=w["ohg"].t[:].unsqueeze(2).to_broadcast([128, 4, 8]), op=ALU.mult), [L, w["ohg"]], [w["sel"]])
                V(lambda e, w=w: e.reduce_sum(out=w["ing"].t[:], in_=w["sel"].t[:].rearrange("p g x -> p x g"), axis=AX.X), [w["sel"]], [w["ing"]])
                V(lambda e, w=w: e.reduce_max(out=w["m1"].t[:], in_=w["ing"].t[:], axis=AX.X), [w["ing"]], [w["m1"]])
                V(lambda e, w=w: e.tensor_scalar(out=w["oh1"].t[:], in0=w["ing"].t[:], scalar1=w["m1"].t[:, 0:1], scalar2=None, op0=ALU.is_equal), [w["ing"], w["m1"]], [w["oh1"]])
                V(lambda e, w=w: e.scalar_tensor_tensor(out=w["msk"].t[:], in0=w["oh1"].t[:], scalar=-1e30, in1=w["ing"].t[:], op0=ALU.mult, op1=ALU.add), [w["oh1"], w["ing"]], [w["msk"]])
                V(lambda e, w=w: e.reduce_max(out=w["m2"].t[:], in_=w["msk"].t[:], axis=AX.X), [w["msk"]], [w["m2"]])
                V(lambda e, w=w: e.tensor_scalar(out=w["oh2"].t[:], in0=w["msk"].t[:], scalar1=w["m2"].t[:, 0:1], scalar2=None, op0=ALU.is_equal), [w["msk"], w["m2"]], [w["oh2"]])
                V(lambda e, w=w: e.tensor_tensor(out=w["dd"].t[:], in0=w["m2"].t[:], in1=w["m1"].t[:], op=ALU.subtract), [w["m1"], w["m2"]], [w["dd"]])
                S.op("act", lambda e, w=w: e.activation(out=w["ee"].t[:], in_=w["dd"].t[:], func=AF.Exp), reads=[w["dd"].r], writes=[w["ee"].r])
                yield
                V(lambda e, w=w: e.tensor_scalar(out=w["dd"].t[:], in0=w["ee"].t[:], scalar1=1.0, scalar2=None, op0=ALU.add), [w["ee"]], [w["dd"]])
                V(lambda e, w=w: e.reciprocal(w["g12"].t[:, 0:1], w["dd"].t[:]), [w["dd"]], [w["g12"]])
                V(lambda e, w=w: e.tensor_tensor(out=w["g12"].t[:, 1:2], in0=w["g12"].t[:, 0:1], in1=w["ee"].t[:], op=ALU.mult), [w["g12"], w["ee"]], [w["g12"]])
                V(lambda e, w=w: e.tensor_scalar(out=w["g12"].t[:], in0=w["g12"].t[:], scalar1=w["pg"].t[:, 0:1], scalar2=None, op0=ALU.mult), [w["g12"], w["pg"]], [w["g12"]])
                for nm, oh in (("A1", "oh1"), ("A2", "oh2")):
                    V(lambda e, w=w, nm=nm, oh=oh: e.tensor_tensor(out=w[nm].t[:], in0=w["ohg"].t[:].unsqueeze(2).to_broadcast([128, 4, 8]),
                                                                    in1=w[oh].t[:].unsqueeze(1).to_broadcast([128, 4, 8]), op=ALU.mult),
                      [w["ohg"], w[oh]], [w[nm]])
                V(lambda e, w=w: e.tensor_tensor(out=w["Ab"].t[:], in0=w["A1"].t[:].rearrange("p g x -> p (g x)"), in1=w["A2"].t[:].rearrange("p g x -> p (g x)"), op=ALU.add),
                  [w["A1"], w["A2"]], [w["Ab"]])
                S.op("pe", lambda e, w=w, pos=pos: e.matmul(pos, lhsT=su.t[:], rhs=w["Ab"].t[:], start=True, stop=False),
                     reads=[su.r, w["Ab"].r], writes=[smm.r])
                S.op("pe", lambda e, pos=pos: e.matmul(pos, lhsT=onesb.t[:], rhs=asum.t[:], start=False, stop=True),
                     reads=[onesb.r, asum.r], writes=[smm.r])
                yield
                V(lambda e, w=w, pos=pos: e.tensor_copy(w["posb"].t[:], pos), [smm], [w["posb"]])
                V(lambda e, w=w: e.tensor_tensor(out=asum.t[:], in0=asum.t[:], in1=w["Ab"].t[:], op=ALU.add), [asum, w["Ab"]], [asum])
                for k, nm in ((0, "A1"), (1, "A2")):
                    Ak = w[nm].t[:].rearrange("p g x -> p (g x)")
                    V(lambda e, w=w, Ak=Ak: e.tensor_tensor(out=w["tmp"].t[:], in0=Ak, in1=w["posb"].t[:], op=ALU.mult), [w[nm], w["posb"]], [w["tmp"]])
                    V(lambda e, w=w, k=k: e.reduce_sum(out=w["sp4"].t[:, k:k + 1], in_=w["tmp"].t[:], axis=AX.X), [w["tmp"]], [w["sp4"]])
                    V(lambda e, w=w, Ak=Ak: e.tensor_tensor(out=w["tmp"].t[:], in0=Ak, in1=ebase.t[:], op=ALU.mult), [w[nm], ebase], [w["tmp"]])
                    V(lambda e, w=w, k=k: e.reduce_sum(out=w["sp4"].t[:, 2 + k:3 + k], in_=w["tmp"].t[:], axis=AX.X), [w["tmp"]], [w["sp4"]])
                V(lambda e, w=w: e.tensor_scalar(out=w["ovf"].t[:], in0=w["sp4"].t[:, 0:2], scalar1=float(CAP) - 0.5, scalar2=None, op0=ALU.is_ge), [w["sp4"]], [w["ovf"]])
                V(lambda e, w=w: e.tensor_tensor(out=w["slf"].t[:], in0=w["sp4"].t[:, 0:2], in1=w["sp4"].t[:, 2:4], op=ALU.add), [w["sp4"]], [w["slf"]])
                V(lambda e, w=w: e.tensor_scalar(out=w["nov"].t[:], in0=w["ovf"].t[:], scalar1=-1.0, scalar2=1.0, op0=ALU.mult, op1=ALU.add), [w["ovf"]], [w["nov"]])
                S.op("dve", lambda e, w=w, i=i: e.tensor_tensor(out=slots_f.t[:, i, :], in0=w["slf"].t[:], in1=w["nov"].t[:], op=ALU.mult),
                     reads=[w["slf"].r, w["nov"].r], writes=[slot_r[i]])
                S.op("dve", lambda e, w=w, i=i: e.scalar_tensor_tensor(out=w["slf"].t[:], in0=w["ovf"].t[:], scalar=pdump.t[:, 0:1], in1=slots_f.t[:, i, :], op0=ALU.mult, op1=ALU.add),
                     reads=[w["ovf"].r, pdump.r, slot_r[i], w["slf"].r], writes=[w["slf"].r])
                S.op("dve", lambda e, w=w, i=i: e.tensor_tensor(out=gates.t[:, i, :], in0=w["g12"].t[:], in1=w["nov"].t[:], op=ALU.mult),
                     reads=[w["g12"].r, w["nov"].r], writes=[gate_r[i]])
                for k in range(2):
                    sik = w["si%d" % k]
                    V(lambda e, w=w, k=k, sik=sik: e.tensor_copy(sik.t[:], w["slf"].t[:, k:k + 1]), [w["slf"]], [sik])
                    S.op("pool", lambda e, i=i, k=k, hbb=hbb, sik=sik: e.indirect_dma_start(
                        out=Xs, out_offset=bass.IndirectOffsetOnAxis(ap=sik.t[:, :], axis=0),
                        in_=hbb.t[:], in_offset=None),
                        reads=[hbb.r, sik.r], writes=[RXs], stream=sc[bi][k])

            interleave([(lambda i=i: rt_gen(i)) for i in range(NT)], 3)

        with S.phase() as a:
            c = make_consts(S, a)
            identb = c["identb"]
            NW = 3
            wg = [Tl(a.sb("wg%d" % i, [128, KD, FF], BF16)) for i in range(NW)]
            wu = [Tl(a.sb("wu%d" % i, [128, KD, FF], BF16)) for i in range(NW)]
            wd = [Tl(a.sb("wd%d" % i, [128, 4, D], BF16)) for i in range(NW)]
            wst = [[S.stream("w%d_%d" % (i, j)) for j in range(3)] for i in range(NW)]
            xb = [Tl(a.sb("xb%d" % i, [128, NB, D], BF16)) for i in range(2)]
            xst = [S.stream("x%d" % i) for i in range(2)]
            xT = [Tl(a.sb("xT%d" % i, [128, KD, CAP], BF16)) for i in range(2)]
            hd = [Tl(a.sb("hd%d" % i, [128, 4, CAP], BF16)) for i in range(2)]
            sg = [Tl(a.sb("sg%d" % i, [128, CAP], F32)) for i in range(2)]
            yo = [Tl(a.sb("yo%d" % i, [128, D], F32)) for i in range(2)]
            yst = [S.stream("y%d" % i) for i in range(2)]
            tpx = [Tl(a.ps("tpx%d" % i, [128, KD * 128], BF16)) for i in range(2)]
            pg_ = [Tl(a.ps("pg%d" % i, [128, 512], F32)) for i in range(2)]
            pu_ = [Tl(a.ps("pu%d" % i, [128, 512], F32)) for i in range(2)]
            py_ = [Tl(a.ps("py%d" % i, [128, 512], F32)) for i in range(2)]
            cnt = {"tp": 0, "fc": 0, "yo": 0, "py": 0}

            def Wl(ex):
                wi = ex % NW
                S.op("pool", lambda e: e.dma_start(out=wg[wi].t[:], in_=W["moe_w_gate"][l, ex].rearrange("(k p) f -> p k f", p=128)), writes=[wg[wi].r], stream=True)
                S.op("pool", lambda e: e.dma_start(out=wu[wi].t[:], in_=W["moe_w_up"][l, ex].rearrange("(k p) f -> p k f", p=128)), writes=[wu[wi].r], stream=True)
                S.op("pool", lambda e: e.dma_start(out=wd[wi].t[:], in_=W["moe_w_down"][l, ex].rearrange("(k p) f -> p k f", p=128)), writes=[wd[wi].r], stream=True)

            def Xl(ex):
                xi = ex % 2
                S.op("sp", lambda e: e.dma_start(out=xb[xi].t[:], in_=Xs[ex * CAP:(ex + 1) * CAP, :].rearrange("(b p) d -> p b d", p=128)),
                     reads=[RXs], writes=[xb[xi].r], stream=True)

            def Tr(ex):
                xi = ex % 2
                for b in range(NB):
                    tpt = tpx[cnt["tp"] % 2]
                    cnt["tp"] += 1
                    for k in range(KD):
                        S.op("pe", lambda e, b=b, k=k, tpt=tpt: e.transpose(tpt.t[:, k * 128:(k + 1) * 128], xb[xi].t[:, b, k * 128:(k + 1) * 128], identb.t[:]),
                             reads=[xb[xi].r, identb.r], writes=[tpt.r])
                    if b % 2 == 0:
                        S.op("act", lambda e, b=b, tpt=tpt: e.copy(xT[xi].t[:, :, b * 128:(b + 1) * 128], tpt.t[:].rearrange("p (k t) -> p k t", k=KD)),
                             reads=[tpt.r], writes=[xT[xi].r])
                    else:
                        S.op("dve", lambda e, b=b, tpt=tpt: e.tensor_copy(xT[xi].t[:, :, b * 128:(b + 1) * 128], tpt.t[:].rearrange("p (k t) -> p k t", k=KD)),
                             reads=[tpt.r], writes=[xT[xi].r])

            def GU(ex):
                xi, wi = ex % 2, ex % NW
                for fc in range(4):
                    pgt, put = pg_[cnt["fc"] % 2], pu_[cnt["fc"] % 2]
                    sgt = sg[cnt["fc"] % 2]
                    cnt["fc"] += 1
                    for k in range(KD):
                        S.op("pe", lambda e, k=k, fc=fc, pgt=pgt: e.matmul(pgt.t[:, 0:CAP], lhsT=wg[wi].t[:, k, fc * 128:(fc + 1) * 128], rhs=xT[xi].t[:, k, :], start=(k == 0), stop=(k == KD - 1)),
                             reads=[wg[wi].r, xT[xi].r], writes=[pgt.r])
                    for k in range(KD):
                        S.op("pe", lambda e, k=k, fc=fc, put=put: e.matmul(put.t[:, 0:CAP], lhsT=wu[wi].t[:, k, fc * 128:(fc + 1) * 128], rhs=xT[xi].t[:, k, :], start=(k == 0), stop=(k == KD - 1)),
                             reads=[wu[wi].r, xT[xi].r], writes=[put.r])
                    S.op("act", lambda e, pgt=pgt, sgt=sgt: e.activation(out=sgt.t[:], in_=pgt.t[:, 0:CAP], func=AF.Silu), reads=[pgt.r], writes=[sgt.r])
                    S.op("dve", lambda e, fc=fc, put=put, sgt=sgt: e.tensor_tensor(out=hd[xi].t[:, fc, :], in0=sgt.t[:], in1=put.t[:, 0:CAP], op=ALU.mult),
                         reads=[sgt.r, put.r], writes=[hd[xi].r])

            def Dn(ex):
                xi, wi = ex % 2, ex % NW
                for b in range(NB):
                    yot = yo[cnt["yo"] % 2]
                    cnt["yo"] += 1
                    for half in range(2):
                        pyt = py_[cnt["py"] % 2]
                        cnt["py"] += 1
                        for k in range(4):
                            S.op("pe", lambda e, k=k, b=b, half=half, pyt=pyt: e.matmul(pyt.t[:], lhsT=hd[xi].t[:, k, b * 128:(b + 1) * 128], rhs=wd[wi].t[:, k, half * 512:(half + 1) * 512], start=(k == 0), stop=(k == 3)),
                                 reads=[hd[xi].r, wd[wi].r], writes=[pyt.r])
                        if half == 0:
                            S.op("act", lambda e, pyt=pyt, yot=yot: e.copy(yot.t[:, 0:512], pyt.t[:]), reads=[pyt.r], writes=[yot.r])
                        else:
                            S.op("dve", lambda e, pyt=pyt, yot=yot: e.tensor_copy(yot.t[:, 512:1024], pyt.t[:]), reads=[pyt.r], writes=[yot.r])
                    S.op("act", lambda e, b=b, yot=yot: e.dma_start(out=Ys[ex * CAP + b * 128: ex * CAP + (b + 1) * 128, :], in_=yot.t[:]),
                         reads=[yot.r], writes=[RYs], stream=True)

            for ex in range(min(NW - 1, NE)):
                Wl(ex)
            Xl(0)
            Xl(1)
            Tr(0)
            for ex in range(NE):
                if ex + NW - 1 < NE:
                    Wl(ex + NW - 1)
                GU(ex)
                if ex + 1 < NE:
                    Tr(ex + 1)
                if ex + 2 < NE:
                    Xl(ex + 2)
                Dn(ex)

        with S.phase() as a:
            gt = Tl(a.sb("lng", [128, D], F32))
            bt = Tl(a.sb("lnb", [128, D], F32))
            cs = S.stream("lnw")
            S.op("sp", lambda e: e.dma_start(out=gt.t[:], in_=W["ln_ffn_g"][l:l + 1, :].to_broadcast([128, D])), writes=[gt.r], stream=cs)
            S.op("sp", lambda e: e.dma_start(out=bt.t[:], in_=W["ln_ffn_b"][l:l + 1, :].to_broadcast([128, D])), writes=[bt.r], stream=cs)
            NBUF = 4
            ht = [Tl(a.sb("cht%d" % i, [128, D], F32)) for i in range(NBUF)]
            y0 = [Tl(a.sb("cy0%d" % i, [128, D], F32)) for i in range(NBUF)]
            y1 = [Tl(a.sb("cy1%d" % i, [128, D], F32)) for i in range(NBUF)]
            acc = [Tl(a.sb("cacc%d" % i, [128, D], F32)) for i in range(NBUF)]
            ot = [Tl(a.sb("cot%d" % i, [128, D], F32)) for i in range(NBUF)]
            stats = [Tl(a.sb("cst%d" % i, [128, 2, 6], F32)) for i in range(NBUF)]
            mv = [Tl(a.sb("cmv%d" % i, [128, 2], F32)) for i in range(NBUF)]
            rstd = [Tl(a.sb("crs%d" % i, [128, 2], F32)) for i in range(NBUF)]
            gidx = [[Tl(a.sb("cgi%d_%d" % (i, k), [128, 1], I32)) for k in range(2)] for i in range(NBUF)]

            def tile_gen(i):
                bi = i % NBUF
                S.op("sp", lambda e: e.dma_start(out=ht[bi].t[:], in_=h_in[i * 128:(i + 1) * 128, :]), reads=[Rh_in], writes=[ht[bi].r], stream=True)
                for k, yy in enumerate((y0[bi], y1[bi])):
                    S.op("dve", lambda e, k=k: e.tensor_copy(gidx[bi][k].t[:], slots_f.t[:, i, k:k + 1]),
                         reads=[slot_r[i]], writes=[gidx[bi][k].r])
                    S.op("pool", lambda e, k=k, yy=yy: e.indirect_dma_start(
                        out=yy.t[:], out_offset=None, in_=Ys, in_offset=bass.IndirectOffsetOnAxis(ap=gidx[bi][k].t[:, :], axis=0)),
                        reads=[RYs, gidx[bi][k].r], writes=[yy.r], stream=True)
                yield
                S.op("act", lambda e: e.activation(out=acc[bi].t[:], in_=y0[bi].t[:], func=AF.Copy, scale=gates.t[:, i, 0:1]),
                     reads=[y0[bi].r, gate_r[i]], writes=[acc[bi].r])
                yield
                S.op("dve", lambda e: e.scalar_tensor_tensor(out=acc[bi].t[:], in0=y1[bi].t[:], scalar=gates.t[:, i, 1:2], in1=acc[bi].t[:], op0=ALU.mult, op1=ALU.add),
                     reads=[y1[bi].r, gate_r[i], acc[bi].r], writes=[acc[bi].r])
                S.op("dve", lambda e: e.scalar_tensor_tensor(out=acc[bi].t[:], in0=ht[bi].t[:], scalar=ALPHA, in1=acc[bi].t[:], op0=ALU.mult, op1=ALU.add),
                     reads=[ht[bi].r, acc[bi].r], writes=[acc[bi].r])
                yield from layer_norm_gen(S, acc[bi], ot[bi], gt, bt, stats[bi], mv[bi], rstd[bi])
                S.op("sp", lambda e: e.dma_start(out=h_out[i * 128:(i + 1) * 128, :], in_=ot[bi].t[:]), reads=[ot[bi].r], writes=[Rh_out], stream=True)

            interleave([(lambda i=i: tile_gen(i)) for i in range(NT)], 4)


def build_hT(S, a, h_in, Rh_in, hT, ident):
    NBUF = 2
    ht = [Tl(a.sb("bh%d" % i, [128, D], F32)) for i in range(NBUF)]
    tp = [Tl(a.ps("btp%d" % i, [128, 2, 512], F32)) for i in range(NBUF)]
    ld = [S.stream("bld%d" % i) for i in range(NBUF)]
    for i in range(NT):
        bi = i % NBUF
        S.op("sp", lambda e, i=i, bi=bi: e.dma_start(out=ht[bi].t[:], in_=h_in[i * 128:(i + 1) * 128, :]),
             reads=[Rh_in], writes=[ht[bi].r], stream=ld[bi])
        for k in range(KD):
            S.op("pe", lambda e, k=k, bi=bi: e.transpose(tp[bi].t[:, k // 4, (k % 4) * 128:(k % 4 + 1) * 128],
                                                          ht[bi].t[:, k * 128:(k + 1) * 128], ident.t[:]),
                 reads=[ht[bi].r, ident.r], writes=[tp[bi].r])
        for j in range(2):
            dst = hT.t[:, j * 4:(j + 1) * 4, i * 128:(i + 1) * 128]
            src = tp[bi].t[:, j, :].rearrange("p (k t) -> p k t", k=4)
            if j == 0:
                S.op("act", lambda e, dst=dst, src=src: e.copy(dst, src), reads=[tp[bi].r], writes=[hT.r])
            else:
                S.op("dve", lambda e, dst=dst, src=src: e.tensor_copy(dst, src), reads=[tp[bi].r], writes=[hT.r])


def outproj_ln_phase(nc, S, YT_src, RYT, Wout_ap, h_in, Rh_in, h_out, Rh_out, g_ap, b_ap):
    with S.phase() as a:
        YT = Tl(a.sb("YT", [128, KD, T], BF16))
        ys = S.stream("yt")
        for k in range(KD):
            S.op("sp", lambda e, k=k: e.dma_start(out=YT.t[:, k, :], in_=YT_src[k]), reads=[RYT], writes=[YT.r], stream=ys)
        wo = Tl(a.sb("wo", [128, KD, D], BF16))
        wos = S.stream("wo")
        S.op("pool", lambda e: e.dma_start(out=wo.t[:], in_=Wout_ap.rearrange("(k p) f -> p k f", p=128)), writes=[wo.r], stream=wos)
        gt = Tl(a.sb("lng", [128, D], F32))
        bt = Tl(a.sb("lnb", [128, D], F32))
        S.op("sp", lambda e: e.dma_start(out=gt.t[:], in_=g_ap.to_broadcast([128, D])), writes=[gt.r], stream=wos)
        S.op("sp", lambda e: e.dma_start(out=bt.t[:], in_=b_ap.to_broadcast([128, D])), writes=[bt.r], stream=wos)
        NBUF = 4
        ht = [Tl(a.sb("oh%d" % i, [128, D], F32)) for i in range(NBUF)]
        acc = [Tl(a.sb("oacc%d" % i, [128, D], F32)) for i in range(NBUF)]
        ot = [Tl(a.sb("oot%d" % i, [128, D], F32)) for i in range(NBUF)]
        stats = [Tl(a.sb("ost%d" % i, [128, 2, 6], F32)) for i in range(NBUF)]
        mv = [Tl(a.sb("omv%d" % i, [128, 2], F32)) for i in range(NBUF)]
        rstd = [Tl(a.sb("ors%d" % i, [128, 2], F32)) for i in range(NBUF)]
        pm = [Tl(a.ps("opm%d" % i, [128, 2, 512], F32)) for i in range(NBUF)]

        def tile_gen(i):
            bi = i % NBUF
            S.op("sp", lambda e: e.dma_start(out=ht[bi].t[:], in_=h_in[i * 128:(i + 1) * 128, :]), reads=[Rh_in], writes=[ht[bi].r], stream=True)
            for half in range(2):
                for k in range(KD):
                    S.op("pe", lambda e, k=k, half=half: e.matmul(pm[bi].t[:, half, :], lhsT=YT.t[:, k, i * 128:(i + 1) * 128], rhs=wo.t[:, k, half * 512:(half + 1) * 512], start=(k == 0), stop=(k == KD - 1)),
                         reads=[YT.r, wo.r], writes=[pm[bi].r])
            yield
            S.op("dve", lambda e: e.scalar_tensor_tensor(out=acc[bi].t[:], in0=ht[bi].t[:], scalar=ALPHA, in1=pm[bi].t[:].rearrange("p a b -> p (a b)"), op0=ALU.mult, op1=ALU.add),
                 reads=[ht[bi].r, pm[bi].r], writes=[acc[bi].r])
            yield from layer_norm_gen(S, acc[bi], ot[bi], gt, bt, stats[bi], mv[bi], rstd[bi])
            S.op("sp", lambda e: e.dma_start(out=h_out[i * 128:(i + 1) * 128, :], in_=ot[bi].t[:]), reads=[ot[bi].r], writes=[Rh_out], stream=True)

        interleave([(lambda i=i: tile_gen(i)) for i in range(NT)], 4)


DILS = (1, 4, 16)


def attn_stage(nc, S, h_in, h_out, W, l, scr, Rh_in, Rh_out):
    QTs, KTs, Vs, OTs = scr["QTs"], scr["KTs"], scr["Vs"], scr["OTs"]
    RQ, RK, RV, RO = Res(), Res(), Res(), Res()
    wqkv = W["w_qkv_c"][0]
    with S.phase() as a:
        c = make_consts(S, a)
        hT = Tl(a.sb("hT", [128, KD, T], BF16))
        build_hT(S, a, h_in, Rh_in, hT, c["ident"])
        wq = [Tl(a.sb("wqkv%d" % j, [128, KD, D], BF16)) for j in range(3)]
        wsm = S.stream("wqkv")
        for j in range(3):
            S.op("pool", lambda e, j=j: e.dma_start(out=wq[j].t[:], in_=wqkv[:, j * D:(j + 1) * D].rearrange("(k p) f -> p k f", p=128)), writes=[wq[j].r], stream=wsm)
        stg = [Tl(a.sb("stg%d" % i, [128, T], BF16)) for i in range(2)]
        sst = [S.stream("sst%d" % i) for i in range(2)]
        pp = [Tl(a.ps("pp%d" % i, [128, 512], F32)) for i in range(4)]
        npp = 0
        nst = 0
        for which, dst, Rd in ((0, QTs, RQ), (1, KTs, RK)):
            for hp in range(8):
                sg = stg[nst % 2]
                ss = sst[nst % 2]
                nst += 1
                for tc in range(8):
                    p = pp[npp % 4]
                    npp += 1
                    for k in range(KD):
                        S.op("pe", lambda e, k=k, hp=hp, tc=tc, p=p, which=which: e.matmul(p.t[:], lhsT=wq[which].t[:, k, hp * 128:(hp + 1) * 128], rhs=hT.t[:, k, tc * 512:(tc + 1) * 512], start=(k == 0), stop=(k == KD - 1)),
                             reads=[wq[which].r, hT.r], writes=[p.r])
                    if tc % 2 == 0:
                        S.op("act", lambda e, tc=tc, p=p, sg=sg: e.copy(sg.t[:, tc * 512:(tc + 1) * 512], p.t[:]), reads=[p.r], writes=[sg.r])
                    else:
                        S.op("dve", lambda e, tc=tc, p=p, sg=sg: e.tensor_copy(sg.t[:, tc * 512:(tc + 1) * 512], p.t[:]), reads=[p.r], writes=[sg.r])
                S.op("sp", lambda e, hp=hp, sg=sg, dst=dst: e.dma_start(out=dst[hp], in_=sg.t[:]), reads=[sg.r], writes=[Rd], stream=ss)
        vst = [Tl(a.sb("vst%d" % i, [128, D], BF16)) for i in range(2)]
        vss = [S.stream("vss%d" % i) for i in range(2)]
        nv = 0
        for di, d in enumerate(DILS[:1]):
            nbr = T // d // 128
            hTv = hT.t[:].rearrange("p k (m s) -> p k m s", s=d)
            for r in range(d):
                for b in range(nbr):
                    nb = r * nbr + b
                    vt = vst[nv % 2]
                    vs_ = vss[nv % 2]
                    nv += 1
                    for half in range(2):
                        p = pp[npp % 4]
                        npp += 1
                        for k in range(KD):
                            S.op("pe", lambda e, k=k, b=b, r=r, half=half, p=p, hTv=hTv: e.matmul(p.t[:], lhsT=hTv[:, k, b * 128:(b + 1) * 128, r], rhs=wq[2].t[:, k, half * 512:(half + 1) * 512], start=(k == 0), stop=(k == KD - 1)),
                                 reads=[hT.r, wq[2].r], writes=[p.r])
                        if half == 0:
                            S.op("act", lambda e, p=p, vt=vt: e.copy(vt.t[:, 0:512], p.t[:]), reads=[p.r], writes=[vt.r])
                        else:
                            S.op("dve", lambda e, p=p, vt=vt: e.tensor_copy(vt.t[:, 512:1024], p.t[:]), reads=[p.r], writes=[vt.r])
                    S.op("sp", lambda e, di=di, nb=nb, vt=vt: e.dma_start(out=Vs[di, nb * 128:(nb + 1) * 128, :], in_=vt.t[:]), reads=[vt.r], writes=[RV], stream=vs_)

    with S.phase() as a:
        for di, d in enumerate(DILS):
            if di == 0:
                continue
            for r in range(d):
                S.op("sp" if r % 2 == 0 else "act", lambda e, di=di, d=d, r=r: e.dma_start(out=Vs[di, r * (T // d):(r + 1) * (T // d), :],
                                                                                          in_=Vs[0].rearrange("(m s) c -> s m c", s=d)[r]),
                     reads=[RV], writes=[RV], stream=True)

    with S.phase() as a:
        c = make_consts(S, a)
        identb = c["identb"]
        negm = Tl(a.sb("mask01", [128, 512], BF16))
        S.op("pool", lambda e: e.memset(negm.t[:], 1.0), writes=[negm.r])
        for hh in range(2):
            S.op("pool", lambda e, hh=hh: e.affine_select(out=negm.t[:, 256 * hh:256 * hh + 128], in_=negm.t[:, 256 * hh:256 * hh + 128], pattern=[[1, 128]], compare_op=ALU.is_ge, fill=0.0, base=0, channel_multiplier=-1),
                 reads=[negm.r], writes=[negm.r])
            S.op("pool", lambda e, hh=hh: e.affine_select(out=negm.t[:, 256 * hh + 128:256 * hh + 256], in_=negm.t[:, 256 * hh + 128:256 * hh + 256], pattern=[[-1, 128]], compare_op=ALU.is_ge, fill=0.0, base=0, channel_multiplier=1),
                 reads=[negm.r], writes=[negm.r])
        ones = Tl(a.sb("ones", [128, 64], BF16))
        S.op("pool", lambda e: e.memset(ones.t[:], 1.0), writes=[ones.r])
        NQ = 2
        qt = [Tl(a.sb("qt%d" % i, [128, T], BF16)) for i in range(NQ)]
        kt = [Tl(a.sb("kt%d" % i, [128, T], BF16)) for i in range(NQ)]
        vv = [[Tl(a.sb("vv%d_%d" % (i, j), [128, NT, 128], BF16)) for j in range(3)] for i in range(NQ)]
        lds = [[S.stream("al%d_%d" % (i, j)) for j in range(5)] for i in range(NQ)]
        oacc = Tl(a.sb("oacc", [128, T], F32))
        sacc = Tl(a.sb("sacc", [128, T], F32))
        otb = [Tl(a.sb("otb%d" % i, [128, T], BF16)) for i in range(2)]
        ots = [S.stream("ots%d" % i) for i in range(2)]
        NPT = 6
        PT = [Tl(a.sb("PT%d" % i, [128, 512], BF16)) for i in range(NPT)]
        sT = [Tl(a.ps("sT%d" % i, [128, 2, 512], F32)) for i in range(2)]
        po = [Tl(a.ps("po%d" % i, [128, 512], F32)) for i in range(2)]
        pS = [Tl(a.ps("pS%d" % i, [128, 512], F32)) for i in range(2)]
        nsT = 0
        nPT = 0
        npo = 0
        for hp in range(8):
            qi = hp % NQ
            Q, K_, V3 = qt[qi], kt[qi], vv[qi]
            S.op("sp", lambda e, hp=hp, Q=Q: e.dma_start(out=Q.t[:], in_=QTs[hp]), reads=[RQ], writes=[Q.r], stream=lds[qi][0])
            S.op("sp", lambda e, hp=hp, K_=K_: e.dma_start(out=K_.t[:], in_=KTs[hp]), reads=[RK], writes=[K_.r], stream=lds[qi][1])
            for di in range(3):
                S.op("act", lambda e, hp=hp, di=di, V3=V3: e.dma_start(out=V3[di].t[:], in_=Vs[di, :, hp * 128:(hp + 1) * 128].rearrange("(b p) c -> p b c", p=128)),
                     reads=[RV], writes=[V3[di].r], stream=lds[qi][2 + di])
            items = []
            for di, d in enumerate(DILS):
                nbr = T // d // 128
                for r in range(d):
                    for b in range(nbr):
                        items.append((di, d, nbr, r, b))
            LAG = 2
            pts = {}
            pvstate = {"pot": None, "pst": None}

            def score_part(k, Q=Q, K_=K_):
                nonlocal nsT, nPT
                di, d, nbr, r, b = items[k]
                Qv = Q.t[:].rearrange("p (m s) -> p m s", s=d)
                Kv = K_.t[:].rearrange("p (m s) -> p m s", s=d)
                nq = 256 if b + 1 < nbr else 128
                st_ = sT[nsT % 2]
                nsT += 1
                for h in range(2):
                    rows = slice(64 * h, 64 * h + 64)
                    S.op("pe", lambda e, h=h, rows=rows: e.matmul(st_.t[:, h, 0:nq], lhsT=Kv[rows, b * 128:(b + 1) * 128, r], rhs=Qv[rows, b * 128:b * 128 + nq, r], start=True, stop=True),
                         reads=[K_.r, Q.r], writes=[st_.r])
                pt = PT[nPT % NPT]
                nPT += 1
                S.op("act", lambda e: e.activation(out=pt.t[:].rearrange("p (h q) -> p h q", h=2)[:, :, 0:nq], in_=st_.t[:, :, 0:nq], func=AF.Exp, scale=0.125),
                     reads=[st_.r], writes=[pt.r])
                if nq == 256:
                    S.op("dve", lambda e: e.tensor_tensor(out=pt.t[:], in0=pt.t[:], in1=negm.t[:], op=ALU.mult), reads=[pt.r, negm.r], writes=[pt.r])
                else:
                    for hh in range(2):
                        S.op("dve", lambda e, hh=hh: e.tensor_tensor(out=pt.t[:, 256 * hh:256 * hh + 128], in0=pt.t[:, 256 * hh:256 * hh + 128], in1=negm.t[:, 256 * hh:256 * hh + 128], op=ALU.mult),
                             reads=[pt.r, negm.r], writes=[pt.r])
                pts[k] = pt

            def pv_part(k, V3=V3):
                nonlocal npo
                di, d, nbr, r, b = items[k]
                Ov = oacc.t[:].rearrange("p (m s) -> p m s", s=d)
                Sv = sacc.t[:].rearrange("p (m s) -> p m s", s=d)
                nb = r * nbr + b
                pt = pts[k]
                prevPT = pts[k - 1] if b > 0 else None
                g = b % 4
                if g == 0:
                    pvstate["pot"], pvstate["pst"] = po[npo % 2], pS[npo % 2]
                    npo += 1
                pot, pst = pvstate["pot"], pvstate["pst"]
                for h in range(2):
                    rows = slice(64 * h, 64 * h + 64)
                    cols = slice(g * 128, (g + 1) * 128)
                    for (dstp, lhs_cur, lhs_prev) in ((pot, V3[di].t[:, nb, rows], V3[di].t[:, nb - 1, rows] if b > 0 else None), (pst, ones.t[:], ones.t[:] if b > 0 else None)):
                        S.op("pe", lambda e, dstp=dstp, rows=rows, cols=cols, lhs_cur=lhs_cur, h=h: e.matmul(dstp.t[rows, cols], lhsT=lhs_cur, rhs=pt.t[:, 256 * h:256 * h + 128], start=True, stop=(b == 0)),
                             reads=[V3[di].r, ones.r, pt.r], writes=[dstp.r])
                        if b > 0:
                            S.op("pe", lambda e, dstp=dstp, rows=rows, cols=cols, lhs_prev=lhs_prev, h=h: e.matmul(dstp.t[rows, cols], lhsT=lhs_prev, rhs=prevPT.t[:, 256 * h + 128:256 * h + 256], start=False, stop=True),
                                 reads=[V3[di].r, ones.r, prevPT.r], writes=[dstp.r])
                if g == 3 or b == nbr - 1:
                    b0 = b - g
                    n = (g + 1) * 128
                    for (acc_v, src, accT) in ((Ov, pot, oacc), (Sv, pst, sacc)):
                        dst = acc_v[:, b0 * 128:b0 * 128 + n, r]
                        if di == 0:
                            S.op("dve", lambda e, dst=dst, src=src, n=n: e.tensor_copy(dst, src.t[:, 0:n]), reads=[src.r], writes=[accT.r])
                        else:
                            S.op("dve", lambda e, dst=dst, src=src, n=n: e.tensor_tensor(out=dst, in0=src.t[:, 0:n], in1=dst, op=ALU.add), reads=[src.r, accT.r], writes=[accT.r])
                pts.pop(k - 1, None)

            for k in range(len(items) + LAG):
                if k < len(items):
                    score_part(k)
                if k - LAG >= 0:
                    pv_part(k - LAG)
            ob = otb[hp % 2]
            S.op("dve", lambda e: e.reciprocal(sacc.t[:], sacc.t[:]), reads=[sacc.r], writes=[sacc.r])
            S.op("dve", lambda e, ob=ob: e.tensor_tensor(out=ob.t[:], in0=oacc.t[:], in1=sacc.t[:], op=ALU.mult), reads=[oacc.r, sacc.r], writes=[ob.r])
            S.op("sp", lambda e, hp=hp, ob=ob: e.dma_start(out=OTs[hp], in_=ob.t[:]), reads=[ob.r], writes=[RO], stream=ots[hp % 2])

    outproj_ln_phase(nc, S, OTs, RO, W["w_out_c"][0], h_in, Rh_in, h_out, Rh_out, W["ln_mix_g"][l:l + 1, :], W["ln_mix_b"][l:l + 1, :])


class Ring:
    def __init__(self, tiles):
        self.tiles = tiles
        self.i = 0

    def get(self):
        t = self.tiles[self.i % len(self.tiles)]
        self.i += 1
        return t


class View:
    def __init__(self, ap, res=None):
        self.ap = ap
        self.r = res if res is not None else Res()


def l0_inproj(nc, S, h_in, Rh_in, W, scr, R, ba_sb, ba_r):
    P0T, ZS = scr["P0T"], scr["ZS"]
    w_in = W["w_in_ab"][0]
    with S.phase() as a:
        c = make_consts(S, a)
        hT = Tl(a.sb("hT", [128, KD, T], BF16))
        build_hT(S, a, h_in, Rh_in, hT, c["ident"])
        NCOL = 2568
        win = Tl(a.sb("win", [128, KD, NCOL], BF16))
        wsm = S.stream("win")
        for k in range(KD):
            S.op("pool", lambda e, k=k: e.dma_start(out=win.t[:, k, :], in_=w_in[k * 128:(k + 1) * 128, :]), writes=[win.r], stream=wsm)
        stg = [Tl(a.sb("stg%d" % i, [128, T], F32)) for i in range(2)]
        sst = [S.stream("sst%d" % i) for i in range(2)]
        pp = [Tl(a.ps("pp%d" % i, [128, 512], F32)) for i in range(3)]
        npp = 0
        chunks = [(h, h * 128) for h in range(4)] + [(4 + h, 512 + h * 128) for h in range(4)] + \
                 [(8 + h, 1024 + h * 128) for h in range(4)] + [(12 + cq, 2056 + cq * 128) for cq in range(4)]
        for ci, (dst, col0) in enumerate(chunks):
            sg, ss = stg[ci % 2], sst[ci % 2]
            for tc in range(8):
                p = pp[npp % 3]
                npp += 1
                for k in range(KD):
                    S.op("pe", lambda e, k=k, col0=col0, tc=tc, p=p: e.matmul(p.t[:], lhsT=win.t[:, k, col0:col0 + 128], rhs=hT.t[:, k, tc * 512:(tc + 1) * 512], start=(k == 0), stop=(k == KD - 1)),
                         reads=[win.r, hT.r], writes=[p.r])
                if tc % 2 == 0:
                    S.op("act", lambda e, tc=tc, p=p, sg=sg: e.copy(sg.t[:, tc * 512:(tc + 1) * 512], p.t[:]), reads=[p.r], writes=[sg.r])
                else:
                    S.op("dve", lambda e, tc=tc, p=p, sg=sg: e.tensor_copy(sg.t[:, tc * 512:(tc + 1) * 512], p.t[:]), reads=[p.r], writes=[sg.r])
            S.op("sp", lambda e, dst=dst, sg=sg: e.dma_start(out=P0T[dst], in_=sg.t[:]), reads=[sg.r], writes=[R["P0T"]], stream=ss)
        zst = [Tl(a.sb("zst%d" % i, [128, 512], F32)) for i in range(2)]
        zss = [S.stream("zss%d" % i) for i in range(2)]
        pb = [Tl(a.ps("pb%d" % i, [128, 512], F32)) for i in range(1)]
        for i in range(NT):
            p = pp[npp % 3]
            npp += 1
            p2 = pb[0]
            for k in range(KD):
                S.op("pe", lambda e, k=k, i=i, p=p: e.matmul(p.t[:], lhsT=hT.t[:, k, i * 128:(i + 1) * 128], rhs=win.t[:, k, 1536:2048], start=(k == 0), stop=(k == KD - 1)),
                     reads=[win.r, hT.r], writes=[p.r])
            for k in range(KD):
                S.op("pe", lambda e, k=k, i=i, p2=p2: e.matmul(p2.t[:, 0:8], lhsT=hT.t[:, k, i * 128:(i + 1) * 128], rhs=win.t[:, k, 2048:2056], start=(k == 0), stop=(k == KD - 1)),
                     reads=[win.r, hT.r], writes=[p2.r])
            zt = zst[i % 2]
            S.op("act", lambda e, p=p, zt=zt: e.activation(out=zt.t[:], in_=p.t[:], func=AF.Silu), reads=[p.r], writes=[zt.r])
            S.op("dve", lambda e, i=i, p2=p2: e.tensor_copy(ba_sb[:, i, :], p2.t[:, 0:8]), reads=[p2.r], writes=[ba_r])
            S.op("sp", lambda e, i=i, zt=zt: e.dma_start(out=ZS[i * 128:(i + 1) * 128, :], in_=zt.t[:]), reads=[zt.r], writes=[R["ZS"]], stream=zss[i % 2])


def l0_gdn(nc, S, W, scr, R, ba_sb, ba_r):
    P0T, ZS, YTs = scr["P0T"], scr["ZS"], scr["YTs"]
    C = 128
    with S.phase() as a:
        c = make_consts(S, a)
        ident = c["ident"]
        cs = S.stream("gconst")
        ones = Tl(a.sb("ones", [128, 128], F32))
        S.op("pool", lambda e: e.memset(ones.t[:], 1.0), writes=[ones.r])
        UT = Tl(a.sb("UT", [128, 128], F32))
        S.op("pool", lambda e: e.memset(UT.t[:], 1.0), writes=[UT.r])
        S.op("pool", lambda e: e.affine_select(out=UT.t[:], in_=UT.t[:], pattern=[[1, 128]], compare_op=ALU.is_ge, fill=0.0, base=0, channel_multiplier=-1),
             reads=[UT.r], writes=[UT.r])
        mge = UT
        mlt = Tl(a.sb("mlt", [128, 128], F32))
        S.op("pool", lambda e: e.memset(mlt.t[:], 1.0), writes=[mlt.r])
        S.op("pool", lambda e: e.affine_select(out=mlt.t[:], in_=mlt.t[:], pattern=[[-1, 128]], compare_op=ALU.is_gt, fill=0.0, base=0, channel_multiplier=1),
             reads=[mlt.r], writes=[mlt.r])
        cwT = Tl(a.sb("cwT", [4, 1536], F32))
        S.op("sp", lambda e: e.dma_start(out=cwT.t[:], in_=W["conv_qkv"][0]), writes=[cwT.r], stream=cs)
        cw = Tl(a.sb("cw", [128, 12, 4], F32))
        pcw = Tl(a.ps("pcw", [128, 512], F32))
        for cc in range(12):
            S.op("pe", lambda e, cc=cc: e.transpose(pcw.t[:, cc * 4:(cc + 1) * 4], cwT.t[0:4, cc * 128:(cc + 1) * 128], ident.t[0:4, 0:4]),
                 reads=[cwT.r, ident.r], writes=[pcw.r])
        S.op("dve", lambda e: e.tensor_copy(cw.t[:].rearrange("p c j -> p (c j)"), pcw.t[:, 0:48]), reads=[pcw.r], writes=[cw.r])
        alog = Tl(a.sb("alog", [128, 4], F32))
        dtb = Tl(a.sb("dtb", [128, 4], F32))
        nw = Tl(a.sb("nw", [128, 128], F32))
        S.op("sp", lambda e: e.dma_start(out=alog.t[:], in_=W["gdn_a_log"][0:1, :].to_broadcast([128, 4])), writes=[alog.r], stream=cs)
        S.op("sp", lambda e: e.dma_start(out=dtb.t[:], in_=W["gdn_dt_bias"][0:1, :].to_broadcast([128, 4])), writes=[dtb.r], stream=cs)
        S.op("sp", lambda e: e.dma_start(out=nw.t[:], in_=W["gdn_norm"][0:1, :].to_broadcast([128, 128])), writes=[nw.r], stream=cs)
        nexpA = Tl(a.sb("nexpA", [128, 4], F32))
        S.op("act", lambda e: e.activation(out=nexpA.t[:], in_=alog.t[:], func=AF.Exp), reads=[alog.r], writes=[nexpA.r])
        S.op("dve", lambda e: e.tensor_scalar(out=nexpA.t[:], in0=nexpA.t[:], scalar1=-1.0, scalar2=None, op0=ALU.mult), reads=[nexpA.r], writes=[nexpA.r])

        G = 4
        NG = NT // G
        pre = [Tl(a.sb("pre%d" % i, [128, T + 3], F32)) for i in range(1)]
        prs = [S.stream("prs%d" % i) for i in range(1)]
        S.op("pool", lambda e: e.memset(pre[0].t[:, 0:3], 0.0), writes=[pre[0].r])
        QT = Tl(a.sb("QT", [128, T], F32))
        KT = Tl(a.sb("KT", [128, T], F32))
        VT = Tl(a.sb("VT", [128, T], F32))
        zhr = Ring([Tl(a.sb("zh%d" % i, [128, G, 128], F32)) for i in range(2)])
        zs_ = S.stream("zh")
        yst = [Tl(a.sb("yst%d" % i, [128, T], BF16)) for i in range(1)]
        yss = [S.stream("yss%d" % i) for i in range(1)]
        beta_all = Tl(a.sb("beta_all", [128, NT], F32))
        nbeta_all = Tl(a.sb("nbeta_all", [128, NT], F32))
        g_all = Tl(a.sb("g_all", [128, NT], F32))
        gtmp = Tl(a.sb("gtmp", [128, NT], F32))
        sqb = [Tl(a.sb("sqb%d" % i, [128, 512], F32)) for i in range(2)]
        rsb = [Tl(a.sb("rsb%d" % i, [128, 512], F32)) for i in range(2)]
        Sst = Tl(a.sb("Sst", [128, 128], F32))
        ring = Ring([Tl(a.sb("rg%d" % i, [128, G, 128], F32)) for i in range(16)])
        keep = Ring([Tl(a.sb("kp%d" % i, [128, G, 128], F32)) for i in range(28)])
        colring = Ring([Tl(a.sb("cr%d" % i, [128, G], F32)) for i in range(40)])
        psb = [a.ps("gps%d" % i, [128, G, 128], F32) for i in range(7)]
        psring = Ring([View(psb[i][:, :, :]) for i in range(6)])
        po_bank = View(psb[6][:, :, :])
        mge_b = mge.t[:].unsqueeze(1).to_broadcast([128, G, 128])
        mlt_b = mlt.t[:].unsqueeze(1).to_broadcast([128, G, 128])
        ident_b = ident.t[:].unsqueeze(1).to_broadcast([128, G, 128])
        nw_b = nw.t[:].unsqueeze(1).to_broadcast([128, G, 128])
        Vv = lambda fn, rd, wr: S.op("dve", fn, reads=[x.r for x in rd], writes=[x.r for x in wr])
        Aa = lambda fn, rd, wr: S.op("act", fn, reads=[x.r for x in rd], writes=[x.r for x in wr])
        Pe = lambda fn, rd, wr: S.op("pe", fn, reads=[x.r for x in rd], writes=[x.r for x in wr])

        def bc(colap):
            return colap.unsqueeze(2).to_broadcast([128, G, 128])

        for h in range(DBG.get('gdn_heads', 4)):
            for which, (dstT, src_idx) in enumerate(((QT, h), (KT, 4 + h), (VT, 8 + h))):
                pr, ps_ = pre[0], prs[0]
                S.op("sp", lambda e, src_idx=src_idx, pr=pr: e.dma_start(out=pr.t[:, 3:T + 3], in_=P0T[src_idx]), reads=[R["P0T"]], writes=[pr.r], stream=ps_)
                cc = which * 4 + h
                S.op("dve", lambda e, pr=pr, dstT=dstT, cc=cc: e.tensor_scalar(out=dstT.t[:], in0=pr.t[:, 0:T], scalar1=cw.t[:, cc, 0:1], scalar2=None, op0=ALU.mult),
                     reads=[pr.r, cw.r], writes=[dstT.r])
                for j in range(1, 4):
                    S.op("dve", lambda e, pr=pr, dstT=dstT, cc=cc, j=j: e.scalar_tensor_tensor(out=dstT.t[:], in0=pr.t[:, j:T + j], scalar=cw.t[:, cc, j:j + 1], in1=dstT.t[:], op0=ALU.mult, op1=ALU.add),
                         reads=[pr.r, cw.r, dstT.r], writes=[dstT.r])
                S.op("act", lambda e, dstT=dstT: e.activation(out=dstT.t[:], in_=dstT.t[:], func=AF.Silu), reads=[dstT.r], writes=[dstT.r])
                if which < 2:
                    for tb in range(8):
                        sl = slice(tb * 512, (tb + 1) * 512)
                        sq, rs = sqb[tb % 2], rsb[tb % 2]
                        pq = psring.get()
                        pqv = pq.ap.rearrange("p g c -> p (g c)")
                        Aa(lambda e, dstT=dstT, sl=sl, sq=sq: e.activation(out=sq.t[:], in_=dstT.t[:, sl], func=AF.Square), [dstT], [sq])
                        Pe(lambda e, sq=sq, pqv=pqv: e.matmul(pqv, lhsT=ones.t[:], rhs=sq.t[:], start=True, stop=True), [ones, sq], [pq])
                        Vv(lambda e, rs=rs, pqv=pqv: e.tensor_scalar(out=rs.t[:], in0=pqv, scalar1=1e-6, scalar2=None, op0=ALU.add), [pq], [rs])
                        Aa(lambda e, rs=rs, which=which: e.activation(out=rs.t[:], in_=rs.t[:], func=AF.Sqrt, scale=(128.0 if which == 0 else 1.0)), [rs], [rs])
                        Vv(lambda e, rs=rs: e.reciprocal(rs.t[:], rs.t[:]), [rs], [rs])
                        Vv(lambda e, dstT=dstT, sl=sl, rs=rs: e.tensor_tensor(out=dstT.t[:, sl], in0=dstT.t[:, sl], in1=rs.t[:], op=ALU.mult), [dstT, rs], [dstT])
            S.op("act", lambda e, h=h: e.activation(out=beta_all.t[:], in_=ba_sb[:, :, h], func=AF.Exp, scale=-1.0), reads=[ba_r], writes=[beta_all.r])
            Vv(lambda e: e.tensor_scalar(out=beta_all.t[:], in0=beta_all.t[:], scalar1=1.0, scalar2=None, op0=ALU.add), [beta_all], [beta_all])
            Vv(lambda e: e.reciprocal(beta_all.t[:], beta_all.t[:]), [beta_all], [beta_all])
            Vv(lambda e: e.tensor_scalar(out=nbeta_all.t[:], in0=beta_all.t[:], scalar1=-1.0, scalar2=None, op0=ALU.mult), [beta_all], [nbeta_all])
            S.op("act", lambda e, h=h: e.activation(out=gtmp.t[:], in_=ba_sb[:, :, 4 + h], func=AF.Exp, bias=dtb.t[:, h:h + 1], scale=1.0), reads=[ba_r, dtb.r], writes=[gtmp.r])
            Aa(lambda e: e.activation(out=gtmp.t[:], in_=gtmp.t[:], func=AF.Ln, bias=1.0, scale=1.0), [gtmp], [gtmp])
            Vv(lambda e, h=h: e.tensor_scalar(out=g_all.t[:], in0=gtmp.t[:], scalar1=nexpA.t[:, h:h + 1], scalar2=None, op0=ALU.mult), [gtmp, nexpA], [g_all])
            S.op("pool", lambda e: e.memset(Sst.t[:], 0.0), writes=[Sst.r])
            ys_t = yst[0]

            def pre_group(gi):
                n0 = gi * G
                gsl = slice(n0 * C, (n0 + G) * C)
                gcols = slice(n0, n0 + G)
                d = {"gi": gi}
                Ktw, Vtw = ring.get(), ring.get()
                for (src, dst_) in ((KT, Ktw), (VT, Vtw)):
                    p = psring.get()
                    for c_ in range(G):
                        Pe(lambda e, src=src, p=p, c_=c_, n0=n0: e.transpose(p.ap[:, c_, :], src.t[:, (n0 + c_) * C:(n0 + c_ + 1) * C], ident.t[:]), [src, ident], [p])
                    Aa(lambda e, p=p, dst_=dst_: e.copy(dst_.t[:], p.ap), [p], [dst_])
                gbcw = ring.get()
                Vv(lambda e, gbcw=gbcw, gcols=gcols: e.tensor_copy(gbcw.t[:], bc(g_all.t[:, gcols])), [g_all], [gbcw])
                pcol = psring.get()
                Pe(lambda e, pcol=pcol, gcols=gcols: e.matmul(pcol.ap[:, 0, 0:G], lhsT=UT.t[:], rhs=g_all.t[:, gcols], start=True, stop=True), [UT, g_all], [pcol])
                prow = psring.get()
                for c_ in range(G):
                    Pe(lambda e, prow=prow, gbcw=gbcw, c_=c_: e.matmul(prow.ap[:, c_, :], lhsT=gbcw.t[:, c_, :], rhs=UT.t[:], start=True, stop=True), [UT, gbcw], [prow])
                gcc = colring.get()
                ngcc = colring.get()
                Vv(lambda e, gcc=gcc, pcol=pcol: e.tensor_copy(gcc.t[:], pcol.ap[:, 0, 0:G]), [pcol], [gcc])
                Vv(lambda e, gcc=gcc, ngcc=ngcc: e.tensor_scalar(out=ngcc.t[:], in0=gcc.t[:], scalar1=-1.0, scalar2=None, op0=ALU.mult), [gcc], [ngcc])
                egc = colring.get()
                Aa(lambda e, egc=egc, gcc=gcc: e.activation(out=egc.t[:], in_=gcc.t[:], func=AF.Exp), [gcc], [egc])
                egrw = ring.get()
                Aa(lambda e, egrw=egrw, prow=prow: e.activation(out=egrw.t[:], in_=prow.ap, func=AF.Exp), [prow], [egrw])
                glc = colring.get()
                Aa(lambda e, glc=glc, prow=prow: e.copy(glc.t[:], prow.ap[:, :, C - 1]), [prow], [glc])
                Dtw, Dmw = ring.get(), ring.get()
                for c_ in range(G):
                    Aa(lambda e, Dtw=Dtw, prow=prow, ngcc=ngcc, c_=c_: e.activation(out=Dtw.t[:, c_, :], in_=prow.ap[:, c_, :], func=AF.Exp, bias=ngcc.t[:, c_:c_ + 1], scale=1.0), [prow, ngcc], [Dtw])
                    Aa(lambda e, Dmw=Dmw, prow=prow, gcc=gcc, c_=c_: e.activation(out=Dmw.t[:, c_, :], in_=prow.ap[:, c_, :], func=AF.Exp, bias=gcc.t[:, c_:c_ + 1], scale=-1.0), [prow, gcc], [Dmw])
                Vv(lambda e, Dtw=Dtw: e.scalar_tensor_tensor(out=Dtw.t[:], in0=Dtw.t[:], scalar=1.0, in1=mge_b, op0=ALU.min, op1=ALU.mult), [Dtw, mge], [Dtw])
                Vv(lambda e, Dmw=Dmw: e.scalar_tensor_tensor(out=Dmw.t[:], in0=Dmw.t[:], scalar=1.0, in1=mlt_b, op0=ALU.min, op1=ALU.mult), [Dmw, mlt], [Dmw])
                Vv(lambda e, Dmw=Dmw, gcols=gcols: e.tensor_tensor(out=Dmw.t[:], in0=Dmw.t[:], in1=bc(nbeta_all.t[:, gcols]), op=ALU.mult), [Dmw, nbeta_all], [Dmw])
                vbw, kbgw, kdecw, qdTw = keep.get(), keep.get(), keep.get(), keep.get()
                Vv(lambda e, vbw=vbw, Vtw=Vtw, gcols=gcols: e.tensor_tensor(out=vbw.t[:], in0=Vtw.t[:], in1=bc(beta_all.t[:, gcols]), op=ALU.mult), [Vtw, beta_all], [vbw])
                bg = colring.get()
                Vv(lambda e, bg=bg, egc=egc, gcols=gcols: e.tensor_tensor(out=bg.t[:], in0=beta_all.t[:, gcols], in1=egc.t[:], op=ALU.mult), [beta_all, egc], [bg])
                Vv(lambda e, kbgw=kbgw, Ktw=Ktw, bg=bg: e.tensor_tensor(out=kbgw.t[:], in0=Ktw.t[:], in1=bc(bg.t[:]), op=ALU.mult), [Ktw, bg], [kbgw])
                kd = colring.get()
                Vv(lambda e, kd=kd, glc=glc, gcc=gcc: e.tensor_tensor(out=kd.t[:], in0=glc.t[:], in1=gcc.t[:], op=ALU.subtract), [glc, gcc], [kd])
                Aa(lambda e, kd=kd: e.activation(out=kd.t[:], in_=kd.t[:], func=AF.Exp), [kd], [kd])
                Vv(lambda e, kdecw=kdecw, Ktw=Ktw, kd=kd: e.tensor_tensor(out=kdecw.t[:], in0=Ktw.t[:], in1=bc(kd.t[:]), op=ALU.mult), [Ktw, kd], [kdecw])
                Vv(lambda e, qdTw=qdTw, egrw=egrw, gsl=gsl: e.tensor_tensor(out=qdTw.t[:], in0=QT.t[:, gsl].rearrange("p (g c) -> p g c", g=G), in1=egrw.t[:], op=ALU.mult), [QT, egrw], [qdTw])
                glast = colring.get()
                Aa(lambda e, glast=glast, glc=glc: e.activation(out=glast.t[:], in_=glc.t[:], func=AF.Exp), [glc], [glast])
                pkk = psring.get()
                for c_ in range(G):
                    Pe(lambda e, pkk=pkk, c_=c_, n0=n0: e.matmul(pkk.ap[:, c_, :], lhsT=KT.t[:, (n0 + c_) * C:(n0 + c_ + 1) * C], rhs=KT.t[:, (n0 + c_) * C:(n0 + c_ + 1) * C], start=True, stop=True), [KT], [pkk])
                M = ring.get()
                Vv(lambda e, M=M, pkk=pkk, Dmw=Dmw: e.tensor_tensor(out=M.t[:], in0=pkk.ap, in1=Dmw.t[:], op=ALU.mult), [pkk, Dmw], [M])
                pnt = psring.get()
                for c_ in range(G):
                    Pe(lambda e, pnt=pnt, M=M, c_=c_: e.transpose(pnt.ap[:, c_, :], M.t[:, c_, :], ident.t[:]), [M, ident], [pnt])
                MT = ring.get()
                Aa(lambda e, MT=MT, pnt=pnt: e.copy(MT.t[:], pnt.ap), [pnt], [MT])
                X = ring.get()
                Vv(lambda e, X=X, MT=MT: e.tensor_tensor(out=X.t[:], in0=MT.t[:], in1=ident_b, op=ALU.add), [MT, ident], [X])
                pqk = psring.get()
                for c_ in range(G):
                    Pe(lambda e, pqk=pqk, c_=c_, n0=n0: e.matmul(pqk.ap[:, c_, :], lhsT=KT.t[:, (n0 + c_) * C:(n0 + c_ + 1) * C], rhs=QT.t[:, (n0 + c_) * C:(n0 + c_ + 1) * C], start=True, stop=True), [KT, QT], [pqk])
                QKmTw = keep.get()
                Vv(lambda e, QKmTw=QKmTw, pqk=pqk, Dtw=Dtw: e.tensor_tensor(out=QKmTw.t[:], in0=pqk.ap, in1=Dtw.t[:], op=ALU.mult), [pqk, Dtw], [QKmTw])
                for lvl in range(1, 7):
                    pm = psring.get()
                    for c_ in range(G):
                        Pe(lambda e, pm=pm, M=M, MT=MT, c_=c_: e.matmul(pm.ap[:, c_, :], lhsT=MT.t[:, c_, :], rhs=M.t[:, c_, :], start=True, stop=True), [M, MT], [pm])
                    M2 = ring.get()
                    Aa(lambda e, M2=M2, pm=pm: e.copy(M2.t[:], pm.ap), [pm], [M2])
                    if lvl < 6:
                        pmt = psring.get()
                        for c_ in range(G):
                            Pe(lambda e, pmt=pmt, M=M, MT=MT, c_=c_: e.matmul(pmt.ap[:, c_, :], lhsT=M.t[:, c_, :], rhs=MT.t[:, c_, :], start=True, stop=True), [M, MT], [pmt])
                        MT2 = ring.get()
                        Vv(lambda e, MT2=MT2, pmt=pmt: e.tensor_copy(MT2.t[:], pmt.ap), [pmt], [MT2])
                    else:
                        MT2 = None
                    px = psring.get()
                    for c_ in range(G):
                        Pe(lambda e, px=px, M2=M2, X=X, c_=c_: e.matmul(px.ap[:, c_, :], lhsT=M2.t[:, c_, :], rhs=X.t[:, c_, :], start=True, stop=True), [M2, X], [px])
                    X2 = ring.get()
                    Vv(lambda e, X2=X2, px=px, X=X: e.tensor_tensor(out=X2.t[:], in0=px.ap, in1=X.t[:], op=ALU.add), [px, X], [X2])
                    M, MT, X = M2, MT2, X2
                Tt = X
                pu = psring.get()
                for c_ in range(G):
                    Pe(lambda e, pu=pu, Tt=Tt, vbw=vbw, c_=c_: e.matmul(pu.ap[:, c_, :], lhsT=Tt.t[:, c_, :], rhs=vbw.t[:, c_, :], start=True, stop=True), [Tt, vbw], [pu])
                U0w = keep.get()
                Aa(lambda e, U0w=U0w, pu=pu: e.copy(U0w.t[:], pu.ap), [pu], [U0w])
                pw = psring.get()
                for c_ in range(G):
                    Pe(lambda e, pw=pw, Tt=Tt, kbgw=kbgw, c_=c_: e.matmul(pw.ap[:, c_, :], lhsT=kbgw.t[:, c_, :], rhs=Tt.t[:, c_, :], start=True, stop=True), [Tt, kbgw], [pw])
                WTw = keep.get()
                Aa(lambda e, WTw=WTw, pw=pw: e.copy(WTw.t[:], pw.ap), [pw], [WTw])
                zht = zhr.get()
                S.op("sp", lambda e, zht=zht, gsl=gsl, h=h: e.dma_start(out=zht.t[:], in_=ZS[gsl, h * 128:(h + 1) * 128].rearrange("(g p) c -> p g c", p=128)), reads=[R["ZS"]], writes=[zht.r], stream=zs_)
                d.update(U0=U0w, WT=WTw, qdT=qdTw, QKmT=QKmTw, kdec=kdecw, glast=glast, zh=zht)
                return d

            def chain_group(d, ys_t=ys_t):
                gi = d["gi"]
                n0 = gi * G
                gsl = slice(n0 * C, (n0 + G) * C)
                po_ = po_bank
                vnw = ring.get()
                for c_ in range(G):
                    p1 = psring.get()
                    Pe(lambda e, p1=p1, d=d, c_=c_: e.matmul(p1.ap[:, 0, :], lhsT=d["WT"].t[:, c_, :], rhs=Sst.t[:], start=True, stop=True), [d["WT"], Sst], [p1])
                    Vv(lambda e, vnw=vnw, p1=p1, d=d, c_=c_: e.tensor_tensor(out=vnw.t[:, c_, :], in0=d["U0"].t[:, c_, :], in1=p1.ap[:, 0, :], op=ALU.subtract), [d["U0"], p1], [vnw])
                    Pe(lambda e, po_=po_, d=d, c_=c_: e.matmul(po_.ap[:, c_, :], lhsT=d["qdT"].t[:, c_, :], rhs=Sst.t[:], start=True, stop=False), [d["qdT"], Sst], [po_])
                    Pe(lambda e, po_=po_, d=d, vnw=vnw, c_=c_: e.matmul(po_.ap[:, c_, :], lhsT=d["QKmT"].t[:, c_, :], rhs=vnw.t[:, c_, :], start=False, stop=True), [d["QKmT"], vnw], [po_])
                    ps_s = psring.get()
                    Pe(lambda e, ps_s=ps_s, d=d, vnw=vnw, c_=c_: e.matmul(ps_s.ap[:, 0, :], lhsT=d["kdec"].t[:, c_, :], rhs=vnw.t[:, c_, :], start=True, stop=True), [d["kdec"], vnw], [ps_s])
                    Vv(lambda e, ps_s=ps_s, d=d, c_=c_: e.scalar_tensor_tensor(out=Sst.t[:], in0=Sst.t[:], scalar=d["glast"].t[:, c_:c_ + 1], in1=ps_s.ap[:, 0, :], op0=ALU.mult, op1=ALU.add),
                       [Sst, d["glast"], ps_s], [Sst])
                ow, sqw = ring.get(), ring.get()
                ssq = colring.get()
                Aa(lambda e, ow=ow, po_=po_: e.copy(ow.t[:], po_.ap), [po_], [ow])
                Aa(lambda e, ow=ow, sqw=sqw: e.activation(out=sqw.t[:], in_=ow.t[:], func=AF.Square), [ow], [sqw])
                Vv(lambda e, ssq=ssq, sqw=sqw: e.reduce_sum(out=ssq.t[:], in_=sqw.t[:], axis=AX.X), [sqw], [ssq])
                Vv(lambda e, ssq=ssq: e.tensor_scalar(out=ssq.t[:], in0=ssq.t[:], scalar1=1.0 / 128.0, scalar2=1e-6, op0=ALU.mult, op1=ALU.add), [ssq], [ssq])
                Aa(lambda e, ssq=ssq: e.activation(out=ssq.t[:], in_=ssq.t[:], func=AF.Sqrt), [ssq], [ssq])
                Vv(lambda e, ssq=ssq: e.reciprocal(ssq.t[:], ssq.t[:]), [ssq], [ssq])
                Vv(lambda e, ow=ow, ssq=ssq: e.tensor_tensor(out=ow.t[:], in0=ow.t[:], in1=bc(ssq.t[:]), op=ALU.mult), [ow, ssq], [ow])
                Vv(lambda e, ow=ow: e.tensor_tensor(out=ow.t[:], in0=ow.t[:], in1=nw_b, op=ALU.mult), [ow, nw], [ow])
                Vv(lambda e, ow=ow, d=d: e.tensor_tensor(out=ow.t[:], in0=ow.t[:], in1=d["zh"].t[:], op=ALU.mult), [ow, d["zh"]], [ow])
                pt = psring.get()
                for c_ in range(G):
                    Pe(lambda e, pt=pt, ow=ow, c_=c_: e.transpose(pt.ap[:, c_, :], ow.t[:, c_, :], ident.t[:]), [ow, ident], [pt])
                Aa(lambda e, pt=pt, gsl=gsl, ys_t=ys_t: e.copy(ys_t.t[:, gsl], pt.ap.rearrange("p g c -> p (g c)")), [pt], [ys_t])

            NGR = DBG.get('gdn_groups', NG)
            pend = pre_group(0)
            for gi in range(NGR):
                nxt = pre_group(gi + 1) if gi + 1 < NGR else None
                chain_group(pend)
                pend = nxt
            S.op("sp", lambda e, h=h, ys_t=ys_t: e.dma_start(out=YTs[h], in_=ys_t.t[:]), reads=[ys_t.r], writes=[R["YTs"]], stream=yss[0])

TWO_PI = 6.283185307179586


def l0_s5(nc, S, W, scr, R):
    P0T, YTs = scr["P0T"], scr["YTs"]
    TB = 512
    NBK = T // TB
    with S.phase() as a:
        c = make_consts(S, a)
        ident = c["ident"]
        cs = S.stream("s5c")
        cs2 = S.stream("s5c2")
        prmT = Tl(a.sb("prmT", [16, 3, 128], F32))
        S.op("sp", lambda e: e.dma_start(out=prmT.t[:, 0, :], in_=W["s5_lam_re"][0].rearrange("(t g) p -> t (g p)", g=2)), writes=[prmT.r], stream=cs)
        S.op("sp", lambda e: e.dma_start(out=prmT.t[:, 1, :], in_=W["s5_lam_im"][0].rearrange("(t g) p -> t (g p)", g=2)), writes=[prmT.r], stream=cs)
        ldt2 = Tl(a.sb("ldt2", [16, 2], F32))
        S.op("sp", lambda e: e.dma_start(out=ldt2.t[:], in_=W["s5_log_dt"][0].rearrange("(t g) -> t g", g=2)), writes=[ldt2.r], stream=cs)
        S.op("dve", lambda e: e.tensor_copy(prmT.t[:, 2, :].rearrange("t (g p) -> t g p", g=2), ldt2.t[:].unsqueeze(2).to_broadcast([16, 2, 64])),
             reads=[ldt2.r], writes=[prmT.r])
        dT = Tl(a.sb("dT", [4, 128], F32))
        S.op("sp", lambda e: e.dma_start(out=dT.t[:], in_=W["s5_d"][0].rearrange("(c p) -> c p", p=128)), writes=[dT.r], stream=cs)
        pp0 = Tl(a.ps("pp0", [128, 512], F32))
        for j in range(3):
            S.op("pe", lambda e, j=j: e.transpose(pp0.t[:, j * 16:(j + 1) * 16], prmT.t[0:16, j, :], ident.t[0:16, 0:16]), reads=[prmT.r, ident.r], writes=[pp0.r])
        S.op("pe", lambda e: e.transpose(pp0.t[:, 48:52], dT.t[0:4, :], ident.t[0:4, 0:4]), reads=[dT.r, ident.r], writes=[pp0.r])
        prm = Tl(a.sb("prm", [128, 52], F32))
        S.op("dve", lambda e: e.tensor_copy(prm.t[:], pp0.t[:, 0:52]), reads=[pp0.r], writes=[prm.r])
        lr, li, ldt, dsk = prm.t[:, 0:16], prm.t[:, 16:32], prm.t[:, 32:48], prm.t[:, 48:52]
        sm = {}
        for nm in ["dt", "lrdt", "th", "mag", "y", "ay", "sn", "cs", "are", "aim", "den", "t1", "t2", "cr", "ci", "nr"]:
            sm[nm] = Tl(a.sb("s5_" + nm, [128, 16], F32))
        ni16 = Tl(a.sb("s5_ni", [128, 16], I32))
        Vv = lambda fn, rd, wr: S.op("dve", fn, reads=[x.r for x in rd], writes=[x.r for x in wr])
        Aa = lambda fn, rd, wr: S.op("act", fn, reads=[x.r for x in rd], writes=[x.r for x in wr])
        Aa(lambda e: e.activation(out=sm["dt"].t[:], in_=ldt, func=AF.Exp), [prm], [sm["dt"]])
        Vv(lambda e: e.tensor_tensor(out=sm["lrdt"].t[:], in0=lr, in1=sm["dt"].t[:], op=ALU.mult), [prm, sm["dt"]], [sm["lrdt"]])
        Vv(lambda e: e.tensor_tensor(out=sm["th"].t[:], in0=li, in1=sm["dt"].t[:], op=ALU.mult), [prm, sm["dt"]], [sm["th"]])
        Aa(lambda e: e.activation(out=sm["mag"].t[:], in_=sm["lrdt"].t[:], func=AF.Exp), [sm["lrdt"]], [sm["mag"]])

        def trig(ang, n_i, y, ay, sn, cs_, rd):
            Vv(lambda e: e.tensor_scalar(out=n_i.t[:], in0=ang.t[:], scalar1=1.0 / TWO_PI, scalar2=None, op0=ALU.mult), rd + [ang], [n_i])
            Vv(lambda e: e.scalar_tensor_tensor(out=y.t[:], in0=n_i.t[:], scalar=-TWO_PI, in1=ang.t[:], op0=ALU.mult, op1=ALU.add), [n_i, ang], [y])
            Aa(lambda e: e.activation(out=sn.t[:], in_=y.t[:], func=AF.Sin, scale=0.999999), [y], [sn])
            Aa(lambda e: e.activation(out=ay.t[:], in_=y.t[:], func=AF.Abs), [y], [ay])
            Aa(lambda e: e.activation(out=cs_.t[:], in_=ay.t[:], func=AF.Sin, bias=halfpi.t[:, 0:1], scale=-0.999999), [ay, halfpi], [cs_])

        halfpi = Tl(a.sb("halfpi", [128, 1], F32))
        S.op("pool", lambda e: e.memset(halfpi.t[:], 1.5707963), writes=[halfpi.r])
        trig(sm["th"], ni16, sm["y"], sm["ay"], sm["sn"], sm["cs"], [])
        Vv(lambda e: e.tensor_tensor(out=sm["are"].t[:], in0=sm["mag"].t[:], in1=sm["cs"].t[:], op=ALU.mult), [sm["mag"], sm["cs"]], [sm["are"]])
        Vv(lambda e: e.tensor_tensor(out=sm["aim"].t[:], in0=sm["mag"].t[:], in1=sm["sn"].t[:], op=ALU.mult), [sm["mag"], sm["sn"]], [sm["aim"]])
        Vv(lambda e: e.tensor_tensor(out=sm["den"].t[:], in0=lr, in1=lr, op=ALU.mult), [prm], [sm["den"]])
        Vv(lambda e: e.tensor_tensor(out=sm["t1"].t[:], in0=li, in1=li, op=ALU.mult), [prm], [sm["t1"]])
        Vv(lambda e: e.tensor_tensor(out=sm["den"].t[:], in0=sm["den"].t[:], in1=sm["t1"].t[:], op=ALU.add), [sm["den"], sm["t1"]], [sm["den"]])
        Vv(lambda e: e.reciprocal(sm["den"].t[:], sm["den"].t[:]), [sm["den"]], [sm["den"]])
        Vv(lambda e: e.tensor_scalar(out=sm["nr"].t[:], in0=sm["are"].t[:], scalar1=-1.0, scalar2=None, op0=ALU.add), [sm["are"]], [sm["nr"]])
        Vv(lambda e: e.tensor_tensor(out=sm["t1"].t[:], in0=sm["nr"].t[:], in1=lr, op=ALU.mult), [sm["nr"], prm], [sm["t1"]])
        Vv(lambda e: e.tensor_tensor(out=sm["t2"].t[:], in0=sm["aim"].t[:], in1=li, op=ALU.mult), [sm["aim"], prm], [sm["t2"]])
        Vv(lambda e: e.tensor_tensor(out=sm["cr"].t[:], in0=sm["t1"].t[:], in1=sm["t2"].t[:], op=ALU.add), [sm["t1"], sm["t2"]], [sm["cr"]])
        Vv(lambda e: e.tensor_tensor(out=sm["cr"].t[:], in0=sm["cr"].t[:], in1=sm["den"].t[:], op=ALU.mult), [sm["cr"], sm["den"]], [sm["cr"]])
        Vv(lambda e: e.tensor_tensor(out=sm["t1"].t[:], in0=sm["aim"].t[:], in1=lr, op=ALU.mult), [sm["aim"], prm], [sm["t1"]])
        Vv(lambda e: e.tensor_tensor(out=sm["t2"].t[:], in0=sm["nr"].t[:], in1=li, op=ALU.mult), [sm["nr"], prm], [sm["t2"]])
        Vv(lambda e: e.tensor_tensor(out=sm["ci"].t[:], in0=sm["t1"].t[:], in1=sm["t2"].t[:], op=ALU.subtract), [sm["t1"], sm["t2"]], [sm["ci"]])
        Vv(lambda e: e.tensor_tensor(out=sm["ci"].t[:], in0=sm["ci"].t[:], in1=sm["den"].t[:], op=ALU.mult), [sm["ci"], sm["den"]], [sm["ci"]])
        c0 = Tl(a.sb("c0", [128, 16, NBK], F32))
        for blk in range(NBK):
            Vv(lambda e, blk=blk: e.tensor_scalar(out=c0.t[:, :, blk], in0=sm["th"].t[:], scalar1=float(blk * TB), scalar2=None, op0=ALU.mult), [sm["th"]], [c0])
        bre = Tl(a.sb("bre", [128, 16, 16], F32))
        bim = Tl(a.sb("bim", [128, 16, 16], F32))
        for g in range(2):
            S.op("sp", lambda e, g=g: e.dma_start(out=bre.t[g * 64:(g + 1) * 64, :, :], in_=W["s5_b_re"][0].rearrange("(t g) p h -> g p t h", g=2)[g]), writes=[bre.r], stream=cs2)
            S.op("sp", lambda e, g=g: e.dma_start(out=bim.t[g * 64:(g + 1) * 64, :, :], in_=W["s5_b_im"][0].rearrange("(t g) p h -> g p t h", g=2)[g]), writes=[bim.r], stream=cs2)
        bbr = Tl(a.sb("bbr", [128, 16, 16], F32))
        bbi = Tl(a.sb("bbi", [128, 16, 16], F32))
        tmpb = Tl(a.sb("tmpb", [128, 16, 16], F32))
        crb = sm["cr"].t[:].unsqueeze(2).to_broadcast([128, 16, 16])
        cib = sm["ci"].t[:].unsqueeze(2).to_broadcast([128, 16, 16])
        Vv(lambda e: e.tensor_tensor(out=bbr.t[:], in0=bre.t[:], in1=crb, op=ALU.mult), [bre, sm["cr"]], [bbr])
        Vv(lambda e: e.tensor_tensor(out=tmpb.t[:], in0=bim.t[:], in1=cib, op=ALU.mult), [bim, sm["ci"]], [tmpb])
        Vv(lambda e: e.tensor_tensor(out=bbr.t[:], in0=bbr.t[:], in1=tmpb.t[:], op=ALU.subtract), [bbr, tmpb], [bbr])
        Vv(lambda e: e.tensor_tensor(out=bbi.t[:], in0=bim.t[:], in1=crb, op=ALU.mult), [bim, sm["cr"]], [bbi])
        Vv(lambda e: e.tensor_tensor(out=tmpb.t[:], in0=bre.t[:], in1=cib, op=ALU.mult), [bre, sm["ci"]], [tmpb])
        Vv(lambda e: e.tensor_tensor(out=bbi.t[:], in0=bbi.t[:], in1=tmpb.t[:], op=ALU.add), [bbi, tmpb], [bbi])
        BBT = [[Tl(a.sb("BBT%d_%d" % (cp, st), [128, 128], F32)) for st in range(16)] for cp in range(2)]
        CT = [[Tl(a.sb("CT%d_%d" % (cp, st), [128, 128], F32)) for st in range(16)] for cp in range(2)]
        bx = [Tl(a.sb("bx%d" % i, [128, 128], F32)) for i in range(2)]
        cx = [Tl(a.sb("cx%d" % i, [128, 128], F32)) for i in range(2)]
        cxs = [S.stream("cx%d" % i) for i in range(2)]
        class _PV:
            pass
        ptp = []
        for i in range(2):
            pv = _PV()
            pv.t = pp0.t[:, 128 * (i + 1):128 * (i + 2)]
            pv.r = pp0.r
            ptp.append(pv)
        k = 0
        for st in range(16):
            s = st % 4
            cq = st // 4
            for cp, bb in ((0, bbr), (1, bbi)):
                b_, p_ = bx[k % 2], ptp[k % 2]
                S.op("pool", lambda e, b_=b_: e.memset(b_.t[:], 0.0), writes=[b_.r])
                for g in range(2):
                    S.op("pool", lambda e, b_=b_, g=g, s=s, st=st, bb=bb: e.tensor_copy(b_.t[g * 64:(g + 1) * 64, 32 * s + 16 * g:32 * s + 16 * g + 16], bb.t[g * 64:(g + 1) * 64, st, :]),
                         reads=[bb.r], writes=[b_.r])
                S.op("pe", lambda e, b_=b_, p_=p_: e.transpose(p_.t[:], b_.t[:], ident.t[:]), reads=[b_.r, ident.r], writes=[p_.r])
                S.op("act", lambda e, p_=p_, cp=cp, st=st: e.copy(BBT[cp][st].t[:], p_.t[:]), reads=[p_.r], writes=[BBT[cp][st].r])
                k += 1
            for cp, cw_ in ((0, W["s5_c_re"][0]), (1, W["s5_c_im"][0])):
                c_, p_ = cx[k % 2], ptp[k % 2]
                S.op("pool", lambda e, c_=c_: e.memset(c_.t[:], 0.0), writes=[c_.r])
                for g in range(2):
                    gg = 2 * st + g
                    r0 = (gg - 8 * cq) * 16
                    S.op("sp", lambda e, c_=c_, g=g, gg=gg, r0=r0, cw_=cw_: e.dma_start(out=c_.t[r0:r0 + 16, g * 64:(g + 1) * 64], in_=cw_[gg]), writes=[c_.r], stream=cxs[k % 2])
                S.op("pe", lambda e, c_=c_, p_=p_: e.transpose(p_.t[:], c_.t[:], ident.t[:]), reads=[c_.r, ident.r], writes=[p_.r])
                if cp == 0:
                    S.op("act", lambda e, p_=p_, st=st: e.copy(CT[0][st].t[:], p_.t[:]), reads=[p_.r], writes=[CT[0][st].r])
                else:
                    S.op("act", lambda e, p_=p_, st=st: e.mul(CT[1][st].t[:], p_.t[:], -1.0), reads=[p_.r], writes=[CT[1][st].r])
                k += 1
        iot = Tl(a.sb("iot", [128, TB], F32))
        S.op("pool", lambda e: e.iota(iot.t[:], pattern=[[1, TB]], base=0, channel_multiplier=0, allow_small_or_imprecise_dtypes=True), writes=[iot.r])
        uT = [Tl(a.sb("uT%d" % i, [128, T], F32)) for i in range(1)]
        yg = Tl(a.sb("yg", [128, 4, T], BF16))
        ring = Ring([Tl(a.sb("s5r%d" % i, [128, TB], F32)) for i in range(40)])
        iring = Ring([Tl(a.sb("s5i%d" % i, [128, TB], I32)) for i in range(4)])
        cring = Ring([Tl(a.sb("s5c%d" % i, [128, 1], F32)) for i in range(24)])
        pbu = [Tl(a.ps("pbu%d" % i, [128, 512], F32)) for i in range(4)]
        pyy = [Tl(a.ps("pyy%d" % i, [128, 512], F32)) for i in range(2)]
        npb = [0]
        npy = [0]
        for cq in range(4):
            u = uT[0]
            S.op("sp", lambda e, cq=cq, u=u: e.dma_start(out=u.t[:], in_=P0T[12 + cq]), reads=[R["P0T"]], writes=[u.r], stream=True)
            cars = {}
            pys = {}

            def it_gen(blk, s, cq=cq, u=u, cars=cars, pys=pys):
                sl = slice(blk * TB, (blk + 1) * TB)
                st = 4 * cq + s
                if s == 0:
                    pys[blk] = pyy[npy[0] % 2]
                    npy[0] += 1
                py = pys[blk]
                pr, pi = pbu[npb[0] % 4], pbu[(npb[0] + 1) % 4]
                npb[0] += 2
                S.op("pe", lambda e: e.matmul(pr.t[:], lhsT=BBT[0][st].t[:], rhs=u.t[:, sl], start=True, stop=True), reads=[BBT[0][st].r, u.r], writes=[pr.r])
                S.op("pe", lambda e: e.matmul(pi.t[:], lhsT=BBT[1][st].t[:], rhs=u.t[:, sl], start=True, stop=True), reads=[BBT[1][st].r, u.r], writes=[pi.r])
                bur, bui, y, ay, sn, cs_ = [ring.get() for _ in range(6)]
                n_i = iring.get()
                Aa(lambda e: e.activation(out=y.t[:], in_=iot.t[:], func=AF.Identity, scale=sm["th"].t[:, st:st + 1], bias=c0.t[:, st, blk:blk + 1]),
                   [iot, sm["th"], c0], [y])
                Aa(lambda e: e.copy(bur.t[:], pr.t[:]), [pr], [bur])
                Aa(lambda e: e.copy(bui.t[:], pi.t[:]), [pi], [bui])
                yield
                Vv(lambda e: e.tensor_scalar(out=n_i.t[:], in0=y.t[:], scalar1=1.0 / TWO_PI, scalar2=None, op0=ALU.mult), [y], [n_i])
                Vv(lambda e: e.scalar_tensor_tensor(out=y.t[:], in0=n_i.t[:], scalar=-TWO_PI, in1=y.t[:], op0=ALU.mult, op1=ALU.add), [n_i, y], [y])
                Aa(lambda e: e.activation(out=sn.t[:], in_=y.t[:], func=AF.Sin, scale=0.999999), [y], [sn])
                Aa(lambda e: e.activation(out=ay.t[:], in_=y.t[:], func=AF.Abs), [y], [ay])
                Aa(lambda e: e.activation(out=cs_.t[:], in_=ay.t[:], func=AF.Sin, bias=halfpi.t[:, 0:1], scale=-0.999999), [ay, halfpi], [cs_])
                yield
                TT = lambda o_, a_, b_, op: Vv(lambda e: e.tensor_tensor(out=o_.t[:], in0=a_.t[:], in1=b_.t[:], op=op), [a_, b_], [o_])
                t1, t2, t3, t4 = [ring.get() for _ in range(4)]
                TT(t1, cs_, bur, ALU.mult)
                TT(t2, sn, bui, ALU.mult)
                TT(t1, t1, t2, ALU.add)
                TT(t3, cs_, bui, ALU.mult)
                TT(t2, sn, bur, ALU.mult)
                TT(t3, t3, t2, ALU.subtract)
                rbc = sm["mag"].t[:, st:st + 1].to_broadcast([128, TB])
                car_r, car_i = cars.get(s, (None, None))
                for (gout, zin, car) in ((t2, t1, car_r), (t4, t3, car_i)):
                    if car is None:
                        Vv(lambda e, gout=gout, zin=zin: e.tensor_tensor_scan(out=gout.t[:], data0=rbc, data1=zin.t[:], initial=0.0, op0=ALU.mult, op1=ALU.add), [sm["mag"], zin], [gout])
                    else:
                        Vv(lambda e, gout=gout, zin=zin, car=car: e.tensor_tensor_scan(out=gout.t[:], data0=rbc, data1=zin.t[:], initial=car.t[:, 0:1], op0=ALU.mult, op1=ALU.add), [sm["mag"], zin, car], [gout])
                gr, gi = t2, t4
                ncr, nci = cring.get(), cring.get()
                S.op("pool", lambda e: e.tensor_copy(ncr.t[:], gr.t[:, TB - 1:TB]), reads=[gr.r], writes=[ncr.r])
                S.op("pool", lambda e: e.tensor_copy(nci.t[:], gi.t[:, TB - 1:TB]), reads=[gi.r], writes=[nci.r])
                cars[s] = (ncr, nci)
                TT(t1, cs_, gr, ALU.mult)
                TT(t3, sn, gi, ALU.mult)
                TT(t1, t1, t3, ALU.subtract)
                TT(t3, sn, gr, ALU.mult)
                TT(bur, cs_, gi, ALU.mult)
                TT(t3, t3, bur, ALU.add)
                S.op("pe", lambda e: e.matmul(py.t[:], lhsT=CT[0][st].t[:], rhs=t1.t[:], start=(s == 0), stop=False), reads=[CT[0][st].r, t1.r], writes=[py.r])
                S.op("pe", lambda e: e.matmul(py.t[:], lhsT=CT[1][st].t[:], rhs=t3.t[:], start=False, stop=(s == 3)), reads=[CT[1][st].r, t3.r], writes=[py.r])
                if s == 3:
                    yb = ring.get()
                    Vv(lambda e: e.scalar_tensor_tensor(out=yb.t[:], in0=u.t[:, sl], scalar=dsk[:, cq:cq + 1], in1=py.t[:], op0=ALU.mult, op1=ALU.add), [u, prm, py], [yb])
                    Aa(lambda e: e.activation(out=yg.t[:, cq, sl], in_=yb.t[:], func=AF.Gelu), [yb], [yg])

            interleave([(lambda blk=blk, s=s: it_gen(blk, s)) for blk in range(NBK) for s in range(4)], 3)
        wgl = Tl(a.sb("wgl", [128, 4, 512], BF16))
        S.op("pool", lambda e: e.dma_start(out=wgl.t[:], in_=W["s5_w_glu"][0].rearrange("(k p) f -> p k f", p=128)), writes=[wgl.r], stream=cs2)
        ygs = [Tl(a.sb("ygs%d" % i, [128, T], BF16)) for i in range(2)]
        ygss = [S.stream("ygss%d" % i) for i in range(2)]
        for oc in range(4):
            og = ygs[oc % 2]
            for tcb in range(8):
                sl = slice(tcb * 512, (tcb + 1) * 512)
                py = pyy[npy[0] % 2]
                npy[0] += 1
                for kc in range(4):
                    S.op("pe", lambda e, py=py, kc=kc, oc=oc, sl=sl: e.matmul(py.t[:], lhsT=wgl.t[:, kc, oc * 128:(oc + 1) * 128], rhs=yg.t[:, kc, sl], start=(kc == 0), stop=(kc == 3)),
                         reads=[wgl.r, yg.r], writes=[py.r])
                sg = ring.get()
                Aa(lambda e, sg=sg, py=py: e.activation(out=sg.t[:], in_=py.t[:], func=AF.Exp, scale=-1.0), [py], [sg])
                Vv(lambda e, sg=sg: e.tensor_scalar(out=sg.t[:], in0=sg.t[:], scalar1=1.0, scalar2=None, op0=ALU.add), [sg], [sg])
                Vv(lambda e, sg=sg: e.reciprocal(sg.t[:], sg.t[:]), [sg], [sg])
                Vv(lambda e, sg=sg, og=og, oc=oc, sl=sl: e.tensor_tensor(out=og.t[:, sl], in0=sg.t[:], in1=yg.t[:, oc, sl], op=ALU.mult), [sg, yg], [og])
            S.op("sp", lambda e, oc=oc, og=og: e.dma_start(out=YTs[4 + oc], in_=og.t[:]), reads=[og.r], writes=[R["YTs"]], stream=ygss[oc % 2])


def l0_stage(nc, S, h_in, h_out, W, l, scr, Rh_in, Rh_out, parts=("inproj", "gdn", "s5", "out")):
    R = {"P0T": Res(), "ZS": Res(), "YTs": Res()}
    with ExitStack() as st0:
        ba = st0.enter_context(nc.sbuf_tensor("ba_sb", [128, NT, 8], F32))
        ba_r = Res()
        if "inproj" in parts:
            l0_inproj(nc, S, h_in, Rh_in, W, scr, R, ba, ba_r)
        else:
            with S.phase() as a:
                S.op("pool", lambda e: e.memset(ba[:], -0.5), writes=[ba_r])
        if "gdn" in parts:
            l0_gdn(nc, S, W, scr, R, ba, ba_r)
        if "s5" in parts:
            l0_s5(nc, S, W, scr, R)
        if "out" in parts:
            outproj_ln_phase(nc, S, scr["YTs"], R["YTs"], W["w_out_ab"][0], h_in, Rh_in, h_out, Rh_out, W["ln_mix_g"][l:l + 1, :], W["ln_mix_b"][l:l + 1, :])


W_SHAPES = [
    ("w_in_ab", [1, D, 2568]), ("conv_qkv", [1, 4, 1536]), ("gdn_a_log", [1, 4]), ("gdn_dt_bias", [1, 4]), ("gdn_norm", [1, 128]),
    ("s5_lam_re", [1, 32, 64]), ("s5_lam_im", [1, 32, 64]), ("s5_log_dt", [1, 32]), ("s5_b_re", [1, 32, 64, 16]), ("s5_b_im", [1, 32, 64, 16]),
    ("s5_c_re", [1, 32, 16, 64]), ("s5_c_im", [1, 32, 16, 64]), ("s5_d", [1, 512]), ("s5_w_glu", [1, 512, 512]), ("w_out_ab", [1, D, D]),
    ("w_qkv_c", [1, D, 3 * D]), ("w_out_c", [1, D, D]), ("ln_mix_g", [2, D]), ("ln_mix_b", [2, D]),
    ("router_group_w", [2, D, 4]), ("router_group_b", [2, 4]), ("router_expert_w", [2, D, 32]), ("router_expert_b", [2, 32]),
    ("moe_w_gate", [2, 32, D, FF]), ("moe_w_up", [2, 32, D, FF]), ("moe_w_down", [2, 32, FF, D]), ("ln_ffn_g", [2, D]), ("ln_ffn_b", [2, D]),
]


def build_program(stages=("l0", "moe0", "attn", "moe1")):
    nc = bass.Bass("TRN2", target_bir_lowering=False)
    x = nc.dram_tensor("x", [T, D], F32, kind="ExternalInput").ap()
    out = nc.dram_tensor("out", [T, D], F32, kind="ExternalOutput").ap()
    W = {nm: nc.dram_tensor(nm, sh, F32, kind="ExternalInput").ap() for nm, sh in W_SHAPES}
    I = lambda n, sh, dt: nc.dram_tensor(n, sh, dt, kind="Internal").ap()
    scr = {
        "Xs": I("Xs", [NSLOT + 128, D], BF16), "Ys": I("Ys", [NSLOT, D], F32),
        "QTs": I("QTs", [8, 128, T], BF16), "KTs": I("KTs", [8, 128, T], BF16), "Vs": I("Vs", [3, T, D], BF16), "OTs": I("OTs", [8, 128, T], BF16),
        "P0T": I("P0T", [16, 128, T], F32), "ZS": I("ZS", [T, 512], F32), "YTs": I("YTs", [8, 128, T], BF16),
    }
    names = list(stages)
    bufs = [x]
    for i in range(len(names) - 1):
        bufs.append(I("hbuf%d" % i, [T, D], F32))
    bufs.append(out)
    with ExitStack() as st:
        S = Sched(nc, st)
        for i, nm in enumerate(names):
            hi, ho = bufs[i], bufs[i + 1]
            Ri, Ro = Res(), Res()
            if nm == "l0":
                l0_stage(nc, S, hi, ho, W, 0, scr, Ri, Ro)
            elif nm == "moe0":
                moe_stage(nc, S, hi, ho, W, 0, scr, Ri, Ro)
            elif nm == "attn":
                attn_stage(nc, S, hi, ho, W, 1, scr, Ri, Ro)
            elif nm == "moe1":
                moe_stage(nc, S, hi, ho, W, 1, scr, Ri, Ro)
        with S.phase(final=True) as a:
            pass
    return nc


_PROG = {}


def kernel(**inputs):
    x = np.ascontiguousarray(np.asarray(inputs["x"], dtype=np.float32))
    B = x.shape[0]
    if "full" not in _PROG:
        _PROG["full"] = build_program()
    nc = _PROG["full"]
    wmap = {nm: np.ascontiguousarray(np.asarray(inputs[nm], dtype=np.float32)) for nm, _ in W_SHAPES}
    in_maps = []
    for b in range(B):
        m = dict(wmap)
        m["x"] = x[b]
        in_maps.append(m)
    res = run_bass_kernel_spmd(nc, in_maps, core_ids=list(range(B)))
    return np.stack([np.asarray(r["out"], dtype=np.float32) for r in res.results], axis=0)
```

```python
import numpy as np
from contextlib import ExitStack, contextmanager
import concourse.bass as bass
import concourse.mybir as mybir
from concourse.bass_utils import run_bass_kernel_spmd

F32 = mybir.dt.float32
BF16 = mybir.dt.bfloat16
I32 = mybir.dt.int32
AF = mybir.ActivationFunctionType
ALU = mybir.AluOpType
AX = mybir.AxisListType

ENGS = ("pe", "act", "dve", "pool", "sp")
DBG = {}
NPOOL = 64
NPOOL_SW = 24

T = 4096
NT = 32
D = 1024
KD = 8
NE = 32
FF = 512
CAP = 384
NB = CAP // 128
NSLOT = NE * CAP
ALPHA = 4.0 ** 0.25
LN_EPS = 1e-5


class Res:
    __slots__ = ("name", "w", "r")

    def __init__(self, name=""):
        self.name = name
        self.w = None
        self.r = []


class Stream:
    def __init__(self, sem):
        self.sem = sem
        self.n = 0


class Op:
    __slots__ = ("eng", "fn", "deps", "stream", "sig", "val", "done")

    def __init__(self, eng, fn, stream):
        self.eng = eng
        self.fn = fn
        self.deps = []
        self.stream = stream
        self.sig = stream is not None
        self.val = None
        self.done = False


class Sched:
    def __init__(self, nc, stack):
        self.nc = nc
        self.stack = stack
        self.ops = {e: [] for e in ENGS}
        self.sems = {e: stack.enter_context(nc.semaphore("sem_" + e)) for e in ENGS if e != "sp"}
        self.cnt = {e: 0 for e in ENGS}
        self.obs = {e: {} for e in ENGS}
        self.streams = [Stream(stack.enter_context(nc.semaphore("dmap%d" % i))) for i in range(NPOOL)]
        for st_ in self.streams:
            st_.last_op = None
        self.pool_i = 0
        self.pool_sw = 0
        self.last = {e: None for e in ENGS}
        self.nstream = 0

    def stream(self, name=""):
        return True

    def op(self, eng, fn, reads=(), writes=(), stream=None):
        deps = set()
        if stream is not None:
            if eng == "pool":
                stream = self.streams[self.pool_sw % NPOOL_SW]
                self.pool_sw += 1
            else:
                stream = self.streams[NPOOL_SW + self.pool_i % (NPOOL - NPOOL_SW)]
                self.pool_i += 1
            if stream.last_op is not None:
                deps.add(stream.last_op)
        o = Op(eng, fn, stream)
        for r in reads:
            if r.w is not None:
                deps.add(r.w)
        for w in writes:
            if w.w is not None:
                deps.add(w.w)
            for rr in w.r:
                deps.add(rr)
        for d in deps:
            if d is o or d.done:
                continue
            if d.eng == eng and eng == "pe" and d.stream is None and stream is None:
                continue
            d.sig = True
            o.deps.append((d, None))
        for r in reads:
            r.r.append(o)
        for w in writes:
            w.w = o
            w.r = []
        if stream is not None:
            stream.n += 1
            o.val = 16 * stream.n
            stream.last_op = o
        self.ops[eng].append(o)
        self.last[eng] = o
        return o

    def barrier(self):
        lasts = [self.last[e] for e in ENGS if self.last[e] is not None and self.last[e].stream is None]
        stream_last = []
        seen = set()
        for e in ENGS:
            for o in reversed(self.ops[e]):
                if o.stream is not None and id(o.stream) not in seen:
                    seen.add(id(o.stream))
                    stream_last.append(o)
        for e in ENGS:
            o = Op(e, None, None)
            for d in lasts + stream_last:
                if d.eng == e and d.stream is None and e == "pe":
                    continue
                d.sig = True
                o.deps.append((d, None))
            self.ops[e].append(o)

    def flush(self, final=False):
        nc = self.nc
        for e in ENGS:
            for o in self.ops[e]:
                if o.stream is None and o.sig and o.fn is not None:
                    assert e != "sp"
                    self.cnt[e] += 1
                    o.val = self.cnt[e]
        sems, obs, ops, streams = self.sems, self.obs, self.ops, self.streams

        def emit(e, engobj):
            ob = obs[e]
            for o in ops[e]:
                for d, need in o.deps:
                    sem = d.stream.sem if d.stream is not None else sems[d.eng]
                    val = need if need is not None else d.val
                    key = id(sem)
                    if ob.get(key, 0) >= val:
                        continue
                    ob[key] = val
                    engobj.wait_ge(sem, val)
                if o.fn is None:
                    continue
                ins = o.fn(engobj)
                if o.stream is not None:
                    ins.then_inc(o.stream.sem, 16)
                elif o.sig:
                    ins.then_inc(sems[e], 1)
            if final and e == "sp":
                for st in streams:
                    if st.n > 0:
                        engobj.wait_ge(st.sem, 16 * st.n)

        with nc.Block() as block:
            @block.tensor
            def _(t):
                emit("pe", t)

            @block.scalar
            def _(t):
                emit("act", t)

            @block.vector
            def _(t):
                emit("dve", t)

            @block.gpsimd
            def _(t):
                emit("pool", t)

            @block.sync
            def _(t):
                emit("sp", t)
        for e in ENGS:
            for o in self.ops[e]:
                o.done = True
        self.ops = {e: [] for e in ENGS}
        self.last = {e: None for e in ENGS}

    @contextmanager
    def phase(self, final=False):
        with ExitStack() as st:
            nc = self.nc

            class A:
                pass
            a = A()
            def _nm(n):
                self.nstream += 1
                return "%s_u%d" % (n, self.nstream)
            a.sb = lambda n, sh, dt=F32: st.enter_context(nc.sbuf_tensor(_nm(n), list(sh), dt))
            a.ps = lambda n, sh, dt=F32: st.enter_context(nc.psum_tensor(_nm(n), list(sh), dt))
            yield a
            self.barrier()
            self.flush(final=final)


def interleave(gen_fns, depth):
    active = []
    it = iter(gen_fns)
    exhausted = False
    while True:
        if not exhausted and len(active) < depth:
            try:
                active.append(next(it)())
            except StopIteration:
                exhausted = True
        if not active:
            if exhausted:
                break
            continue
        for g in list(active):
            try:
                next(g)
            except StopIteration:
                active.remove(g)


class Tl:
    def __init__(self, t, name=""):
        self.t = t
        self.r = Res(name)


def make_consts(S, a):
    c = {}
    ident = Tl(a.sb("ident", [128, 128], F32))
    S.op("pool", lambda e: e.memset(ident.t[:], 0.0), writes=[ident.r])
    S.op("pool", lambda e: e.affine_select(out=ident.t[:], in_=ident.t[:], pattern=[[-1, 128]],
                                            compare_op=ALU.not_equal, fill=1.0, base=0, channel_multiplier=1),
         reads=[ident.r], writes=[ident.r])
    c["ident"] = ident
    identb = Tl(a.sb("identb", [128, 128], BF16))
    S.op("pool", lambda e: e.tensor_copy(identb.t[:], ident.t[:]), reads=[ident.r], writes=[identb.r])
    c["identb"] = identb
    return c


def layer_norm_gen(S, r, out, g, b, stats, mv, rstd):
    for j in range(2):
        S.op("dve", lambda e, j=j: e.bn_stats(stats.t[:, j, :], r.t[:, j * 512:(j + 1) * 512]),
             reads=[r.r], writes=[stats.r])
    S.op("dve", lambda e: e.bn_aggr(mv.t[:], stats.t[:]), reads=[stats.r], writes=[mv.r])
    S.op("dve", lambda e: e.tensor_scalar(out=rstd.t[:, 0:1], in0=mv.t[:, 1:2], scalar1=LN_EPS, scalar2=None,
                                          op0=ALU.add), reads=[mv.r], writes=[rstd.r])
    S.op("act", lambda e: e.sqrt(rstd.t[:, 0:1], rstd.t[:, 0:1]), reads=[rstd.r], writes=[rstd.r])
    yield
    S.op("dve", lambda e: e.reciprocal(rstd.t[:, 0:1], rstd.t[:, 0:1]), reads=[rstd.r], writes=[rstd.r])
    S.op("dve", lambda e: e.scalar_tensor_tensor(out=rstd.t[:, 1:2], in0=mv.t[:, 0:1], scalar=-1.0, in1=rstd.t[:, 0:1], op0=ALU.mult, op1=ALU.mult),
         reads=[mv.r, rstd.r], writes=[rstd.r])
    S.op("act", lambda e: e.activation(out=out.t[:], in_=r.t[:], func=AF.Identity, scale=rstd.t[:, 0:1], bias=rstd.t[:, 1:2]),
         reads=[r.r, rstd.r], writes=[out.r])
    yield
    S.op("dve", lambda e: e.tensor_tensor(out=out.t[:], in0=out.t[:], in1=g.t[:], op=ALU.mult),
         reads=[out.r, g.r], writes=[out.r])
    S.op("dve", lambda e: e.tensor_tensor(out=out.t[:], in0=out.t[:], in1=b.t[:], op=ALU.add),
         reads=[out.r, b.r], writes=[out.r])


def moe_stage(nc, S, h_in, h_out, W, l, scr, Rh_in, Rh_out):
    Xs, Ys = scr["Xs"], scr["Ys"]
    RXs, RYs = Res("Xs"), Res("Ys")

    with ExitStack() as st0:
        slots_f = Tl(st0.enter_context(nc.sbuf_tensor("slots_f%d" % l, [128, NT, 2], F32)))
        gates = Tl(st0.enter_context(nc.sbuf_tensor("gates%d" % l, [128, NT, 2], F32)))
        slot_r = [Res() for _ in range(NT)]
        gate_r = [Res() for _ in range(NT)]

        with S.phase() as a:
            c = make_consts(S, a)
            ident = c["ident"]
            su = Tl(a.sb("su", [128, 128], BF16))
            onesb = Tl(a.sb("onesb", [128, 128], BF16))
            ones1 = Tl(a.sb("ones1", [1, 128], F32))
            ebase = Tl(a.sb("ebase", [128, NE], F32))
            S.op("pool", lambda e: e.memset(su.t[:], 1.0), writes=[su.r])
            S.op("pool", lambda e: e.affine_select(out=su.t[:], in_=su.t[:], pattern=[[1, 128]],
                                                    compare_op=ALU.is_gt, fill=0.0, base=0, channel_multiplier=-1),
                 reads=[su.r], writes=[su.r])
            S.op("pool", lambda e: e.memset(onesb.t[:], 1.0), writes=[onesb.r])
            S.op("pool", lambda e: e.memset(ones1.t[:], 1.0), writes=[ones1.r])
            S.op("pool", lambda e: e.iota(ebase.t[:], pattern=[[CAP, NE]], base=0, channel_multiplier=0,
                                          allow_small_or_imprecise_dtypes=True), writes=[ebase.r])
            pdump = Tl(a.sb("pdump", [128, 1], F32))
            S.op("pool", lambda e: e.iota(pdump.t[:], pattern=[[0, 1]], base=NSLOT, channel_multiplier=1,
                                          allow_small_or_imprecise_dtypes=True), writes=[pdump.r])
            wr = Tl(a.sb("wr", [128, KD, 36], F32))
            brow = Tl(a.sb("brow", [1, 36], F32))
            ws = S.stream("wr")
            S.op("sp", lambda e: e.dma_start(out=wr.t[:, :, 0:4], in_=W["router_group_w"][l].rearrange("(k p) g -> p k g", p=128)),
                 writes=[wr.r], stream=ws)
            S.op("sp", lambda e: e.dma_start(out=wr.t[:, :, 4:36], in_=W["router_expert_w"][l].rearrange("(k p) g -> p k g", p=128)),
                 writes=[wr.r], stream=ws)
            S.op("sp", lambda e: e.dma_start(out=brow.t[:, 0:4], in_=W["router_group_b"][l:l + 1, :]), writes=[brow.r], stream=ws)
            S.op("sp", lambda e: e.dma_start(out=brow.t[:, 4:36], in_=W["router_expert_b"][l:l + 1, :]), writes=[brow.r], stream=ws)
            zt = Tl(a.sb("zt", [128, 8192], BF16))
            S.op("pool", lambda e: e.memset(zt.t[:], 0.0), writes=[zt.r])
            zs = S.stream("zs")
            Xs_v = Xs[0:NSLOT, :].rearrange("(c p r) d -> c p (r d)", p=128, r=8)
            for ci in range(NSLOT // 1024):
                S.op("act", lambda e, ci=ci: e.dma_start(out=Xs_v[ci], in_=zt.t[:]), reads=[zt.r], writes=[RXs], stream=zs)

            asum = Tl(a.sb("asum", [128, NE], BF16))
            S.op("dve", lambda e: e.memset(asum.t[:], 0.0), writes=[asum.r])

            NBUF = 4
            ht = [Tl(a.sb("ht%d" % i, [128, D], F32)) for i in range(NBUF)]
            hb = [Tl(a.sb("hb%d" % i, [128, D], BF16)) for i in range(NBUF)]
            hT = [Tl(a.sb("hT%d" % i, [128, KD, 128], F32)) for i in range(NBUF)]
            tp = [Tl(a.ps("tp%d" % i, [128, 2, 512], F32)) for i in range(2)]
            sm = [Tl(a.ps("sm%d" % i, [128, 512], F32)) for i in range(NBUF)]
            ld = [S.stream("ld%d" % i) for i in range(NBUF)]
            sc = [[S.stream("sc%d_%d" % (i, k)) for k in range(2)] for i in range(NBUF)]
            def smalls(i):
                d = {}
                for nm, sh, dt in [("L", [128, 36], F32), ("gmax", [128, 1], F32), ("negm", [128, 1], F32),
                                   ("ohg", [128, 4], F32), ("gexp", [128, 4], F32), ("gsum", [128, 1], F32),
                                   ("pg", [128, 1], F32), ("sel", [128, 4, 8], F32), ("ing", [128, 8], F32),
                                   ("m1", [128, 1], F32), ("oh1", [128, 8], F32), ("msk", [128, 8], F32),
                                   ("m2", [128, 1], F32), ("oh2", [128, 8], F32), ("dd", [128, 1], F32),
                                   ("ee", [128, 1], F32), ("g12", [128, 2], F32), ("A1", [128, 4, 8], F32),
                                   ("A2", [128, 4, 8], F32), ("Ab", [128, NE], BF16), ("posb", [128, NE], F32),
                                   ("tmp", [128, NE], F32), ("sp4", [128, 4], F32), ("ovf", [128, 2], F32),
                                   ("slf", [128, 2], F32), ("nov", [128, 2], F32), ("si0", [128, 1], I32), ("si1", [128, 1], I32)]:
                    d[nm] = Tl(a.sb("%s_%d" % (nm, i), sh, dt))
                return d
            sml = [smalls(i) for i in range(NBUF)]

            def rt_gen(i):
                bi = i % NBUF
                h, hbb, hTt, tpp, smm, w = ht[bi], hb[bi], hT[bi], tp[i % 2], sm[bi], sml[bi]
                S.op("sp", lambda e, i=i, h=h: e.dma_start(out=h.t[:], in_=h_in[i * 128:(i + 1) * 128, :]),
                     reads=[Rh_in], writes=[h.r], stream=ld[bi])
                for k in range(KD):
                    S.op("pe", lambda e, k=k, h=h, tpp=tpp: e.transpose(tpp.t[:, k // 4, (k % 4) * 128:(k % 4 + 1) * 128],
                                                                        h.t[:, k * 128:(k + 1) * 128], ident.t[:]),
                         reads=[h.r, ident.r], writes=[tpp.r])
                for j in range(2):
                    S.op("act", lambda e, j=j, hTt=hTt, tpp=tpp: e.copy(hTt.t[:, j * 4:(j + 1) * 4, :].rearrange("p k t -> p (k t)"), tpp.t[:, j, :]),
                         reads=[tpp.r], writes=[hTt.r])
                S.op("act", lambda e, h=h, hbb=hbb: e.copy(hbb.t[:], h.t[:]), reads=[h.r], writes=[hbb.r])
                yield
                lg = smm.t[:, 0:36]
                pos = smm.t[:, 64:96]
                for k in range(KD):
                    S.op("pe", lambda e, k=k, hTt=hTt, lg=lg: e.matmul(lg, lhsT=hTt.t[:, k, :], rhs=wr.t[:, k, :], start=(k == 0), stop=False),
                         reads=[hTt.r, wr.r], writes=[smm.r])
                S.op("pe", lambda e, lg=lg: e.matmul(lg, lhsT=ones1.t[:, :], rhs=brow.t[:, :], start=False, stop=True),
                     reads=[ones1.r, brow.r], writes=[smm.r])
                yield
                V = lambda fn, rd, wrt: S.op("dve", fn, reads=[x.r for x in rd], writes=[x.r for x in wrt])
                L = w["L"]
                V(lambda e, L=L, lg=lg: e.tensor_copy(L.t[:], lg), [smm], [L])
                gl = L.t[:, 0:4]
                el = L.t[:, 4:36].rearrange("p (g x) -> p g x", g=4)
                V(lambda e, w=w, gl=gl: e.reduce_max(out=w["gmax"].t[:], in_=gl, axis=AX.X), [L], [w["gmax"]])
                V(lambda e, w=w: e.tensor_scalar(out=w["negm"].t[:], in0=w["gmax"].t[:], scalar1=-1.0, scalar2=None, op0=ALU.mult), [w["gmax"]], [w["negm"]])
                V(lambda e, w=w, gl=gl: e.tensor_scalar(out=w["ohg"].t[:], in0=gl, scalar1=w["gmax"].t[:, 0:1], scalar2=None, op0=ALU.is_equal), [L, w["gmax"]], [w["ohg"]])
                S.op("act", lambda e, w=w, gl=gl: e.activation(out=w["gexp"].t[:], in_=gl, func=AF.Exp, bias=w["negm"].t[:, 0:1], scale=1.0, accum_out=w["gsum"].t[:, 0:1]),
                     reads=[L.r, w["negm"].r], writes=[w["gexp"].r, w["gsum"].r])
                yield
                V(lambda e, w=w: e.reciprocal(w["pg"].t[:], w["gsum"].t[:]), [w["gsum"]], [w["pg"]])
                V(lambda e, w=w, el=el: e.tensor_tensor(out=w["sel"].t[:], in0=el, in1=w["ohg"].t[:].unsqueeze(2).to_broadcast([128, 4, 8]), op=ALU.mult), [L, w["ohg"]], [w["sel"]])
                V(lambda e, w=w: e.reduce_sum(out=w["ing"].t[:], in_=w["sel"].t[:].rearrange("p g x -> p x g"), axis=AX.X), [w["sel"]], [w["ing"]])
                V(lambda e, w=w: e.reduce_max(out=w["m1"].t[:], in_=w["ing"].t[:], axis=AX.X), [w["ing"]], [w["m1"]])
                V(lambda e, w=w: e.tensor_scalar(out=w["oh1"].t[:], in0=w["ing"].t[:], scalar1=w["m1"].t[:, 0:1], scalar2=None, op0=ALU.is_equal), [w["ing"], w["m1"]], [w["oh1"]])
                V(lambda e, w=w: e.scalar_tensor_tensor(out=w["msk"].t[:], in0=w["oh1"].t[:], scalar=-1e30, in1=w["ing"].t[:], op0=ALU.mult, op1=ALU.add), [w["oh1"], w["ing"]], [w["msk"]])
                V(lambda e, w=w: e.reduce_max(out=w["m2"].t[:], in_=w["msk"].t[:], axis=AX.X), [w["msk"]], [w["m2"]])
                V(lambda e, w=w: e.tensor_scalar(out=w["oh2"].t[:], in0=w["msk"].t[:], scalar1=w["m2"].t[:, 0:1], scalar2=None, op0=ALU.is_equal), [w["msk"], w["m2"]], [w["oh2"]])
                V(lambda e, w=w: e.tensor_tensor(out=w["dd"].t[:], in0=w["m2"].t[:], in1=w["m1"].t[:], op=ALU.subtract), [w["m1"], w["m2"]], [w["dd"]])
                S.op("act", lambda e, w=w: e.activation(out=w["ee"].t[:], in_=w["dd"].t[:], func=AF.Exp), reads=[w["dd"].r], writes=[w["ee"].r])
                yield
                V(lambda e, w=w: e.tensor_scalar(out=w["dd"].t[:], in0=w["ee"].t[:], scalar1=1.0, scalar2=None, op0=ALU.add), [w["ee"]], [w["dd"]])
                V(lambda e, w=w: e.reciprocal(w["g12"].t[:, 0:1], w["dd"].t[:]), [w["dd"]], [w["g12"]])
                V(lambda e, w=w: e.tensor_tensor(out=w["g12"].t[:, 1:2], in0=w["g12"].t[:, 0:1], in1=w["ee"].t[:], op=ALU.mult), [w["g12"], w["ee"]], [w["g12"]])
                V(lambda e, w=w: e.tensor_scalar(out=w["g12"].t[:], in0=w["g12"].t[:], scalar1=w["pg"].t[:, 0:1], scalar2=None, op0=ALU.mult), [w["g12"], w["pg"]], [w["g12"]])
                for nm, oh in (("A1", "oh1"), ("A2", "oh2")):
                    V(lambda e, w=w, nm=nm, oh=oh: e.tensor_tensor(out=w[nm].t[:], in0=w["ohg"].t[:].unsqueeze(2).to_broadcast([128, 4, 8]),
                                                                    in1=w[oh].t[:].unsqueeze(1).to_broadcast([128, 4, 8]), op=ALU.mult),
                      [w["ohg"], w[oh]], [w[nm]])
                V(lambda e, w=w: e.tensor_tensor(out=w["Ab"].t[:], in0=w["A1"].t[:].rearrange("p g x -> p (g x)"), in1=w["A2"].t[:].rearrange("p g x -> p (g x)"), op=ALU.add),
                  [w["A1"], w["A2"]], [w["Ab"]])
                S.op("pe", lambda e, w=w, pos=pos: e.matmul(pos, lhsT=su.t[:], rhs=w["Ab"].t[:], start=True, stop=False),
                     reads=[su.r, w["Ab"].r], writes=[smm.r])
                S.op("pe", lambda e, pos=pos: e.matmul(pos, lhsT=onesb.t[:], rhs=asum.t[:], start=False, stop=True),
                     reads=[onesb.r, asum.r], writes=[smm.r])
                yield
                V(lambda e, w=w, pos=pos: e.tensor_copy(w["posb"].t[:], pos), [smm], [w["posb"]])
                V(lambda e, w=w: e.tensor_tensor(out=asum.t[:], in0=asum.t[:], in1=w["Ab"].t[:], op=ALU.add), [asum, w["Ab"]], [asum])
                for k, nm in ((0, "A1"), (1, "A2")):
                    Ak = w[nm].t[:].rearrange("p g x -> p (g x)")
                    V(lambda e, w=w, Ak=Ak: e.tensor_tensor(out=w["tmp"].t[:], in0=Ak, in1=w["posb"].t[:], op=ALU.mult), [w[nm], w["posb"]], [w["tmp"]])
                    V(lambda e, w=w, k=k: e.reduce_sum(out=w["sp4"].t[:, k:k + 1], in_=w["tmp"].t[:], axis=AX.X), [w["tmp"]], [w["sp4"]])
                    V(lambda e, w=w, Ak=Ak: e.tensor_tensor(out=w["tmp"].t[:], in0=Ak, in1=ebase.t[:], op=ALU.mult), [w[nm], ebase], [w["tmp"]])
                    V(lambda e, w=w, k=k: e.reduce_sum(out=w["sp4"].t[:, 2 + k:3 + k], in_=w["tmp"].t[:], axis=AX.X), [w["tmp"]], [w["sp4"]])
                V(lambda e, w=w: e.tensor_scalar(out=w["ovf"].t[:], in0=w["sp4"].t[:, 0:2], scalar1=float(CAP) - 0.5, scalar2=None, op0=ALU.is_ge), [w["sp4"]], [w["ovf"]])
                V(lambda e, w=w: e.tensor_tensor(out=w["slf"].t[:], in0=w["sp4"].t[:, 0:2], in1=w["sp4"].t[:, 2:4], op=ALU.add), [w["sp4"]], [w["slf"]])
                V(lambda e, w=w: e.tensor_scalar(out=w["nov"].t[:], in0=w["ovf"].t[:], scalar1=-1.0, scalar2=1.0, op0=ALU.mult, op1=ALU.add), [w["ovf"]], [w["nov"]])
                S.op("dve", lambda e, w=w, i=i: e.tensor_tensor(out=slots_f.t[:, i, :], in0=w["slf"].t[:], in1=w["nov"].t[:], op=ALU.mult),
                     reads=[w["slf"].r, w["nov"].r], writes=[slot_r[i]])
                S.op("dve", lambda e, w=w, i=i: e.scalar_tensor_tensor(out=w["slf"].t[:], in0=w["ovf"].t[:], scalar=pdump.t[:, 0:1], in1=slots_f.t[:, i, :], op0=ALU.mult, op1=ALU.add),
                     reads=[w["ovf"].r, pdump.r, slot_r[i], w["slf"].r], writes=[w["slf"].r])
                S.op("dve", lambda e, w=w, i=i: e.tensor_tensor(out=gates.t[:, i, :], in0=w["g12"].t[:], in1=w["nov"].t[:], op=ALU.mult),
                     reads=[w["g12"].r, w["nov"].r], writes=[gate_r[i]])
                for k in range(2):
                    sik = w["si%d" % k]
                    V(lambda e, w=w, k=k, sik=sik: e.tensor_copy(sik.t[:], w["slf"].t[:, k:k + 1]), [w["slf"]], [sik])
                    S.op("pool", lambda e, i=i, k=k, hbb=hbb, sik=sik: e.indirect_dma_start(
                        out=Xs, out_offset=bass.IndirectOffsetOnAxis(ap=sik.t[:, :], axis=0),
                        in_=hbb.t[:], in_offset=None),
                        reads=[hbb.r, sik.r], writes=[RXs], stream=sc[bi][k])

            interleave([(lambda i=i: rt_gen(i)) for i in range(NT)], 4)

        with S.phase() as a:
            c = make_consts(S, a)
            identb = c["identb"]
            NW = 3
            wg = [Tl(a.sb("wg%d" % i, [128, KD, FF], BF16)) for i in range(NW)]
            wu = [Tl(a.sb("wu%d" % i, [128, KD, FF], BF16)) for i in range(NW)]
            wd = [Tl(a.sb("wd%d" % i, [128, 4, D], BF16)) for i in range(NW)]
            wst = [[S.stream("w%d_%d" % (i, j)) for j in range(3)] for i in range(NW)]
            xb = [Tl(a.sb("xb%d" % i, [128, NB, D], BF16)) for i in range(2)]
            xst = [S.stream("x%d" % i) for i in range(2)]
            xT = [Tl(a.sb("xT%d" % i, [128, KD, CAP], BF16)) for i in range(2)]
            hd = [Tl(a.sb("hd%d" % i, [128, 4, CAP], BF16)) for i in range(2)]
            sg = [Tl(a.sb("sg%d" % i, [128, CAP], F32)) for i in range(2)]
            yo = [Tl(a.sb("yo%d" % i, [128, D], F32)) for i in range(2)]
            yst = [S.stream("y%d" % i) for i in range(2)]
            tpx = [Tl(a.ps("tpx%d" % i, [128, KD * 128], BF16)) for i in range(2)]
            pg_ = [Tl(a.ps("pg%d" % i, [128, 512], F32)) for i in range(2)]
            pu_ = [Tl(a.ps("pu%d" % i, [128, 512], F32)) for i in range(2)]
            py_ = [Tl(a.ps("py%d" % i, [128, 512], F32)) for i in range(2)]
            cnt = {"tp": 0, "fc": 0, "yo": 0, "py": 0}

            def Wl(ex):
                wi = ex % NW
                S.op("pool", lambda e: e.dma_start(out=wg[wi].t[:], in_=W["moe_w_gate"][l, ex].rearrange("(k p) f -> p k f", p=128)), writes=[wg[wi].r], stream=True)
                S.op("pool", lambda e: e.dma_start(out=wu[wi].t[:], in_=W["moe_w_up"][l, ex].rearrange("(k p) f -> p k f", p=128)), writes=[wu[wi].r], stream=True)
                S.op("pool", lambda e: e.dma_start(out=wd[wi].t[:], in_=W["moe_w_down"][l, ex].rearrange("(k p) f -> p k f", p=128)), writes=[wd[wi].r], stream=True)

            def Xl(ex):
                xi = ex % 2
                S.op("sp", lambda e: e.dma_start(out=xb[xi].t[:], in_=Xs[ex * CAP:(ex + 1) * CAP, :].rearrange("(b p) d -> p b d", p=128)),
                     reads=[RXs], writes=[xb[xi].r], stream=True)

            def Tr(ex):
                xi = ex % 2
                for b in range(NB):
                    tpt = tpx[cnt["tp"] % 2]
                    cnt["tp"] += 1
                    for k in range(KD):
                        S.op("pe", lambda e, b=b, k=k, tpt=tpt: e.transpose(tpt.t[:, k * 128:(k + 1) * 128], xb[xi].t[:, b, k * 128:(k + 1) * 128], identb.t[:]),
                             reads=[xb[xi].r, identb.r], writes=[tpt.r])
                    if b % 2 == 0:
                        S.op("act", lambda e, b=b, tpt=tpt: e.copy(xT[xi].t[:, :, b * 128:(b + 1) * 128], tpt.t[:].rearrange("p (k t) -> p k t", k=KD)),
                             reads=[tpt.r], writes=[xT[xi].r])
                    else:
                        S.op("dve", lambda e, b=b, tpt=tpt: e.tensor_copy(xT[xi].t[:, :, b * 128:(b + 1) * 128], tpt.t[:].rearrange("p (k t) -> p k t", k=KD)),
                             reads=[tpt.r], writes=[xT[xi].r])

            def GU(ex):
                xi, wi = ex % 2, ex % NW
                for fc in range(4):
                    pgt, put = pg_[cnt["fc"] % 2], pu_[cnt["fc"] % 2]
                    sgt = sg[cnt["fc"] % 2]
                    cnt["fc"] += 1
                    for k in range(KD):
                        S.op("pe", lambda e, k=k, fc=fc, pgt=pgt: e.matmul(pgt.t[:, 0:CAP], lhsT=wg[wi].t[:, k, fc * 128:(fc + 1) * 128], rhs=xT[xi].t[:, k, :], start=(k == 0), stop=(k == KD - 1)),
                             reads=[wg[wi].r, xT[xi].r], writes=[pgt.r])
                    for k in range(KD):
                        S.op("pe", lambda e, k=k, fc=fc, put=put: e.matmul(put.t[:, 0:CAP], lhsT=wu[wi].t[:, k, fc * 128:(fc + 1) * 128], rhs=xT[xi].t[:, k, :], start=(k == 0), stop=(k == KD - 1)),
                             reads=[wu[wi].r, xT[xi].r], writes=[put.r])
                    S.op("act", lambda e, pgt=pgt, sgt=sgt: e.activation(out=sgt.t[:], in_=pgt.t[:, 0:CAP], func=AF.Silu), reads=[pgt.r], writes=[sgt.r])
                    S.op("dve", lambda e, fc=fc, put=put, sgt=sgt: e.tensor_tensor(out=hd[xi].t[:, fc, :], in0=sgt.t[:], in1=put.t[:, 0:CAP], op=ALU.mult),
                         reads=[sgt.r, put.r], writes=[hd[xi].r])

            def Dn(ex):
                xi, wi = ex % 2, ex % NW
                for b in range(NB):
                    yot = yo[cnt["yo"] % 2]
                    cnt["yo"] += 1
                    for half in range(2):
                        pyt = py_[cnt["py"] % 2]
                        cnt["py"] += 1
                        for k in range(4):
                            S.op("pe", lambda e, k=k, b=b, half=half, pyt=pyt: e.matmul(pyt.t[:], lhsT=hd[xi].t[:, k, b * 128:(b + 1) * 128], rhs=wd[wi].t[:, k, half * 512:(half + 1) * 512], start=(k == 0), stop=(k == 3)),
                                 reads=[hd[xi].r, wd[wi].r], writes=[pyt.r])
                        if half == 0:
                            S.op("act", lambda e, pyt=pyt, yot=yot: e.copy(yot.t[:, 0:512], pyt.t[:]), reads=[pyt.r], writes=[yot.r])
                        else:
                            S.op("dve", lambda e, pyt=pyt, yot=yot: e.tensor_copy(yot.t[:, 512:1024], pyt.t[:]), reads=[pyt.r], writes=[yot.r])
                    S.op("act", lambda e, b=b, yot=yot: e.dma_start(out=Ys[ex * CAP + b * 128: ex * CAP + (b + 1) * 128, :], in_=yot.t[:]),
                         reads=[yot.r], writes=[RYs], stream=True)

            for ex in range(min(NW - 1, NE)):
                Wl(ex)
            Xl(0)
            Xl(1)
            Tr(0)
            for ex in range(NE):
                if ex + NW - 1 < NE:
                    Wl(ex + NW - 1)
                GU(ex)
                if ex + 1 < NE:
                    Tr(ex + 1)
                if ex + 2 < NE:
                    Xl(ex + 2)
                Dn(ex)

        with S.phase() as a:
            gt = Tl(a.sb("lng", [128, D], F32))
            bt = Tl(a.sb("lnb", [128, D], F32))
            cs = S.stream("lnw")
            S.op("sp", lambda e: e.dma_start(out=gt.t[:], in_=W["ln_ffn_g"][l:l + 1, :].to_broadcast([128, D])), writes=[gt.r], stream=cs)
            S.op("sp", lambda e: e.dma_start(out=bt.t[:], in_=W["ln_ffn_b"][l:l + 1, :].to_broadcast([128, D])), writes=[bt.r], stream=cs)
            NBUF = 4
            ht = [Tl(a.sb("cht%d" % i, [128, D], F32)) for i in range(NBUF)]
            y0 = [Tl(a.sb("cy0%d" % i, [128, D], F32)) for i in range(NBUF)]
            y1 = [Tl(a.sb("cy1%d" % i, [128, D], F32)) for i in range(NBUF)]
            acc = [Tl(a.sb("cacc%d" % i, [128, D], F32)) for i in range(NBUF)]
            ot = [Tl(a.sb("cot%d" % i, [128, D], F32)) for i in range(NBUF)]
            stats = [Tl(a.sb("cst%d" % i, [128, 2, 6], F32)) for i in range(NBUF)]
            mv = [Tl(a.sb("cmv%d" % i, [128, 2], F32)) for i in range(NBUF)]
            rstd = [Tl(a.sb("crs%d" % i, [128, 2], F32)) for i in range(NBUF)]
            gidx = [[Tl(a.sb("cgi%d_%d" % (i, k), [128, 1], I32)) for k in range(2)] for i in range(NBUF)]

            def tile_gen(i):
                bi = i % NBUF
                S.op("sp", lambda e: e.dma_start(out=ht[bi].t[:], in_=h_in[i * 128:(i + 1) * 128, :]), reads=[Rh_in], writes=[ht[bi].r], stream=True)
                for k, yy in enumerate((y0[bi], y1[bi])):
                    S.op("dve", lambda e, k=k: e.tensor_copy(gidx[bi][k].t[:], slots_f.t[:, i, k:k + 1]),
                         reads=[slot_r[i]], writes=[gidx[bi][k].r])
                    S.op("pool", lambda e, k=k, yy=yy: e.indirect_dma_start(
                        out=yy.t[:], out_offset=None, in_=Ys, in_offset=bass.IndirectOffsetOnAxis(ap=gidx[bi][k].t[:, :], axis=0)),
                        reads=[RYs, gidx[bi][k].r], writes=[yy.r], stream=True)
                yield
                S.op("act", lambda e: e.activation(out=acc[bi].t[:], in_=y0[bi].t[:], func=AF.Copy, scale=gates.t[:, i, 0:1]),
                     reads=[y0[bi].r, gate_r[i]], writes=[acc[bi].r])
                yield
                S.op("dve", lambda e: e.scalar_tensor_tensor(out=acc[bi].t[:], in0=y1[bi].t[:], scalar=gates.t[:, i, 1:2], in1=acc[bi].t[:], op0=ALU.mult, op1=ALU.add),
                     reads=[y1[bi].r, gate_r[i], acc[bi].r], writes=[acc[bi].r])
                S.op("dve", lambda e: e.scalar_tensor_tensor(out=acc[bi].t[:], in0=ht[bi].t[:], scalar=ALPHA, in1=acc[bi].t[:], op0=ALU.mult, op1=ALU.add),
                     reads=[ht[bi].r, acc[bi].r], writes=[acc[bi].r])
                yield from layer_norm_gen(S, acc[bi], ot[bi], gt, bt, stats[bi], mv[bi], rstd[bi])
                S.op("sp", lambda e: e.dma_start(out=h_out[i * 128:(i + 1) * 128, :], in_=ot[bi].t[:]), reads=[ot[bi].r], writes=[Rh_out], stream=True)

            interleave([(lambda i=i: tile_gen(i)) for i in range(NT)], 4)


def build_hT(S, a, h_in, Rh_in, hT, ident):
    NBUF = 2
    ht = [Tl(a.sb("bh%d" % i, [128, D], F32)) for i in range(NBUF)]
    tp = [Tl(a.ps("btp%d" % i, [128, 2, 512], F32)) for i in range(NBUF)]
    ld = [S.stream("bld%d" % i) for i in range(NBUF)]
    for i in range(NT):
        bi = i % NBUF
        S.op("sp", lambda e, i=i, bi=bi: e.dma_start(out=ht[bi].t[:], in_=h_in[i * 128:(i + 1) * 128, :]),
             reads=[Rh_in], writes=[ht[bi].r], stream=ld[bi])
        for k in range(KD):
            S.op("pe", lambda e, k=k, bi=bi: e.transpose(tp[bi].t[:, k // 4, (k % 4) * 128:(k % 4 + 1) * 128],
                                                          ht[bi].t[:, k * 128:(k + 1) * 128], ident.t[:]),
                 reads=[ht[bi].r, ident.r], writes=[tp[bi].r])
        for j in range(2):
            dst = hT.t[:, j * 4:(j + 1) * 4, i * 128:(i + 1) * 128]
            src = tp[bi].t[:, j, :].rearrange("p (k t) -> p k t", k=4)
            if j == 0:
                S.op("act", lambda e, dst=dst, src=src: e.copy(dst, src), reads=[tp[bi].r], writes=[hT.r])
            else:
                S.op("dve", lambda e, dst=dst, src=src: e.tensor_copy(dst, src), reads=[tp[bi].r], writes=[hT.r])


def outproj_ln_phase(nc, S, YT_src, RYT, Wout_ap, h_in, Rh_in, h_out, Rh_out, g_ap, b_ap):
    with S.phase() as a:
        YT = Tl(a.sb("YT", [128, KD, T], BF16))
        ys = S.stream("yt")
        for k in range(KD):
            S.op("sp", lambda e, k=k: e.dma_start(out=YT.t[:, k, :], in_=YT_src[k]), reads=[RYT], writes=[YT.r], stream=ys)
        wo = Tl(a.sb("wo", [128, KD, D], BF16))
        wos = S.stream("wo")
        S.op("pool", lambda e: e.dma_start(out=wo.t[:], in_=Wout_ap.rearrange("(k p) f -> p k f", p=128)), writes=[wo.r], stream=wos)
        gt = Tl(a.sb("lng", [128, D], F32))
        bt = Tl(a.sb("lnb", [128, D], F32))
        S.op("sp", lambda e: e.dma_start(out=gt.t[:], in_=g_ap.to_broadcast([128, D])), writes=[gt.r], stream=wos)
        S.op("sp", lambda e: e.dma_start(out=bt.t[:], in_=b_ap.to_broadcast([128, D])), writes=[bt.r], stream=wos)
        NBUF = 4
        ht = [Tl(a.sb("oh%d" % i, [128, D], F32)) for i in range(NBUF)]
        acc = [Tl(a.sb("oacc%d" % i, [128, D], F32)) for i in range(NBUF)]
        ot = [Tl(a.sb("oot%d" % i, [128, D], F32)) for i in range(NBUF)]
        stats = [Tl(a.sb("ost%d" % i, [128, 2, 6], F32)) for i in range(NBUF)]
        mv = [Tl(a.sb("omv%d" % i, [128, 2], F32)) for i in range(NBUF)]
        rstd = [Tl(a.sb("ors%d" % i, [128, 2], F32)) for i in range(NBUF)]
        pm = [Tl(a.ps("opm%d" % i, [128, 2, 512], F32)) for i in range(NBUF)]

        def tile_gen(i):
            bi = i % NBUF
            S.op("sp", lambda e: e.dma_start(out=ht[bi].t[:], in_=h_in[i * 128:(i + 1) * 128, :]), reads=[Rh_in], writes=[ht[bi].r], stream=True)
            for half in range(2):
                for k in range(KD):
                    S.op("pe", lambda e, k=k, half=half: e.matmul(pm[bi].t[:, half, :], lhsT=YT.t[:, k, i * 128:(i + 1) * 128], rhs=wo.t[:, k, half * 512:(half + 1) * 512], start=(k == 0), stop=(k == KD - 1)),
                         reads=[YT.r, wo.r], writes=[pm[bi].r])
            yield
            S.op("dve", lambda e: e.scalar_tensor_tensor(out=acc[bi].t[:], in0=ht[bi].t[:], scalar=ALPHA, in1=pm[bi].t[:].rearrange("p a b -> p (a b)"), op0=ALU.mult, op1=ALU.add),
                 reads=[ht[bi].r, pm[bi].r], writes=[acc[bi].r])
            yield from layer_norm_gen(S, acc[bi], ot[bi], gt, bt, stats[bi], mv[bi], rstd[bi])
            S.op("sp", lambda e: e.dma_start(out=h_out[i * 128:(i + 1) * 128, :], in_=ot[bi].t[:]), reads=[ot[bi].r], writes=[Rh_out], stream=True)

        interleave([(lambda i=i: tile_gen(i)) for i in range(NT)], 4)


DILS = (1, 4, 16)


def attn_stage(nc, S, h_in, h_out, W, l, scr, Rh_in, Rh_out):
    QTs, KTs, Vs, OTs = scr["QTs"], scr["KTs"], scr["Vs"], scr["OTs"]
    RQ, RK, RV, RO = Res(), Res(), Res(), Res()
    wqkv = W["w_qkv_c"][0]
    with S.phase() as a:
        c = make_consts(S, a)
        hT = Tl(a.sb("hT", [128, KD, T], BF16))
        build_hT(S, a, h_in, Rh_in, hT, c["ident"])
        wq = [Tl(a.sb("wqkv%d" % j, [128, KD, D], BF16)) for j in range(3)]
        wsm = S.stream("wqkv")
        for j in range(3):
            S.op("pool", lambda e, j=j: e.dma_start(out=wq[j].t[:], in_=wqkv[:, j * D:(j + 1) * D].rearrange("(k p) f -> p k f", p=128)), writes=[wq[j].r], stream=wsm)
        stg = [Tl(a.sb("stg%d" % i, [128, T], BF16)) for i in range(2)]
        sst = [S.stream("sst%d" % i) for i in range(2)]
        pp = [Tl(a.ps("pp%d" % i, [128, 512], F32)) for i in range(4)]
        npp = 0
        nst = 0
        for which, dst, Rd in ((0, QTs, RQ), (1, KTs, RK)):
            for hp in range(8):
                sg = stg[nst % 2]
                ss = sst[nst % 2]
                nst += 1
                for tc in range(8):
                    p = pp[npp % 4]
                    npp += 1
                    for k in range(KD):
                        S.op("pe", lambda e, k=k, hp=hp, tc=tc, p=p, which=which: e.matmul(p.t[:], lhsT=wq[which].t[:, k, hp * 128:(hp + 1) * 128], rhs=hT.t[:, k, tc * 512:(tc + 1) * 512], start=(k == 0), stop=(k == KD - 1)),
                             reads=[wq[which].r, hT.r], writes=[p.r])
                    if tc % 2 == 0:
                        S.op("act", lambda e, tc=tc, p=p, sg=sg: e.copy(sg.t[:, tc * 512:(tc + 1) * 512], p.t[:]), reads=[p.r], writes=[sg.r])
                    else:
                        S.op("dve", lambda e, tc=tc, p=p, sg=sg: e.tensor_copy(sg.t[:, tc * 512:(tc + 1) * 512], p.t[:]), reads=[p.r], writes=[sg.r])
                S.op("sp", lambda e, hp=hp, sg=sg, dst=dst: e.dma_start(out=dst[hp], in_=sg.t[:]), reads=[sg.r], writes=[Rd], stream=ss)
        vst = [Tl(a.sb("vst%d" % i, [128, D], BF16)) for i in range(2)]
        vss = [S.stream("vss%d" % i) for i in range(2)]
        nv = 0
        for di, d in enumerate(DILS[:1]):
            nbr = T // d // 128
            hTv = hT.t[:].rearrange("p k (m s) -> p k m s", s=d)
            for r in range(d):
                for b in range(nbr):
                    nb = r * nbr + b
                    vt = vst[nv % 2]
                    vs_ = vss[nv % 2]
                    nv += 1
                    for half in range(2):
                        p = pp[npp % 4]
                        npp += 1
                        for k in range(KD):
                            S.op("pe", lambda e, k=k, b=b, r=r, half=half, p=p, hTv=hTv: e.matmul(p.t[:], lhsT=hTv[:, k, b * 128:(b + 1) * 128, r], rhs=wq[2].t[:, k, half * 512:(half + 1) * 512], start=(k == 0), stop=(k == KD - 1)),
                                 reads=[hT.r, wq[2].r], writes=[p.r])
                        if half == 0:
                            S.op("act", lambda e, p=p, vt=vt: e.copy(vt.t[:, 0:512], p.t[:]), reads=[p.r], writes=[vt.r])
                        else:
                            S.op("dve", lambda e, p=p, vt=vt: e.tensor_copy(vt.t[:, 512:1024], p.t[:]), reads=[p.r], writes=[vt.r])
                    S.op("sp", lambda e, di=di, nb=nb, vt=vt: e.dma_start(out=Vs[di, nb * 128:(nb + 1) * 128, :], in_=vt.t[:]), reads=[vt.r], writes=[RV], stream=vs_)

    with S.phase() as a:
        for di, d in enumerate(DILS):
            if di == 0:
                continue
            for r in range(d):
                S.op("sp" if r % 2 == 0 else "act", lambda e, di=di, d=d, r=r: e.dma_start(out=Vs[di, r * (T // d):(r + 1) * (T // d), :],
                                                                                          in_=Vs[0].rearrange("(m s) c -> s m c", s=d)[r]),
                     reads=[RV], writes=[RV], stream=True)

    with S.phase() as a:
        c = make_consts(S, a)
        identb = c["identb"]
        negm = Tl(a.sb("mask01", [128, 512], BF16))
        S.op("pool", lambda e: e.memset(negm.t[:], 1.0), writes=[negm.r])
        for hh in range(2):
            S.op("pool", lambda e, hh=hh: e.affine_select(out=negm.t[:, 256 * hh:256 * hh + 128], in_=negm.t[:, 256 * hh:256 * hh + 128], pattern=[[1, 128]], compare_op=ALU.is_ge, fill=0.0, base=0, channel_multiplier=-1),
                 reads=[negm.r], writes=[negm.r])
            S.op("pool", lambda e, hh=hh: e.affine_select(out=negm.t[:, 256 * hh + 128:256 * hh + 256], in_=negm.t[:, 256 * hh + 128:256 * hh + 256], pattern=[[-1, 128]], compare_op=ALU.is_ge, fill=0.0, base=0, channel_multiplier=1),
                 reads=[negm.r], writes=[negm.r])
        ones = Tl(a.sb("ones", [128, 64], BF16))
        S.op("pool", lambda e: e.memset(ones.t[:], 1.0), writes=[ones.r])
        NQ = 2
        qt = [Tl(a.sb("qt%d" % i, [128, T], BF16)) for i in range(NQ)]
        kt = [Tl(a.sb("kt%d" % i, [128, T], BF16)) for i in range(NQ)]
        vv = [[Tl(a.sb("vv%d_%d" % (i, j), [128, NT, 128], BF16)) for j in range(3)] for i in range(NQ)]
        lds = [[S.stream("al%d_%d" % (i, j)) for j in range(5)] for i in range(NQ)]
        oacc = Tl(a.sb("oacc", [128, T], F32))
        sacc = Tl(a.sb("sacc", [128, T], F32))
        otb = [Tl(a.sb("otb%d" % i, [128, T], BF16)) for i in range(2)]
        ots = [S.stream("ots%d" % i) for i in range(2)]
        NPT = 6
        PT = [Tl(a.sb("PT%d" % i, [128, 512], BF16)) for i in range(NPT)]
        sT = [Tl(a.ps("sT%d" % i, [128, 2, 512], F32)) for i in range(2)]
        po = [Tl(a.ps("po%d" % i, [128, 512], F32)) for i in range(2)]
        pS = [Tl(a.ps("pS%d" % i, [128, 512], F32)) for i in range(2)]
        nsT = 0
        nPT = 0
        npo = 0
        for hp in range(8):
            qi = hp % NQ
            Q, K_, V3 = qt[qi], kt[qi], vv[qi]
            S.op("sp", lambda e, hp=hp, Q=Q: e.dma_start(out=Q.t[:], in_=QTs[hp]), reads=[RQ], writes=[Q.r], stream=lds[qi][0])
            S.op("sp", lambda e, hp=hp, K_=K_: e.dma_start(out=K_.t[:], in_=KTs[hp]), reads=[RK], writes=[K_.r], stream=lds[qi][1])
            for di in range(3):
                S.op("act", lambda e, hp=hp, di=di, V3=V3: e.dma_start(out=V3[di].t[:], in_=Vs[di, :, hp * 128:(hp + 1) * 128].rearrange("(b p) c -> p b c", p=128)),
                     reads=[RV], writes=[V3[di].r], stream=lds[qi][2 + di])
            items = []
            for di, d in enumerate(DILS):
                nbr = T // d // 128
                for r in range(d):
                    for b in range(nbr):
                        items.append((di, d, nbr, r, b))
            LAG = 2
            pts = {}
            pvstate = {"pot": None, "pst": None}

            def score_part(k, Q=Q, K_=K_):
                nonlocal nsT, nPT
                di, d, nbr, r, b = items[k]
                Qv = Q.t[:].rearrange("p (m s) -> p m s", s=d)
                Kv = K_.t[:].rearrange("p (m s) -> p m s", s=d)
                nq = 256 if b + 1 < nbr else 128
                st_ = sT[nsT % 2]
                nsT += 1
                for h in range(2):
                    rows = slice(64 * h, 64 * h + 64)
                    S.op("pe", lambda e, h=h, rows=rows: e.matmul(st_.t[:, h, 0:nq], lhsT=Kv[rows, b * 128:(b + 1) * 128, r], rhs=Qv[rows, b * 128:b * 128 + nq, r], start=True, stop=True),
                         reads=[K_.r, Q.r], writes=[st_.r])
                pt = PT[nPT % NPT]
                nPT += 1
                S.op("act", lambda e: e.activation(out=pt.t[:].rearrange("p (h q) -> p h q", h=2)[:, :, 0:nq], in_=st_.t[:, :, 0:nq], func=AF.Exp, scale=0.125),
                     reads=[st_.r], writes=[pt.r])
                if nq == 256:
                    S.op("dve", lambda e: e.tensor_tensor(out=pt.t[:], in0=pt.t[:], in1=negm.t[:], op=ALU.mult), reads=[pt.r, negm.r], writes=[pt.r])
                else:
                    for hh in range(2):
                        S.op("dve", lambda e, hh=hh: e.tensor_tensor(out=pt.t[:, 256 * hh:256 * hh + 128], in0=pt.t[:, 256 * hh:256 * hh + 128], in1=negm.t[:, 256 * hh:256 * hh + 128], op=ALU.mult),
                             reads=[pt.r, negm.r], writes=[pt.r])
                pts[k] = pt

            def pv_part(k, V3=V3):
                nonlocal npo
                di, d, nbr, r, b = items[k]
                Ov = oacc.t[:].rearrange("p (m s) -> p m s", s=d)
                Sv = sacc.t[:].rearrange("p (m s) -> p m s", s=d)
                nb = r * nbr + b
                pt = pts[k]
                prevPT = pts[k - 1] if b > 0 else None
                g = b % 4
                if g == 0:
                    pvstate["pot"], pvstate["pst"] = po[npo % 2], pS[npo % 2]
                    npo += 1
                pot, pst = pvstate["pot"], pvstate["pst"]
                for h in range(2):
                    rows = slice(64 * h, 64 * h + 64)
                    cols = slice(g * 128, (g + 1) * 128)
                    for (dstp, lhs_cur, lhs_prev) in ((pot, V3[di].t[:, nb, rows], V3[di].t[:, nb - 1, rows] if b > 0 else None), (pst, ones.t[:], ones.t[:] if b > 0 else None)):
                        S.op("pe", lambda e, dstp=dstp, rows=rows, cols=cols, lhs_cur=lhs_cur, h=h: e.matmul(dstp.t[rows, cols], lhsT=lhs_cur, rhs=pt.t[:, 256 * h:256 * h + 128], start=True, stop=(b == 0)),
                             reads=[V3[di].r, ones.r, pt.r], writes=[dstp.r])
                        if b > 0:
                            S.op("pe", lambda e, dstp=dstp, rows=rows, cols=cols, lhs_prev=lhs_prev, h=h: e.matmul(dstp.t[rows, cols], lhsT=lhs_prev, rhs=prevPT.t[:, 256 * h + 128:256 * h + 256], start=False, stop=True),
                                 reads=[V3[di].r, ones.r, prevPT.r], writes=[dstp.r])
                if g == 3 or b == nbr - 1:
                    b0 = b - g
                    n = (g + 1) * 128
                    for (acc_v, src, accT) in ((Ov, pot, oacc), (Sv, pst, sacc)):
                        dst = acc_v[:, b0 * 128:b0 * 128 + n, r]
                        if di == 0:
                            S.op("dve", lambda e, dst=dst, src=src, n=n: e.tensor_copy(dst, src.t[:, 0:n]), reads=[src.r], writes=[accT.r])
                        else:
                            S.op("dve", lambda e, dst=dst, src=src, n=n: e.tensor_tensor(out=dst, in0=src.t[:, 0:n], in1=dst, op=ALU.add), reads=[src.r, accT.r], writes=[accT.r])
                pts.pop(k - 1, None)

            for k in range(len(items) + LAG):
                if k < len(items):
                    score_part(k)
                if k - LAG >= 0:
                    pv_part(k - LAG)
            ob = otb[hp % 2]
            S.op("dve", lambda e: e.reciprocal(sacc.t[:], sacc.t[:]), reads=[sacc.r], writes=[sacc.r])
            S.op("dve", lambda e, ob=ob: e.tensor_tensor(out=ob.t[:], in0=oacc.t[:], in1=sacc.t[:], op=ALU.mult), reads=[oacc.r, sacc.r], writes=[ob.r])
            S.op("sp", lambda e, hp=hp, ob=ob: e.dma_start(out=OTs[hp], in_=ob.t[:]), reads=[ob.r], writes=[RO], stream=ots[hp % 2])

    outproj_ln_phase(nc, S, OTs, RO, W["w_out_c"][0], h_in, Rh_in, h_out, Rh_out, W["ln_mix_g"][l:l + 1, :], W["ln_mix_b"][l:l + 1, :])


class Ring:
    def __init__(self, tiles):
        self.tiles = tiles
        self.i = 0

    def get(self):
        t = self.tiles[self.i % len(self.tiles)]
        self.i += 1
        return t


class View:
    def __init__(self, ap, res=None):
        self.ap = ap
        self.r = res if res is not None else Res()


def l0_inproj(nc, S, h_in, Rh_in, W, scr, R, ba_sb, ba_r):
    P0T, ZS = scr["P0T"], scr["ZS"]
    w_in = W["w_in_ab"][0]
    with S.phase() as a:
        c = make_consts(S, a)
        hT = Tl(a.sb("hT", [128, KD, T], BF16))
        build_hT(S, a, h_in, Rh_in, hT, c["ident"])
        NCOL = 2568
        win = Tl(a.sb("win", [128, KD, NCOL], BF16))
        wsm = S.stream("win")
        for k in range(KD):
            S.op("pool", lambda e, k=k: e.dma_start(out=win.t[:, k, :], in_=w_in[k * 128:(k + 1) * 128, :]), writes=[win.r], stream=wsm)
        stg = [Tl(a.sb("stg%d" % i, [128, T], F32)) for i in range(2)]
        sst = [S.stream("sst%d" % i) for i in range(2)]
        pp = [Tl(a.ps("pp%d" % i, [128, 512], F32)) for i in range(3)]
        npp = 0
        chunks = [(h, h * 128) for h in range(4)] + [(4 + h, 512 + h * 128) for h in range(4)] + \
                 [(8 + h, 1024 + h * 128) for h in range(4)] + [(12 + cq, 2056 + cq * 128) for cq in range(4)]
        for ci, (dst, col0) in enumerate(chunks):
            sg, ss = stg[ci % 2], sst[ci % 2]
            for tc in range(8):
                p = pp[npp % 3]
                npp += 1
                for k in range(KD):
                    S.op("pe", lambda e, k=k, col0=col0, tc=tc, p=p: e.matmul(p.t[:], lhsT=win.t[:, k, col0:col0 + 128], rhs=hT.t[:, k, tc * 512:(tc + 1) * 512], start=(k == 0), stop=(k == KD - 1)),
                         reads=[win.r, hT.r], writes=[p.r])
                if tc % 2 == 0:
                    S.op("act", lambda e, tc=tc, p=p, sg=sg: e.copy(sg.t[:, tc * 512:(tc + 1) * 512], p.t[:]), reads=[p.r], writes=[sg.r])
                else:
                    S.op("dve", lambda e, tc=tc, p=p, sg=sg: e.tensor_copy(sg.t[:, tc * 512:(tc + 1) * 512], p.t[:]), reads=[p.r], writes=[sg.r])
            S.op("sp", lambda e, dst=dst, sg=sg: e.dma_start(out=P0T[dst], in_=sg.t[:]), reads=[sg.r], writes=[R["P0T"]], stream=ss)
        zst = [Tl(a.sb("zst%d" % i, [128, 512], F32)) for i in range(2)]
        zss = [S.stream("zss%d" % i) for i in range(2)]
        pb = [Tl(a.ps("pb%d" % i, [128, 512], F32)) for i in range(1)]
        for i in range(NT):
            p = pp[npp % 3]
            npp += 1
            p2 = pb[0]
            for k in range(KD):
                S.op("pe", lambda e, k=k, i=i, p=p: e.matmul(p.t[:], lhsT=hT.t[:, k, i * 128:(i + 1) * 128], rhs=win.t[:, k, 1536:2048], start=(k == 0), stop=(k == KD - 1)),
                     reads=[win.r, hT.r], writes=[p.r])
            for k in range(KD):
                S.op("pe", lambda e, k=k, i=i, p2=p2: e.matmul(p2.t[:, 0:8], lhsT=hT.t[:, k, i * 128:(i + 1) * 128], rhs=win.t[:, k, 2048:2056], start=(k == 0), stop=(k == KD - 1)),
                     reads=[win.r, hT.r], writes=[p2.r])
            zt = zst[i % 2]
            S.op("act", lambda e, p=p, zt=zt: e.activation(out=zt.t[:], in_=p.t[:], func=AF.Silu), reads=[p.r], writes=[zt.r])
            S.op("dve", lambda e, i=i, p2=p2: e.tensor_copy(ba_sb[:, i, :], p2.t[:, 0:8]), reads=[p2.r], writes=[ba_r])
            S.op("sp", lambda e, i=i, zt=zt: e.dma_start(out=ZS[i * 128:(i + 1) * 128, :], in_=zt.t[:]), reads=[zt.r], writes=[R["ZS"]], stream=zss[i % 2])


def l0_gdn(nc, S, W, scr, R, ba_sb, ba_r):
    P0T, ZS, YTs = scr["P0T"], scr["ZS"], scr["YTs"]
    C = 128
    with S.phase() as a:
        c = make_consts(S, a)
        ident = c["ident"]
        cs = S.stream("gconst")
        ones = Tl(a.sb("ones", [128, 128], F32))
        S.op("pool", lambda e: e.memset(ones.t[:], 1.0), writes=[ones.r])
        UT = Tl(a.sb("UT", [128, 128], F32))
        S.op("pool", lambda e: e.memset(UT.t[:], 1.0), writes=[UT.r])
        S.op("pool", lambda e: e.affine_select(out=UT.t[:], in_=UT.t[:], pattern=[[1, 128]], compare_op=ALU.is_ge, fill=0.0, base=0, channel_multiplier=-1),
             reads=[UT.r], writes=[UT.r])
        mge = UT
        mlt = Tl(a.sb("mlt", [128, 128], F32))
        S.op("pool", lambda e: e.memset(mlt.t[:], 1.0), writes=[mlt.r])
        S.op("pool", lambda e: e.affine_select(out=mlt.t[:], in_=mlt.t[:], pattern=[[-1, 128]], compare_op=ALU.is_gt, fill=0.0, base=0, channel_multiplier=1),
             reads=[mlt.r], writes=[mlt.r])
        cwT = Tl(a.sb("cwT", [4, 1536], F32))
        S.op("sp", lambda e: e.dma_start(out=cwT.t[:], in_=W["conv_qkv"][0]), writes=[cwT.r], stream=cs)
        cw = Tl(a.sb("cw", [128, 12, 4], F32))
        pcw = Tl(a.ps("pcw", [128, 512], F32))
        for cc in range(12):
            S.op("pe", lambda e, cc=cc: e.transpose(pcw.t[:, cc * 4:(cc + 1) * 4], cwT.t[0:4, cc * 128:(cc + 1) * 128], ident.t[0:4, 0:4]),
                 reads=[cwT.r, ident.r], writes=[pcw.r])
        S.op("dve", lambda e: e.tensor_copy(cw.t[:].rearrange("p c j -> p (c j)"), pcw.t[:, 0:48]), reads=[pcw.r], writes=[cw.r])
        alog = Tl(a.sb("alog", [128, 4], F32))
        dtb = Tl(a.sb("dtb", [128, 4], F32))
        nw = Tl(a.sb("nw", [128, 128], F32))
        S.op("sp", lambda e: e.dma_start(out=alog.t[:], in_=W["gdn_a_log"][0:1, :].to_broadcast([128, 4])), writes=[alog.r], stream=cs)
        S.op("sp", lambda e: e.dma_start(out=dtb.t[:], in_=W["gdn_dt_bias"][0:1, :].to_broadcast([128, 4])), writes=[dtb.r], stream=cs)
        S.op("sp", lambda e: e.dma_start(out=nw.t[:], in_=W["gdn_norm"][0:1, :].to_broadcast([128, 128])), writes=[nw.r], stream=cs)
        nexpA = Tl(a.sb("nexpA", [128, 4], F32))
        S.op("act", lambda e: e.activation(out=nexpA.t[:], in_=alog.t[:], func=AF.Exp), reads=[alog.r], writes=[nexpA.r])
        S.op("dve", lambda e: e.tensor_scalar(out=nexpA.t[:], in0=nexpA.t[:], scalar1=-1.0, scalar2=None, op0=ALU.mult), reads=[nexpA.r], writes=[nexpA.r])

        G = 4
        NG = NT // G
        pre = [Tl(a.sb("pre%d" % i, [128, T + 3], F32)) for i in range(1)]
        prs = [S.stream("prs%d" % i) for i in range(1)]
        S.op("pool", lambda e: e.memset(pre[0].t[:, 0:3], 0.0), writes=[pre[0].r])
        QT = Tl(a.sb("QT", [128, T], F32))
        KT = Tl(a.sb("KT", [128, T], F32))
        VT = Tl(a.sb("VT", [128, T], F32))
        zhr = Ring([Tl(a.sb("zh%d" % i, [128, G, 128], F32)) for i in range(2)])
        zs_ = S.stream("zh")
        yst = [Tl(a.sb("yst%d" % i, [128, T], BF16)) for i in range(1)]
        yss = [S.stream("yss%d" % i) for i in range(1)]
        beta_all = Tl(a.sb("beta_all", [128, NT], F32))
        nbeta_all = Tl(a.sb("nbeta_all", [128, NT], F32))
        g_all = Tl(a.sb("g_all", [128, NT], F32))
        gtmp = Tl(a.sb("gtmp", [128, NT], F32))
        sqb = [Tl(a.sb("sqb%d" % i, [128, 512], F32)) for i in range(2)]
        rsb = [Tl(a.sb("rsb%d" % i, [128, 512], F32)) for i in range(2)]
        Sst = Tl(a.sb("Sst", [128, 128], F32))
        ring = Ring([Tl(a.sb("rg%d" % i, [128, G, 128], F32)) for i in range(16)])
        keep = Ring([Tl(a.sb("kp%d" % i, [128, G, 128], F32)) for i in range(28)])
        colring = Ring([Tl(a.sb("cr%d" % i, [128, G], F32)) for i in range(40)])
        psb = [a.ps("gps%d" % i, [128, G, 128], F32) for i in range(7)]
        psring = Ring([View(psb[i][:, :, :]) for i in range(6)])
        po_bank = View(psb[6][:, :, :])
        mge_b = mge.t[:].unsqueeze(1).to_broadcast([128, G, 128])
        mlt_b = mlt.t[:].unsqueeze(1).to_broadcast([128, G, 128])
        ident_b = ident.t[:].unsqueeze(1).to_broadcast([128, G, 128])
        nw_b = nw.t[:].unsqueeze(1).to_broadcast([128, G, 128])
        Vv = lambda fn, rd, wr: S.op("dve", fn, reads=[x.r for x in rd], writes=[x.r for x in wr])
        Aa = lambda fn, rd, wr: S.op("act", fn, reads=[x.r for x in rd], writes=[x.r for x in wr])
        Pe = lambda fn, rd, wr: S.op("pe", fn, reads=[x.r for x in rd], writes=[x.r for x in wr])

        def bc(colap):
            return colap.unsqueeze(2).to_broadcast([128, G, 128])

        for h in range(DBG.get('gdn_heads', 4)):
            for which, (dstT, src_idx) in enumerate(((QT, h), (KT, 4 + h), (VT, 8 + h))):
                pr, ps_ = pre[0], prs[0]
                S.op("sp", lambda e, src_idx=src_idx, pr=pr: e.dma_start(out=pr.t[:, 3:T + 3], in_=P0T[src_idx]), reads=[R["P0T"]], writes=[pr.r], stream=ps_)
                cc = which * 4 + h
                S.op("dve", lambda e, pr=pr, dstT=dstT, cc=cc: e.tensor_scalar(out=dstT.t[:], in0=pr.t[:, 0:T], scalar1=cw.t[:, cc, 0:1], scalar2=None, op0=ALU.mult),
                     reads=[pr.r, cw.r], writes=[dstT.r])
                for j in range(1, 4):
                    S.op("dve", lambda e, pr=pr, dstT=dstT, cc=cc, j=j: e.scalar_tensor_tensor(out=dstT.t[:], in0=pr.t[:, j:T + j], scalar=cw.t[:, cc, j:j + 1], in1=dstT.t[:], op0=ALU.mult, op1=ALU.add),
                         reads=[pr.r, cw.r, dstT.r], writes=[dstT.r])
                S.op("act", lambda e, dstT=dstT: e.activation(out=dstT.t[:], in_=dstT.t[:], func=AF.Silu), reads=[dstT.r], writes=[dstT.r])
                if which < 2:
                    for tb in range(8):
                        sl = slice(tb * 512, (tb + 1) * 512)
                        sq, rs = sqb[tb % 2], rsb[tb % 2]
                        pq = psring.get()
                        pqv = pq.ap.rearrange("p g c -> p (g c)")
                        Aa(lambda e, dstT=dstT, sl=sl, sq=sq: e.activation(out=sq.t[:], in_=dstT.t[:, sl], func=AF.Square), [dstT], [sq])
                        Pe(lambda e, sq=sq, pqv=pqv: e.matmul(pqv, lhsT=ones.t[:], rhs=sq.t[:], start=True, stop=True), [ones, sq], [pq])
                        Vv(lambda e, rs=rs, pqv=pqv: e.tensor_scalar(out=rs.t[:], in0=pqv, scalar1=1e-6, scalar2=None, op0=ALU.add), [pq], [rs])
                        Aa(lambda e, rs=rs, which=which: e.activation(out=rs.t[:], in_=rs.t[:], func=AF.Sqrt, scale=(128.0 if which == 0 else 1.0)), [rs], [rs])
                        Vv(lambda e, rs=rs: e.reciprocal(rs.t[:], rs.t[:]), [rs], [rs])
                        Vv(lambda e, dstT=dstT, sl=sl, rs=rs: e.tensor_tensor(out=dstT.t[:, sl], in0=dstT.t[:, sl], in1=rs.t[:], op=ALU.mult), [dstT, rs], [dstT])
            S.op("act", lambda e, h=h: e.activation(out=beta_all.t[:], in_=ba_sb[:, :, h], func=AF.Exp, scale=-1.0), reads=[ba_r], writes=[beta_all.r])
            Vv(lambda e: e.tensor_scalar(out=beta_all.t[:], in0=beta_all.t[:], scalar1=1.0, scalar2=None, op0=ALU.add), [beta_all], [beta_all])
            Vv(lambda e: e.reciprocal(beta_all.t[:], beta_all.t[:]), [beta_all], [beta_all])
            Vv(lambda e: e.tensor_scalar(out=nbeta_all.t[:], in0=beta_all.t[:], scalar1=-1.0, scalar2=None, op0=ALU.mult), [beta_all], [nbeta_all])
            S.op("act", lambda e, h=h: e.activation(out=gtmp.t[:], in_=ba_sb[:, :, 4 + h], func=AF.Exp, bias=dtb.t[:, h:h + 1], scale=1.0), reads=[ba_r, dtb.r], writes=[gtmp.r])
            Aa(lambda e: e.activation(out=gtmp.t[:], in_=gtmp.t[:], func=AF.Ln, bias=1.0, scale=1.0), [gtmp], [gtmp])
            Vv(lambda e, h=h: e.tensor_scalar(out=g_all.t[:], in0=gtmp.t[:], scalar1=nexpA.t[:, h:h + 1], scalar2=None, op0=ALU.mult), [gtmp, nexpA], [g_all])
            S.op("pool", lambda e: e.memset(Sst.t[:], 0.0), writes=[Sst.r])
            ys_t = yst[0]

            def pre_group(gi):
                n0 = gi * G
                gsl = slice(n0 * C, (n0 + G) * C)
                gcols = slice(n0, n0 + G)
                d = {"gi": gi}
                Ktw, Vtw = ring.get(), ring.get()
                for (src, dst_) in ((KT, Ktw), (VT, Vtw)):
                    p = psring.get()
                    for c_ in range(G):
                        Pe(lambda e, src=src, p=p, c_=c_, n0=n0: e.transpose(p.ap[:, c_, :], src.t[:, (n0 + c_) * C:(n0 + c_ + 1) * C], ident.t[:]), [src, ident], [p])
                    Aa(lambda e, p=p, dst_=dst_: e.copy(dst_.t[:], p.ap), [p], [dst_])
                gbcw = ring.get()
                Vv(lambda e, gbcw=gbcw, gcols=gcols: e.tensor_copy(gbcw.t[:], bc(g_all.t[:, gcols])), [g_all], [gbcw])
                pcol = psring.get()
                Pe(lambda e, pcol=pcol, gcols=gcols: e.matmul(pcol.ap[:, 0, 0:G], lhsT=UT.t[:], rhs=g_all.t[:, gcols], start=True, stop=True), [UT, g_all], [pcol])
                prow = psring.get()
                for c_ in range(G):
                    Pe(lambda e, prow=prow, gbcw=gbcw, c_=c_: e.matmul(prow.ap[:, c_, :], lhsT=gbcw.t[:, c_, :], rhs=UT.t[:], start=True, stop=True), [UT, gbcw], [prow])
                gcc = colring.get()
                ngcc = colring.get()
                Vv(lambda e, gcc=gcc, pcol=pcol: e.tensor_copy(gcc.t[:], pcol.ap[:, 0, 0:G]), [pcol], [gcc])
                Vv(lambda e, gcc=gcc, ngcc=ngcc: e.tensor_scalar(out=ngcc.t[:], in0=gcc.t[:], scalar1=-1.0, scalar2=None, op0=ALU.mult), [gcc], [ngcc])
                egc = colring.get()
                Aa(lambda e, egc=egc, gcc=gcc: e.activation(out=egc.t[:], in_=gcc.t[:], func=AF.Exp), [gcc], [egc])
                egrw = ring.get()
                Aa(lambda e, egrw=egrw, prow=prow: e.activation(out=egrw.t[:], in_=prow.ap, func=AF.Exp), [prow], [egrw])
                glc = colring.get()
                Aa(lambda e, glc=glc, prow=prow: e.copy(glc.t[:], prow.ap[:, :, C - 1]), [prow], [glc])
                Dtw, Dmw = ring.get(), ring.get()
                for c_ in range(G):
                    Aa(lambda e, Dtw=Dtw, prow=prow, ngcc=ngcc, c_=c_: e.activation(out=Dtw.t[:, c_, :], in_=prow.ap[:, c_, :], func=AF.Exp, bias=ngcc.t[:, c_:c_ + 1], scale=1.0), [prow, ngcc], [Dtw])
                    Aa(lambda e, Dmw=Dmw, prow=prow, gcc=gcc, c_=c_: e.activation(out=Dmw.t[:, c_, :], in_=prow.ap[:, c_, :], func=AF.Exp, bias=gcc.t[:, c_:c_ + 1], scale=-1.0), [prow, gcc], [Dmw])
                Vv(lambda e, Dtw=Dtw: e.scalar_tensor_tensor(out=Dtw.t[:], in0=Dtw.t[:], scalar=1.0, in1=mge_b, op0=ALU.min, op1=ALU.mult), [Dtw, mge], [Dtw])
                Vv(lambda e, Dmw=Dmw: e.scalar_tensor_tensor(out=Dmw.t[:], in0=Dmw.t[:], scalar=1.0, in1=mlt_b, op0=ALU.min, op1=ALU.mult), [Dmw, mlt], [Dmw])
                Vv(lambda e, Dmw=Dmw, gcols=gcols: e.tensor_tensor(out=Dmw.t[:], in0=Dmw.t[:], in1=bc(nbeta_all.t[:, gcols]), op=ALU.mult), [Dmw, nbeta_all], [Dmw])
                vbw, kbgw, kdecw, qdTw = keep.get(), keep.get(), keep.get(), keep.get()
                Vv(lambda e, vbw=vbw, Vtw=Vtw, gcols=gcols: e.tensor_tensor(out=vbw.t[:], in0=Vtw.t[:], in1=bc(beta_all.t[:, gcols]), op=ALU.mult), [Vtw, beta_all], [vbw])
                bg = colring.get()
                Vv(lambda e, bg=bg, egc=egc, gcols=gcols: e.tensor_tensor(out=bg.t[:], in0=beta_all.t[:, gcols], in1=egc.t[:], op=ALU.mult), [beta_all, egc], [bg])
                Vv(lambda e, kbgw=kbgw, Ktw=Ktw, bg=bg: e.tensor_tensor(out=kbgw.t[:], in0=Ktw.t[:], in1=bc(bg.t[:]), op=ALU.mult), [Ktw, bg], [kbgw])
                kd = colring.get()
                Vv(lambda e, kd=kd, glc=glc, gcc=gcc: e.tensor_tensor(out=kd.t[:], in0=glc.t[:], in1=gcc.t[:], op=ALU.subtract), [glc, gcc], [kd])
                Aa(lambda e, kd=kd: e.activation(out=kd.t[:], in_=kd.t[:], func=AF.Exp), [kd], [kd])
                Vv(lambda e, kdecw=kdecw, Ktw=Ktw, kd=kd: e.tensor_tensor(out=kdecw.t[:], in0=Ktw.t[:], in1=bc(kd.t[:]), op=ALU.mult), [Ktw, kd], [kdecw])
                Vv(lambda e, qdTw=qdTw, egrw=egrw, gsl=gsl: e.tensor_tensor(out=qdTw.t[:], in0=QT.t[:, gsl].rearrange("p (g c) -> p g c", g=G), in1=egrw.t[:], op=ALU.mult), [QT, egrw], [qdTw])
                glast = colring.get()
                Aa(lambda e, glast=glast, glc=glc: e.activation(out=glast.t[:], in_=glc.t[:], func=AF.Exp), [glc], [glast])
                pkk = psring.get()
                for c_ in range(G):
                    Pe(lambda e, pkk=pkk, c_=c_, n0=n0: e.matmul(pkk.ap[:, c_, :], lhsT=KT.t[:, (n0 + c_) * C:(n0 + c_ + 1) * C], rhs=KT.t[:, (n0 + c_) * C:(n0 + c_ + 1) * C], start=True, stop=True), [KT], [pkk])
                M = ring.get()
                Vv(lambda e, M=M, pkk=pkk, Dmw=Dmw: e.tensor_tensor(out=M.t[:], in0=pkk.ap, in1=Dmw.t[:], op=ALU.mult), [pkk, Dmw], [M])
                pnt = psring.get()
                for c_ in range(G):
                    Pe(lambda e, pnt=pnt, M=M, c_=c_: e.transpose(pnt.ap[:, c_, :], M.t[:, c_, :], ident.t[:]), [M, ident], [pnt])
                MT = ring.get()
                Aa(lambda e, MT=MT, pnt=pnt: e.copy(MT.t[:], pnt.ap), [pnt], [MT])
                X = ring.get()
                Vv(lambda e, X=X, MT=MT: e.tensor_tensor(out=X.t[:], in0=MT.t[:], in1=ident_b, op=ALU.add), [MT, ident], [X])
                pqk = psring.get()
                for c_ in range(G):
                    Pe(lambda e, pqk=pqk, c_=c_, n0=n0: e.matmul(pqk.ap[:, c_, :], lhsT=KT.t[:, (n0 + c_) * C:(n0 + c_ + 1) * C], rhs=QT.t[:, (n0 + c_) * C:(n0 + c_ + 1) * C], start=True, stop=True), [KT, QT], [pqk])
                QKmTw = keep.get()
                Vv(lambda e, QKmTw=QKmTw, pqk=pqk, Dtw=Dtw: e.tensor_tensor(out=QKmTw.t[:], in0=pqk.ap, in1=Dtw.t[:], op=ALU.mult), [pqk, Dtw], [QKmTw])
                for lvl in range(1, 7):
                    pm = psring.get()
                    for c_ in range(G):
                        Pe(lambda e, pm=pm, M=M, MT=MT, c_=c_: e.matmul(pm.ap[:, c_, :], lhsT=MT.t[:, c_, :], rhs=M.t[:, c_, :], start=True, stop=True), [M, MT], [pm])
                    M2 = ring.get()
                    Aa(lambda e, M2=M2, pm=pm: e.copy(M2.t[:], pm.ap), [pm], [M2])
                    if lvl < 6:
                        pmt = psring.get()
                        for c_ in range(G):
                            Pe(lambda e, pmt=pmt, M=M, MT=MT, c_=c_: e.matmul(pmt.ap[:, c_, :], lhsT=M.t[:, c_, :], rhs=MT.t[:, c_, :], start=True, stop=True), [M, MT], [pmt])
                        MT2 = ring.get()
                        Vv(lambda e, MT2=MT2, pmt=pmt: e.tensor_copy(MT2.t[:], pmt.ap), [pmt], [MT2])
                    else:
                        MT2 = None
                    px = psring.get()
                    for c_ in range(G):
                        Pe(lambda e, px=px, M2=M2, X=X, c_=c_: e.matmul(px.ap[:, c_, :], lhsT=M2.t[:, c_, :], rhs=X.t[:, c_, :], start=True, stop=True), [M2, X], [px])
                    X2 = ring.get()
                    Vv(lambda e, X2=X2, px=px, X=X: e.tensor_tensor(out=X2.t[:], in0=px.ap, in1=X.t[:], op=ALU.add), [px, X], [X2])
                    M, MT, X = M2, MT2, X2
                Tt = X
                pu = psring.get()
                for c_ in range(G):
                    Pe(lambda e, pu=pu, Tt=Tt, vbw=vbw, c_=c_: e.matmul(pu.ap[:, c_, :], lhsT=Tt.t[:, c_, :], rhs=vbw.t[:, c_, :], start=True, stop=True), [Tt, vbw], [pu])
                U0w = keep.get()
                Aa(lambda e, U0w=U0w, pu=pu: e.copy(U0w.t[:], pu.ap), [pu], [U0w])
                pw = psring.get()
                for c_ in range(G):
                    Pe(lambda e, pw=pw, Tt=Tt, kbgw=kbgw, c_=c_: e.matmul(pw.ap[:, c_, :], lhsT=kbgw.t[:, c_, :], rhs=Tt.t[:, c_, :], start=True, stop=True), [Tt, kbgw], [pw])
                WTw = keep.get()
                Aa(lambda e, WTw=WTw, pw=pw: e.copy(WTw.t[:], pw.ap), [pw], [WTw])
                zht = zhr.get()
                S.op("sp", lambda e, zht=zht, gsl=gsl, h=h: e.dma_start(out=zht.t[:], in_=ZS[gsl, h * 128:(h + 1) * 128].rearrange("(g p) c -> p g c", p=128)), reads=[R["ZS"]], writes=[zht.r], stream=zs_)
                d.update(U0=U0w, WT=WTw, qdT=qdTw, QKmT=QKmTw, kdec=kdecw, glast=glast, zh=zht)
                return d

            def chain_group(d, ys_t=ys_t):
                gi = d["gi"]
                n0 = gi * G
                gsl = slice(n0 * C, (n0 + G) * C)
                po_ = po_bank
                vnw = ring.get()
                for c_ in range(G):
                    p1 = psring.get()
                    Pe(lambda e, p1=p1, d=d, c_=c_: e.matmul(p1.ap[:, 0, :], lhsT=d["WT"].t[:, c_, :], rhs=Sst.t[:], start=True, stop=True), [d["WT"], Sst], [p1])
                    Vv(lambda e, vnw=vnw, p1=p1, d=d, c_=c_: e.tensor_tensor(out=vnw.t[:, c_, :], in0=d["U0"].t[:, c_, :], in1=p1.ap[:, 0, :], op=ALU.subtract), [d["U0"], p1], [vnw])
                    Pe(lambda e, po_=po_, d=d, c_=c_: e.matmul(po_.ap[:, c_, :], lhsT=d["qdT"].t[:, c_, :], rhs=Sst.t[:], start=True, stop=False), [d["qdT"], Sst], [po_])
                    Pe(lambda e, po_=po_, d=d, vnw=vnw, c_=c_: e.matmul(po_.ap[:, c_, :], lhsT=d["QKmT"].t[:, c_, :], rhs=vnw.t[:, c_, :], start=False, stop=True), [d["QKmT"], vnw], [po_])
                    ps_s = psring.get()
                    Pe(lambda e, ps_s=ps_s, d=d, vnw=vnw, c_=c_: e.matmul(ps_s.ap[:, 0, :], lhsT=d["kdec"].t[:, c_, :], rhs=vnw.t[:, c_, :], start=True, stop=True), [d["kdec"], vnw], [ps_s])
                    Vv(lambda e, ps_s=ps_s, d=d, c_=c_: e.scalar_tensor_tensor(out=Sst.t[:], in0=Sst.t[:], scalar=d["glast"].t[:, c_:c_ + 1], in1=ps_s.ap[:, 0, :], op0=ALU.mult, op1=ALU.add),
                       [Sst, d["glast"], ps_s], [Sst])
                ow, sqw = ring.get(), ring.get()
                ssq = colring.get()
                Aa(lambda e, ow=ow, po_=po_: e.copy(ow.t[:], po_.ap), [po_], [ow])
                Aa(lambda e, ow=ow, sqw=sqw: e.activation(out=sqw.t[:], in_=ow.t[:], func=AF.Square), [ow], [sqw])
                Vv(lambda e, ssq=ssq, sqw=sqw: e.reduce_sum(out=ssq.t[:], in_=sqw.t[:], axis=AX.X), [sqw], [ssq])
                Vv(lambda e, ssq=ssq: e.tensor_scalar(out=ssq.t[:], in0=ssq.t[:], scalar1=1.0 / 128.0, scalar2=1e-6, op0=ALU.mult, op1=ALU.add), [ssq], [ssq])
                Aa(lambda e, ssq=ssq: e.activation(out=ssq.t[:], in_=ssq.t[:], func=AF.Sqrt), [ssq], [ssq])
                Vv(lambda e, ssq=ssq: e.reciprocal(ssq.t[:], ssq.t[:]), [ssq], [ssq])
                Vv(lambda e, ow=ow, ssq=ssq: e.tensor_tensor(out=ow.t[:], in0=ow.t[:], in1=bc(ssq.t[:]), op=ALU.mult), [ow, ssq], [ow])
                Vv(lambda e, ow=ow: e.tensor_tensor(out=ow.t[:], in0=ow.t[:], in1=nw_b, op=ALU.mult), [ow, nw], [ow])
                Vv(lambda e, ow=ow, d=d: e.tensor_tensor(out=ow.t[:], in0=ow.t[:], in1=d["zh"].t[:], op=ALU.mult), [ow, d["zh"]], [ow])
                pt = psring.get()
                for c_ in range(G):
                    Pe(lambda e, pt=pt, ow=ow, c_=c_: e.transpose(pt.ap[:, c_, :], ow.t[:, c_, :], ident.t[:]), [ow, ident], [pt])
                Aa(lambda e, pt=pt, gsl=gsl, ys_t=ys_t: e.copy(ys_t.t[:, gsl], pt.ap.rearrange("p g c -> p (g c)")), [pt], [ys_t])

            NGR = DBG.get('gdn_groups', NG)
            pend = pre_group(0)
            for gi in range(NGR):
                nxt = pre_group(gi + 1) if gi + 1 < NGR else None
                chain_group(pend)
                pend = nxt
            S.op("sp", lambda e, h=h, ys_t=ys_t: e.dma_start(out=YTs[h], in_=ys_t.t[:]), reads=[ys_t.r], writes=[R["YTs"]], stream=yss[0])

TWO_PI = 6.283185307179586


def l0_s5(nc, S, W, scr, R):
    P0T, YTs = scr["P0T"], scr["YTs"]
    TB = 512
    NBK = T // TB
    with S.phase() as a:
        c = make_consts(S, a)
        ident = c["ident"]
        cs = S.stream("s5c")
        cs2 = S.stream("s5c2")
        prmT = Tl(a.sb("prmT", [16, 3, 128], F32))
        S.op("sp", lambda e: e.dma_start(out=prmT.t[:, 0, :], in_=W["s5_lam_re"][0].rearrange("(t g) p -> t (g p)", g=2)), writes=[prmT.r], stream=cs)
        S.op("sp", lambda e: e.dma_start(out=prmT.t[:, 1, :], in_=W["s5_lam_im"][0].rearrange("(t g) p -> t (g p)", g=2)), writes=[prmT.r], stream=cs)
        ldt2 = Tl(a.sb("ldt2", [16, 2], F32))
        S.op("sp", lambda e: e.dma_start(out=ldt2.t[:], in_=W["s5_log_dt"][0].rearrange("(t g) -> t g", g=2)), writes=[ldt2.r], stream=cs)
        S.op("dve", lambda e: e.tensor_copy(prmT.t[:, 2, :].rearrange("t (g p) -> t g p", g=2), ldt2.t[:].unsqueeze(2).to_broadcast([16, 2, 64])),
             reads=[ldt2.r], writes=[prmT.r])
        dT = Tl(a.sb("dT", [4, 128], F32))
        S.op("sp", lambda e: e.dma_start(out=dT.t[:], in_=W["s5_d"][0].rearrange("(c p) -> c p", p=128)), writes=[dT.r], stream=cs)
        pp0 = Tl(a.ps("pp0", [128, 512], F32))
        for j in range(3):
            S.op("pe", lambda e, j=j: e.transpose(pp0.t[:, j * 16:(j + 1) * 16], prmT.t[0:16, j, :], ident.t[0:16, 0:16]), reads=[prmT.r, ident.r], writes=[pp0.r])
        S.op("pe", lambda e: e.transpose(pp0.t[:, 48:52], dT.t[0:4, :], ident.t[0:4, 0:4]), reads=[dT.r, ident.r], writes=[pp0.r])
        prm = Tl(a.sb("prm", [128, 52], F32))
        S.op("dve", lambda e: e.tensor_copy(prm.t[:], pp0.t[:, 0:52]), reads=[pp0.r], writes=[prm.r])
        lr, li, ldt, dsk = prm.t[:, 0:16], prm.t[:, 16:32], prm.t[:, 32:48], prm.t[:, 48:52]
        sm = {}
        for nm in ["dt", "lrdt", "th", "mag", "y", "ay", "sn", "cs", "are", "aim", "den", "t1", "t2", "cr", "ci", "nr"]:
            sm[nm] = Tl(a.sb("s5_" + nm, [128, 16], F32))
        ni16 = Tl(a.sb("s5_ni", [128, 16], I32))
        Vv = lambda fn, rd, wr: S.op("dve", fn, reads=[x.r for x in rd], writes=[x.r for x in wr])
        Aa = lambda fn, rd, wr: S.op("act", fn, reads=[x.r for x in rd], writes=[x.r for x in wr])
        Aa(lambda e: e.activation(out=sm["dt"].t[:], in_=ldt, func=AF.Exp), [prm], [sm["dt"]])
        Vv(lambda e: e.tensor_tensor(out=sm["lrdt"].t[:], in0=lr, in1=sm["dt"].t[:], op=ALU.mult), [prm, sm["dt"]], [sm["lrdt"]])
        Vv(lambda e: e.tensor_tensor(out=sm["th"].t[:], in0=li, in1=sm["dt"].t[:], op=ALU.mult), [prm, sm["dt"]], [sm["th"]])
        Aa(lambda e: e.activation(out=sm["mag"].t[:], in_=sm["lrdt"].t[:], func=AF.Exp), [sm["lrdt"]], [sm["mag"]])

        def trig(ang, n_i, y, ay, sn, cs_, rd):
            Vv(lambda e: e.tensor_scalar(out=n_i.t[:], in0=ang.t[:], scalar1=1.0 / TWO_PI, scalar2=None, op0=ALU.mult), rd + [ang], [n_i])
            Vv(lambda e: e.scalar_tensor_tensor(out=y.t[:], in0=n_i.t[:], scalar=-TWO_PI, in1=ang.t[:], op0=ALU.mult, op1=ALU.add), [n_i, ang], [y])
            Aa(lambda e: e.activation(out=sn.t[:], in_=y.t[:], func=AF.Sin, scale=0.999999), [y], [sn])
            Aa(lambda e: e.activation(out=ay.t[:], in_=y.t[:], func=AF.Abs), [y], [ay])
            Aa(lambda e: e.activation(out=cs_.t[:], in_=ay.t[:], func=AF.Sin, bias=halfpi.t[:, 0:1], scale=-0.999999), [ay, halfpi], [cs_])

        halfpi = Tl(a.sb("halfpi", [128, 1], F32))
        S.op("pool", lambda e: e.memset(halfpi.t[:], 1.5707963), writes=[halfpi.r])
        trig(sm["th"], ni16, sm["y"], sm["ay"], sm["sn"], sm["cs"], [])
        Vv(lambda e: e.tensor_tensor(out=sm["are"].t[:], in0=sm["mag"].t[:], in1=sm["cs"].t[:], op=ALU.mult), [sm["mag"], sm["cs"]], [sm["are"]])
        Vv(lambda e: e.tensor_tensor(out=sm["aim"].t[:], in0=sm["mag"].t[:], in1=sm["sn"].t[:], op=ALU.mult), [sm["mag"], sm["sn"]], [sm["aim"]])
        Vv(lambda e: e.tensor_tensor(out=sm["den"].t[:], in0=lr, in1=lr, op=ALU.mult), [prm], [sm["den"]])
        Vv(lambda e: e.tensor_tensor(out=sm["t1"].t[:], in0=li, in1=li, op=ALU.mult), [prm], [sm["t1"]])
        Vv(lambda e: e.tensor_tensor(out=sm["den"].t[:], in0=sm["den"].t[:], in1=sm["t1"].t[:], op=ALU.add), [sm["den"], sm["t1"]], [sm["den"]])
        Vv(lambda e: e.reciprocal(sm["den"].t[:], sm["den"].t[:]), [sm["den"]], [sm["den"]])
        Vv(lambda e: e.tensor_scalar(out=sm["nr"].t[:], in0=sm["are"].t[:], scalar1=-1.0, scalar2=None, op0=ALU.add), [sm["are"]], [sm["nr"]])
        Vv(lambda e: e.tensor_tensor(out=sm["t1"].t[:], in0=sm["nr"].t[:], in1=lr, op=ALU.mult), [sm["nr"], prm], [sm["t1"]])
        Vv(lambda e: e.tensor_tensor(out=sm["t2"].t[:], in0=sm["aim"].t[:], in1=li, op=ALU.mult), [sm["aim"], prm], [sm["t2"]])
        Vv(lambda e: e.tensor_tensor(out=sm["cr"].t[:], in0=sm["t1"].t[:], in1=sm["t2"].t[:], op=ALU.add), [sm["t1"], sm["t2"]], [sm["cr"]])
        Vv(lambda e: e.tensor_tensor(out=sm["cr"].t[:], in0=sm["cr"].t[:], in1=sm["den"].t[:], op=ALU.mult), [sm["cr"], sm["den"]], [sm["cr"]])
        Vv(lambda e: e.tensor_tensor(out=sm["t1"].t[:], in0=sm["aim"].t[:], in1=lr, op=ALU.mult), [sm["aim"], prm], [sm["t1"]])
        Vv(lambda e: e.tensor_tensor(out=sm["t2"].t[:], in0=sm["nr"].t[:], in1=li, op=ALU.mult), [sm["nr"], prm], [sm["t2"]])
        Vv(lambda e: e.tensor_tensor(out=sm["ci"].t[:], in0=sm["t1"].t[:], in1=sm["t2"].t[:], op=ALU.subtract), [sm["t1"], sm["t2"]], [sm["ci"]])
        Vv(lambda e: e.tensor_tensor(out=sm["ci"].t[:], in0=sm["ci"].t[:], in1=sm["den"].t[:], op=ALU.mult), [sm["ci"], sm["den"]], [sm["ci"]])
        c0 = Tl(a.sb("c0", [128, 16, NBK], F32))
        for blk in range(NBK):
            Vv(lambda e, blk=blk: e.tensor_scalar(out=c0.t[:, :, blk], in0=sm["th"].t[:], scalar1=float(blk * TB), scalar2=None, op0=ALU.mult), [sm["th"]], [c0])
        bre = Tl(a.sb("bre", [128, 16, 16], F32))
        bim = Tl(a.sb("bim", [128, 16, 16], F32))
        for g in range(2):
            S.op("sp", lambda e, g=g: e.dma_start(out=bre.t[g * 64:(g + 1) * 64, :, :], in_=W["s5_b_re"][0].rearrange("(t g) p h -> g p t h", g=2)[g]), writes=[bre.r], stream=cs2)
            S.op("sp", lambda e, g=g: e.dma_start(out=bim.t[g * 64:(g + 1) * 64, :, :], in_=W["s5_b_im"][0].rearrange("(t g) p h -> g p t h", g=2)[g]), writes=[bim.r], stream=cs2)
        bbr = Tl(a.sb("bbr", [128, 16, 16], F32))
        bbi = Tl(a.sb("bbi", [128, 16, 16], F32))
        tmpb = Tl(a.sb("tmpb", [128, 16, 16], F32))
        crb = sm["cr"].t[:].unsqueeze(2).to_broadcast([128, 16, 16])
        cib = sm["ci"].t[:].unsqueeze(2).to_broadcast([128, 16, 16])
        Vv(lambda e: e.tensor_tensor(out=bbr.t[:], in0=bre.t[:], in1=crb, op=ALU.mult), [bre, sm["cr"]], [bbr])
        Vv(lambda e: e.tensor_tensor(out=tmpb.t[:], in0=bim.t[:], in1=cib, op=ALU.mult), [bim, sm["ci"]], [tmpb])
        Vv(lambda e: e.tensor_tensor(out=bbr.t[:], in0=bbr.t[:], in1=tmpb.t[:], op=ALU.subtract), [bbr, tmpb], [bbr])
        Vv(lambda e: e.tensor_tensor(out=bbi.t[:], in0=bim.t[:], in1=crb, op=ALU.mult), [bim, sm["cr"]], [bbi])
        Vv(lambda e: e.tensor_tensor(out=tmpb.t[:], in0=bre.t[:], in1=cib, op=ALU.mult), [bre, sm["ci"]], [tmpb])
        Vv(lambda e: e.tensor_tensor(out=bbi.t[:], in0=bbi.t[:], in1=tmpb.t[:], op=ALU.add), [bbi, tmpb], [bbi])
        BBT = [[Tl(a.sb("BBT%d_%d" % (cp, st), [128, 128], F32)) for st in range(16)] for cp in range(2)]
        CT = [[Tl(a.sb("CT%d_%d" % (cp, st), [128, 128], F32)) for st in range(16)] for cp in range(2)]
        bx = [Tl(a.sb("bx%d" % i, [128, 128], F32)) for i in range(2)]
        cx = [Tl(a.sb("cx%d" % i, [128, 128], F32)) for i in range(2)]
        cxs = [S.stream("cx%d" % i) for i in range(2)]
        class _PV:
            pass
        ptp = []
        for i in range(2):
            pv = _PV()
            pv.t = pp0.t[:, 128 * (i + 1):128 * (i + 2)]
            pv.r = pp0.r
            ptp.append(pv)
        k = 0
        for st in range(16):
            s = st % 4
            cq = st // 4
            for cp, bb in ((0, bbr), (1, bbi)):
                b_, p_ = bx[k % 2], ptp[k % 2]
                S.op("pool", lambda e, b_=b_: e.memset(b_.t[:], 0.0), writes=[b_.r])
                for g in range(2):
                    S.op("pool", lambda e, b_=b_, g=g, s=s, st=st, bb=bb: e.tensor_copy(b_.t[g * 64:(g + 1) * 64, 32 * s + 16 * g:32 * s + 16 * g + 16], bb.t[g * 64:(g + 1) * 64, st, :]),
                         reads=[bb.r], writes=[b_.r])
                S.op("pe", lambda e, b_=b_, p_=p_: e.transpose(p_.t[:], b_.t[:], ident.t[:]), reads=[b_.r, ident.r], writes=[p_.r])
                S.op("act", lambda e, p_=p_, cp=cp, st=st: e.copy(BBT[cp][st].t[:], p_.t[:]), reads=[p_.r], writes=[BBT[cp][st].r])
                k += 1
            for cp, cw_ in ((0, W["s5_c_re"][0]), (1, W["s5_c_im"][0])):
                c_, p_ = cx[k % 2], ptp[k % 2]
                S.op("pool", lambda e, c_=c_: e.memset(c_.t[:], 0.0), writes=[c_.r])
                for g in range(2):
                    gg = 2 * st + g
                    r0 = (gg - 8 * cq) * 16
                    S.op("sp", lambda e, c_=c_, g=g, gg=gg, r0=r0, cw_=cw_: e.dma_start(out=c_.t[r0:r0 + 16, g * 64:(g + 1) * 64], in_=cw_[gg]), writes=[c_.r], stream=cxs[k % 2])
                S.op("pe", lambda e, c_=c_, p_=p_: e.transpose(p_.t[:], c_.t[:], ident.t[:]), reads=[c_.r, ident.r], writes=[p_.r])
                if cp == 0:
                    S.op("act", lambda e, p_=p_, st=st: e.copy(CT[0][st].t[:], p_.t[:]), reads=[p_.r], writes=[CT[0][st].r])
                else:
                    S.op("act", lambda e, p_=p_, st=st: e.mul(CT[1][st].t[:], p_.t[:], -1.0), reads=[p_.r], writes=[CT[1][st].r])
                k += 1
        iot = Tl(a.sb("iot", [128, TB], F32))
        S.op("pool", lambda e: e.iota(iot.t[:], pattern=[[1, TB]], base=0, channel_multiplier=0, allow_small_or_imprecise_dtypes=True), writes=[iot.r])
        uT = [Tl(a.sb("uT%d" % i, [128, T], F32)) for i in range(1)]
        yg = Tl(a.sb("yg", [128, 4, T], BF16))
        ring = Ring([Tl(a.sb("s5r%d" % i, [128, TB], F32)) for i in range(40)])
        iring = Ring([Tl(a.sb("s5i%d" % i, [128, TB], I32)) for i in range(4)])
        cring = Ring([Tl(a.sb("s5c%d" % i, [128, 1], F32)) for i in range(24)])
        pbu = [Tl(a.ps("pbu%d" % i, [128, 512], F32)) for i in range(4)]
        pyy = [Tl(a.ps("pyy%d" % i, [128, 512], F32)) for i in range(2)]
        npb = [0]
        npy = [0]
        for cq in range(4):
            u = uT[0]
            S.op("sp", lambda e, cq=cq, u=u: e.dma_start(out=u.t[:], in_=P0T[12 + cq]), reads=[R["P0T"]], writes=[u.r], stream=True)
            cars = {}
            pys = {}

            def it_gen(blk, s, cq=cq, u=u, cars=cars, pys=pys):
                sl = slice(blk * TB, (blk + 1) * TB)
                st = 4 * cq + s
                if s == 0:
                    pys[blk] = pyy[npy[0] % 2]
                    npy[0] += 1
                py = pys[blk]
                pr, pi = pbu[npb[0] % 4], pbu[(npb[0] + 1) % 4]
                npb[0] += 2
                S.op("pe", lambda e: e.matmul(pr.t[:], lhsT=BBT[0][st].t[:], rhs=u.t[:, sl], start=True, stop=True), reads=[BBT[0][st].r, u.r], writes=[pr.r])
                S.op("pe", lambda e: e.matmul(pi.t[:], lhsT=BBT[1][st].t[:], rhs=u.t[:, sl], start=True, stop=True), reads=[BBT[1][st].r, u.r], writes=[pi.r])
                bur, bui, y, ay, sn, cs_ = [ring.get() for _ in range(6)]
                n_i = iring.get()
                Aa(lambda e: e.activation(out=y.t[:], in_=iot.t[:], func=AF.Identity, scale=sm["th"].t[:, st:st + 1], bias=c0.t[:, st, blk:blk + 1]),
                   [iot, sm["th"], c0], [y])
                Aa(lambda e: e.copy(bur.t[:], pr.t[:]), [pr], [bur])
                Aa(lambda e: e.copy(bui.t[:], pi.t[:]), [pi], [bui])
                yield
                Vv(lambda e: e.tensor_scalar(out=n_i.t[:], in0=y.t[:], scalar1=1.0 / TWO_PI, scalar2=None, op0=ALU.mult), [y], [n_i])
                Vv(lambda e: e.scalar_tensor_tensor(out=y.t[:], in0=n_i.t[:], scalar=-TWO_PI, in1=y.t[:], op0=ALU.mult, op1=ALU.add), [n_i, y], [y])
                Aa(lambda e: e.activation(out=sn.t[:], in_=y.t[:], func=AF.Sin, scale=0.999999), [y], [sn])
                Aa(lambda e: e.activation(out=ay.t[:], in_=y.t[:], func=AF.Abs), [y], [ay])
                Aa(lambda e: e.activation(out=cs_.t[:], in_=ay.t[:], func=AF.Sin, bias=halfpi.t[:, 0:1], scale=-0.999999), [ay, halfpi], [cs_])
                yield
                TT = lambda o_, a_, b_, op: Vv(lambda e: e.tensor_tensor(out=o_.t[:], in0=a_.t[:], in1=b_.t[:], op=op), [a_, b_], [o_])
                t1, t2, t3, t4 = [ring.get() for _ in range(4)]
                TT(t1, cs_, bur, ALU.mult)
                TT(t2, sn, bui, ALU.mult)
                TT(t1, t1, t2, ALU.add)
                TT(t3, cs_, bui, ALU.mult)
                TT(t2, sn, bur, ALU.mult)
                TT(t3, t3, t2, ALU.subtract)
                rbc = sm["mag"].t[:, st:st + 1].to_broadcast([128, TB])
                car_r, car_i = cars.get(s, (None, None))
                for (gout, zin, car) in ((t2, t1, car_r), (t4, t3, car_i)):
                    if car is None:
                        Vv(lambda e, gout=gout, zin=zin: e.tensor_tensor_scan(out=gout.t[:], data0=rbc, data1=zin.t[:], initial=0.0, op0=ALU.mult, op1=ALU.add), [sm["mag"], zin], [gout])
                    else:
                        Vv(lambda e, gout=gout, zin=zin, car=car: e.tensor_tensor_scan(out=gout.t[:], data0=rbc, data1=zin.t[:], initial=car.t[:, 0:1], op0=ALU.mult, op1=ALU.add), [sm["mag"], zin, car], [gout])
                gr, gi = t2, t4
                ncr, nci = cring.get(), cring.get()
                S.op("pool", lambda e: e.tensor_copy(ncr.t[:], gr.t[:, TB - 1:TB]), reads=[gr.r], writes=[ncr.r])
                S.op("pool", lambda e: e.tensor_copy(nci.t[:], gi.t[:, TB - 1:TB]), reads=[gi.r], writes=[nci.r])
                cars[s] = (ncr, nci)
                TT(t1, cs_, gr, ALU.mult)
                TT(t3, sn, gi, ALU.mult)
                TT(t1, t1, t3, ALU.subtract)
                TT(t3, sn, gr, ALU.mult)
                TT(bur, cs_, gi, ALU.mult)
                TT(t3, t3, bur, ALU.add)
                S.op("pe", lambda e: e.matmul(py.t[:], lhsT=CT[0][st].t[:], rhs=t1.t[:], start=(s == 0), stop=False), reads=[CT[0][st].r, t1.r], writes=[py.r])
                S.op("pe", lambda e: e.matmul(py.t[:], lhsT=CT[1][st].t[:], rhs=t3.t[:], start=False, stop=(s == 3)), reads=[CT[1][st].r, t3.r], writes=[py.r])
                if s == 3:
                    yb = ring.get()
                    Vv(lambda e: e.scalar_tensor_tensor(out=yb.t[:], in0=u.t[:, sl], scalar=dsk[:, cq:cq + 1], in1=py.t[:], op0=ALU.mult, op1=ALU.add), [u, prm, py], [yb])
                    Aa(lambda e: e.activation(out=yg.t[:, cq, sl], in_=yb.t[:], func=AF.Gelu), [yb], [yg])

            interleave([(lambda blk=blk, s=s: it_gen(blk, s)) for blk in range(NBK) for s in range(4)], 3)
        wgl = Tl(a.sb("wgl", [128, 4, 512], BF16))
        S.op("pool", lambda e: e.dma_start(out=wgl.t[:], in_=W["s5_w_glu"][0].rearrange("(k p) f -> p k f", p=128)), writes=[wgl.r], stream=cs2)
        ygs = [Tl(a.sb("ygs%d" % i, [128, T], BF16)) for i in range(2)]
        ygss = [S.stream("ygss%d" % i) for i in range(2)]
        for oc in range(4):
            og = ygs[oc % 2]
            for tcb in range(8):
                sl = slice(tcb * 512, (tcb + 1) * 512)
                py = pyy[npy[0] % 2]
                npy[0] += 1
                for kc in range(4):
                    S.op("pe", lambda e, py=py, kc=kc, oc=oc, sl=sl: e.matmul(py.t[:], lhsT=wgl.t[:, kc, oc * 128:(oc + 1) * 128], rhs=yg.t[:, kc, sl], start=(kc == 0), stop=(kc == 3)),
                         reads=[wgl.r, yg.r], writes=[py.r])
                sg = ring.get()
                Aa(lambda e, sg=sg, py=py: e.activation(out=sg.t[:], in_=py.t[:], func=AF.Exp, scale=-1.0), [py], [sg])
                Vv(lambda e, sg=sg: e.tensor_scalar(out=sg.t[:], in0=sg.t[:], scalar1=1.0, scalar2=None, op0=ALU.add), [sg], [sg])
                Vv(lambda e, sg=sg: e.reciprocal(sg.t[:], sg.t[:]), [sg], [sg])
                Vv(lambda e, sg=sg, og=og, oc=oc, sl=sl: e.tensor_tensor(out=og.t[:, sl], in0=sg.t[:], in1=yg.t[:, oc, sl], op=ALU.mult), [sg, yg], [og])
            S.op("sp", lambda e, oc=oc, og=og: e.dma_start(out=YTs[4 + oc], in_=og.t[:]), reads=[og.r], writes=[R["YTs"]], stream=ygss[oc % 2])


def l0_stage(nc, S, h_in, h_out, W, l, scr, Rh_in, Rh_out, parts=("inproj", "gdn", "s5", "out")):
    R = {"P0T": Res(), "ZS": Res(), "YTs": Res()}
    with ExitStack() as st0:
        ba = st0.enter_context(nc.sbuf_tensor("ba_sb", [128, NT, 8], F32))
        ba_r = Res()
        if "inproj" in parts:
            l0_inproj(nc, S, h_in, Rh_in, W, scr, R, ba, ba_r)
        else:
            with S.phase() as a:
                S.op("pool", lambda e: e.memset(ba[:], -0.5), writes=[ba_r])
        if "gdn" in parts:
            l0_gdn(nc, S, W, scr, R, ba, ba_r)
        if "s5" in parts:
            l0_s5(nc, S, W, scr, R)
        if "out" in parts:
            outproj_ln_phase(nc, S, scr["YTs"], R["YTs"], W["w_out_ab"][0], h_in, Rh_in, h_out, Rh_out, W["ln_mix_g"][l:l + 1, :], W["ln_mix_b"][l:l + 1, :])


W_SHAPES = [
    ("w_in_ab", [1, D, 2568]), ("conv_qkv", [1, 4, 1536]), ("gdn_a_log", [1, 4]), ("gdn_dt_bias", [1, 4]), ("gdn_norm", [1, 128]),
    ("s5_lam_re", [1, 32, 64]), ("s5_lam_im", [1, 32, 64]), ("s5_log_dt", [1, 32]), ("s5_b_re", [1, 32, 64, 16]), ("s5_b_im", [1, 32, 64, 16]),
    ("s5_c_re", [1, 32, 16, 64]), ("s5_c_im", [1, 32, 16, 64]), ("s5_d", [1, 512]), ("s5_w_glu", [1, 512, 512]), ("w_out_ab", [1, D, D]),
    ("w_qkv_c", [1, D, 3 * D]), ("w_out_c", [1, D, D]), ("ln_mix_g", [2, D]), ("ln_mix_b", [2, D]),
    ("router_group_w", [2, D, 4]), ("router_group_b", [2, 4]), ("router_expert_w", [2, D, 32]), ("router_expert_b", [2, 32]),
    ("moe_w_gate", [2, 32, D, FF]), ("moe_w_up", [2, 32, D, FF]), ("moe_w_down", [2, 32, FF, D]), ("ln_ffn_g", [2, D]), ("ln_ffn_b", [2, D]),
]


def build_program(stages=("l0", "moe0", "attn", "moe1")):
    nc = bass.Bass("TRN2", target_bir_lowering=False)
    x = nc.dram_tensor("x", [T, D], F32, kind="ExternalInput").ap()
    out = nc.dram_tensor("out", [T, D], F32, kind="ExternalOutput").ap()
    W = {nm: nc.dram_tensor(nm, sh, F32, kind="ExternalInput").ap() for nm, sh in W_SHAPES}
    I = lambda n, sh, dt: nc.dram_tensor(n, sh, dt, kind="Internal").ap()
    scr = {
        "Xs": I("Xs", [NSLOT + 128, D], BF16), "Ys": I("Ys", [NSLOT, D], F32),
        "QTs": I("QTs", [8, 128, T], BF16), "KTs": I("KTs", [8, 128, T], BF16), "Vs": I("Vs", [3, T, D], BF16), "OTs": I("OTs", [8, 128, T], BF16),
        "P0T": I("P0T", [16, 128, T], F32), "ZS": I("ZS", [T, 512], F32), "YTs": I("YTs", [8, 128, T], BF16),
    }
    names = list(stages)
    bufs = [x]
    for i in range(len(names) - 1):
        bufs.append(I("hbuf%d" % i, [T, D], F32))
    bufs.append(out)
    with ExitStack() as st:
        S = Sched(nc, st)
        for i, nm in enumerate(names):
            hi, ho = bufs[i], bufs[i + 1]
            Ri, Ro = Res(), Res()
            if nm == "l0":
                l0_stage(nc, S, hi, ho, W, 0, scr, Ri, Ro)
            elif nm == "moe0":
                moe_stage(nc, S, hi, ho, W, 0, scr, Ri, Ro)
            elif nm == "attn":
                attn_stage(nc, S, hi, ho, W, 1, scr, Ri, Ro)
            elif nm == "moe1":
                moe_stage(nc, S, hi, ho, W, 1, scr, Ri, Ro)
        with S.phase(final=True) as a:
            pass
    return nc


_PROG = {}


def kernel(**inputs):
    x = np.ascontiguousarray(np.asarray(inputs["x"], dtype=np.float32))
    B = x.shape[0]
    if "full" not in _PROG:
        _PROG["full"] = build_program()
    nc = _PROG["full"]
    wmap = {nm: np.ascontiguousarray(np.asarray(inputs[nm], dtype=np.float32)) for nm, _ in W_SHAPES}
    in_maps = []
    for b in range(B):
        m = dict(wmap)
        m["x"] = x[b]
        in_maps.append(m)
    res = run_bass_kernel_spmd(nc, in_maps, core_ids=list(range(B)))
    return np.stack([np.asarray(r["out"], dtype=np.float32) for r in res.results], axis=0)
```
